# Optimizing a Trainium2 kernel written in Bass

```python
import math
import jax, jax.numpy as jnp
from jax import lax
import numpy as np

D_MODEL = 2048
BATCH = 8
SEQ = 2048
DEPTH = 4

GRID_W = 64
CTX_LEN = 256
N_MIXERS = 3
N_ATTN_LAYERS = (DEPTH + 2) // 3
N_RET_LAYERS = (DEPTH + 1) // 3
N_HYENA_LAYERS = DEPTH // 3
N_MOD = 6
EPS = 1e-6

ATTN_HEADS = 16
ATTN_KV_HEADS = 4
ATTN_HEAD_DIM = D_MODEL // ATTN_HEADS
Q_BLOCK = 128
ROPE_THETA = 10000.0

RET_HEADS = 8
RET_QK_DIM = D_MODEL // RET_HEADS
RET_V_DIM = 2 * RET_QK_DIM
RET_CHUNK = 128

HYENA_EMB = 33
HYENA_FILTER_WIDTH = 64
HYENA_TARGET = 1e-2
HYENA_FAST = 0.3
HYENA_SLOW = 1.5
HYENA_SHIFT = 0.0

N_EXPERTS = 16
CAPACITY_FACTOR = 2
EXPERT_FF = D_MODEL // 2

kernel_name = 'hybrid_interleaved_dit_trunk'


def rms_norm(x, w):
    xf = x.astype(jnp.float32)
    y = xf * lax.rsqrt(jnp.mean(xf * xf, axis=-1, keepdims=True) + EPS)
    return (y * w.astype(jnp.float32)).astype(x.dtype)


def modulate(h, shift, scale):
    return h * (1 + scale) + shift


def axial_rope_angles(seq_len, dim):
    rows = seq_len // GRID_W
    row_id = jnp.repeat(jnp.arange(rows, dtype=jnp.float32), GRID_W)
    col_id = jnp.tile(jnp.arange(GRID_W, dtype=jnp.float32), rows)
    n_freq = dim // 4
    inv = ROPE_THETA ** (-jnp.arange(n_freq, dtype=jnp.float32) / n_freq)
    ang = jnp.concatenate([row_id[:, None] * inv, col_id[:, None] * inv], axis=-1)
    return jnp.cos(ang), jnp.sin(ang)


def apply_axial_rope(x, cos, sin):
    b, l, h, d = x.shape
    nf = d // 4
    xr = x.reshape(b, l, h, 2, 2, nf)
    x1, x2 = xr[..., 0, :], xr[..., 1, :]
    cos = cos.reshape(l, 1, 2, nf).astype(x.dtype)
    sin = sin.reshape(l, 1, 2, nf).astype(x.dtype)
    out = jnp.stack([x1 * cos - x2 * sin, x1 * sin + x2 * cos], axis=-2)
    return out.reshape(b, l, h, d)


def gqa_attend(q, k, v):
    b, lq, h, hd = q.shape
    kvh = k.shape[2]
    g = h // kvh
    nb = lq // Q_BLOCK
    qb = q.reshape(b, nb, Q_BLOCK, kvh, g, hd).transpose(1, 0, 3, 4, 2, 5)
    kt = k.transpose(0, 2, 1, 3)
    vt = v.transpose(0, 2, 1, 3)
    scale = hd ** -0.5

    def one_block(qi):
        s = jnp.einsum('bkgqd,bksd->bkgqs', qi, kt).astype(jnp.float32) * scale
        p = jax.nn.softmax(s, axis=-1).astype(vt.dtype)
        return jnp.einsum('bkgqs,bksd->bkgqd', p, vt)

    o = lax.map(one_block, qb)
    return o.transpose(1, 0, 4, 2, 3, 5).reshape(b, lq, h * hd)


def attention_mixer(h_lat, h_ctx, w_qkv, q_norm, k_norm, w_o, with_ctx_out):
    b, l, _ = h_lat.shape
    H, KV, hd = ATTN_HEADS, ATTN_KV_HEADS, ATTN_HEAD_DIM
    nq, nkv = H * hd, KV * hd
    p = h_lat @ w_qkv
    ql = rms_norm(p[..., :nq].reshape(b, l, H, hd), q_norm)
    kl = rms_norm(p[..., nq:nq + nkv].reshape(b, l, KV, hd), k_norm)
    vl = p[..., nq + nkv:].reshape(b, l, KV, hd)
    cos, sin = axial_rope_angles(l, hd)
    ql = apply_axial_rope(ql, cos, sin)
    kl = apply_axial_rope(kl, cos, sin)
    pc = h_ctx @ w_qkv[:, nq:]
    kc = rms_norm(pc[..., :nkv].reshape(b, -1, KV, hd), k_norm)
    vc = pc[..., nkv:].reshape(b, -1, KV, hd)
    k_all = jnp.concatenate([kc, kl], axis=1)
    v_all = jnp.concatenate([vc, vl], axis=1)
    out_lat = gqa_attend(ql, k_all, v_all) @ w_o
    out_ctx = None
    if with_ctx_out:
        qc = rms_norm((h_ctx @ w_qkv[:, :nq]).reshape(b, -1, H, hd), q_norm)
        out_ctx = gqa_attend(qc, kc, vc) @ w_o
    return out_ctx, out_lat


def retention_scan(q, k, v, log_gamma, state0, inclusive):
    b, l, h, _ = q.shape
    dv = v.shape[-1]
    C = RET_CHUNK
    n = l // C

    def chunks(t):
        return t.reshape(b, n, C, h, t.shape[-1]).transpose(1, 0, 3, 2, 4)

    idx = jnp.arange(C, dtype=jnp.float32)
    diff = idx[:, None] - idx[None, :]
    causal = diff >= 0 if inclusive else diff > 0
    lg = log_gamma[:, None, None]
    dmask = jnp.where(causal, jnp.exp(jnp.where(causal, diff, 0.0) * lg), 0.0).astype(q.dtype)
    q_dec = jnp.exp((idx + 1.0) * log_gamma[:, None])[..., None].astype(q.dtype)
    k_dec = jnp.exp((C - 1.0 - idx) * log_gamma[:, None])[..., None].astype(q.dtype)
    c_dec = jnp.exp(C * log_gamma)[:, None, None].astype(q.dtype)

    def step(S, inp):
        qc, kc, vc = inp
        inner = jnp.einsum('bhqd,bhkd->bhqk', qc, kc) * dmask
        o = jnp.einsum('bhqk,bhkv->bhqv', inner, vc) + jnp.einsum('bhqd,bhdv->bhqv', qc * q_dec, S)
        S = S * c_dec + jnp.einsum('bhkd,bhkv->bhdv', kc * k_dec, vc)
        return S, o

    S, o = lax.scan(step, state0, (chunks(q), chunks(k), chunks(v)))
    return o.transpose(1, 0, 3, 2, 4).reshape(b, l, h, dv), S


def retention_mixer(h_lat, h_ctx, w_in, decay_logit, w_o, with_ctx_out):
    H, dk, dv = RET_HEADS, RET_QK_DIM, RET_V_DIM
    b, l, _ = h_lat.shape
    log_g = jax.nn.log_sigmoid(decay_logit.astype(jnp.float32))

    def project(hx, rope):
        p = hx @ w_in
        q, k, v, g = jnp.split(p, [H * dk, 2 * H * dk, 2 * H * dk + H * dv], axis=-1)
        q = q.reshape(b, -1, H, dk)
        k = k.reshape(b, -1, H, dk) * (dk ** -0.5)
        v = v.reshape(b, -1, H, dv)
        if rope is not None:
            q = apply_axial_rope(q, *rope)
            k = apply_axial_rope(k, *rope)
        return q, k, v, g

    def flip(t):
        return t[:, ::-1]

    def readout(o, g):
        of = o.astype(jnp.float32)
        of = of * lax.rsqrt(jnp.mean(of * of, axis=-1, keepdims=True) + EPS)
        return (jax.nn.silu(g) * of.astype(g.dtype).reshape(b, -1, H * dv)) @ w_o

    qc, kc, vc, gc = project(h_ctx, None)
    zero = jnp.zeros((b, H, dk, dv), qc.dtype)
    oc_f, sc_f = retention_scan(qc, kc, vc, log_g[0], zero, True)
    oc_b, sc_b = retention_scan(flip(qc), flip(kc), flip(vc), log_g[1], zero, False)
    ql, kl, vl, gl = project(h_lat, axial_rope_angles(l, dk))
    ol_f, _ = retention_scan(ql, kl, vl, log_g[0], sc_f, True)
    ol_b, _ = retention_scan(flip(ql), flip(kl), flip(vl), log_g[1], sc_b, False)
    out_lat = readout(ol_f + flip(ol_b), gl)
    out_ctx = readout(oc_f + flip(oc_b), gc) if with_ctx_out else None
    return out_ctx, out_lat


def hyena_filter(seq_len, w1, b1, fr1, w2, b2, fr2, w3):
    f32 = jnp.float32
    t = jnp.linspace(0.0, 1.0, seq_len, dtype=f32)[:, None]
    bands = (HYENA_EMB - 1) // 2
    w = 2.0 * math.pi * jnp.arange(seq_len, dtype=f32)[:, None] / seq_len
    f = jnp.linspace(1e-4, bands - 1, bands, dtype=f32)[None, :]
    z = jnp.concatenate([t, jnp.cos(f * w), -jnp.sin(f * w)], axis=-1)
    h = jnp.sin(fr1.astype(f32) * (z @ w1.astype(f32) + b1.astype(f32)))
    h = jnp.sin(fr2.astype(f32) * (h @ w2.astype(f32) + b2.astype(f32)))
    h = h @ w3.astype(f32)
    deltas = jnp.abs(jnp.linspace(math.log(HYENA_TARGET) / HYENA_SLOW, math.log(HYENA_TARGET) / HYENA_FAST, D_MODEL, dtype=f32))
    decay = jnp.exp(-t * deltas)
    h = h.reshape(seq_len, 2, D_MODEL) * (decay[:, None, :] + HYENA_SHIFT)
    h_f, h_b = h[:, 0], h[:, 1]
    taps = jnp.concatenate([h_f, jnp.zeros((1, D_MODEL), f32), h_b[:0:-1]], axis=0)
    return taps / jnp.sum(jnp.abs(taps), axis=0, keepdims=True)


def hyena_operator(hx, w_in, conv_w, conv_b, taps, skip):
    b, l, _ = hx.shape
    z = hx @ w_in
    zp = jnp.pad(z, ((0, 0), (1, 1), (0, 0)))
    z = zp[:, :-2] * conv_w[0] + zp[:, 1:-1] * conv_w[1] + zp[:, 2:] * conv_w[2] + conv_b
    x0, x1, v = jnp.split(z, 3, axis=-1)
    u = (v * x1).astype(jnp.float32)
    n = 2 * l
    y = jnp.fft.irfft(jnp.fft.rfft(u, n=n, axis=1) * jnp.fft.rfft(taps, n=n, axis=0)[None], n=n, axis=1)[:, :l]
    y = (y + u * skip.astype(jnp.float32)).astype(hx.dtype)
    return y * x0


def hyena_mixer(h_lat, h_ctx, w_in, conv_w, conv_b, f_w1, f_b1, f_fr1, f_w2, f_b2, f_fr2, f_w3, skip, w_out, with_ctx_out):
    fparams = (f_w1, f_b1, f_fr1, f_w2, f_b2, f_fr2, f_w3)
    out_lat = hyena_operator(h_lat, w_in, conv_w, conv_b, hyena_filter(h_lat.shape[1], *fparams), skip) @ w_out
    out_ctx = None
    if with_ctx_out:
        out_ctx = hyena_operator(h_ctx, w_in, conv_w, conv_b, hyena_filter(h_ctx.shape[1], *fparams), skip) @ w_out
    return out_ctx, out_lat


def expert_choice_moe(h, w_router, w_gate, w_up, w_down):
    b, n, d = h.shape
    cap = CAPACITY_FACTOR * n // N_EXPERTS
    probs = jax.nn.softmax((h @ w_router).astype(jnp.float32), axis=-1)
    gate, idx = lax.top_k(jnp.swapaxes(probs, 1, 2), cap)
    xg = jax.vmap(lambda hb, ib: hb[ib])(h, idx)
    a = jnp.einsum('becd,edf->becf', xg, w_gate)
    u = jnp.einsum('becd,edf->becf', xg, w_up)
    y = jnp.einsum('becf,efd->becd', jax.nn.silu(a) * u, w_down) * gate[..., None].astype(h.dtype)
    return jax.vmap(lambda ib, yb: jnp.zeros((n, d), yb.dtype).at[ib.reshape(-1)].add(yb.reshape(-1, d)))(idx, y)


def setup_inputs(seed: int = 0) -> dict:
    key = jax.random.key(seed)
    keys = jax.random.split(key, 32)
    counter = iter(range(32))
    f32 = jnp.float32

    def nrm(shape, scale):
        return jax.random.normal(keys[next(counter)], shape, f32) * scale

    D = D_MODEL
    qkv_w = (ATTN_HEADS + 2 * ATTN_KV_HEADS) * ATTN_HEAD_DIM
    ret_w = 2 * RET_HEADS * RET_QK_DIM + 2 * RET_HEADS * RET_V_DIM
    decay_init = jnp.log(2.0 ** (5.0 + jnp.arange(RET_HEADS, dtype=f32)) - 1.0)
    return {
        'x': nrm((BATCH, SEQ, D), 1.0),
        'c': nrm((BATCH, D), 1.0),
        'ctx': nrm((BATCH, CTX_LEN, D), 1.0),
        'c_ctx': nrm((D,), 1.0),
        'w_mod': nrm((DEPTH, D, N_MOD * D), 0.5 * D ** -0.5),
        'b_mod': nrm((DEPTH, N_MOD * D), 0.02),
        'norm_w': 1.0 + nrm((DEPTH, 2, D), 0.02),
        'attn_w_qkv': nrm((N_ATTN_LAYERS, D, qkv_w), D ** -0.5),
        'attn_q_norm': 1.0 + nrm((N_ATTN_LAYERS, ATTN_HEAD_DIM), 0.02),
        'attn_k_norm': 1.0 + nrm((N_ATTN_LAYERS, ATTN_HEAD_DIM), 0.02),
        'attn_w_o': nrm((N_ATTN_LAYERS, ATTN_HEADS * ATTN_HEAD_DIM, D), (ATTN_HEADS * ATTN_HEAD_DIM) ** -0.5),
        'ret_w_in': nrm((N_RET_LAYERS, D, ret_w), D ** -0.5),
        'ret_decay_logit': decay_init[None, None, :] + nrm((N_RET_LAYERS, 2, RET_HEADS), 0.1),
        'ret_w_o': nrm((N_RET_LAYERS, RET_HEADS * RET_V_DIM, D), (RET_HEADS * RET_V_DIM) ** -0.5),
        'hy_w_in': nrm((N_HYENA_LAYERS, D, 3 * D), D ** -0.5),
        'hy_conv_w': nrm((N_HYENA_LAYERS, 3, 3 * D), 3 ** -0.5),
        'hy_conv_b': nrm((N_HYENA_LAYERS, 3 * D), 0.02),
        'hy_f_w1': nrm((N_HYENA_LAYERS, HYENA_EMB, HYENA_FILTER_WIDTH), HYENA_EMB ** -0.5),
        'hy_f_b1': nrm((N_HYENA_LAYERS, HYENA_FILTER_WIDTH), 0.02),
        'hy_f_freq1': 1.0 + nrm((N_HYENA_LAYERS, HYENA_FILTER_WIDTH), 0.02),
        'hy_f_w2': nrm((N_HYENA_LAYERS, HYENA_FILTER_WIDTH, HYENA_FILTER_WIDTH), HYENA_FILTER_WIDTH ** -0.5),
        'hy_f_b2': nrm((N_HYENA_LAYERS, HYENA_FILTER_WIDTH), 0.02),
        'hy_f_freq2': 1.0 + nrm((N_HYENA_LAYERS, HYENA_FILTER_WIDTH), 0.02),
        'hy_f_w3': nrm((N_HYENA_LAYERS, HYENA_FILTER_WIDTH, 2 * D), HYENA_FILTER_WIDTH ** -0.5),
        'hy_skip': nrm((N_HYENA_LAYERS, D), 0.5),
        'hy_w_out': nrm((N_HYENA_LAYERS, D, D), D ** -0.5),
        'moe_router': nrm((DEPTH, D, N_EXPERTS), D ** -0.5),
        'moe_w_gate': nrm((DEPTH, N_EXPERTS, D, EXPERT_FF), D ** -0.5),
        'moe_w_up': nrm((DEPTH, N_EXPERTS, D, EXPERT_FF), D ** -0.5),
        'moe_w_down': nrm((DEPTH, N_EXPERTS, EXPERT_FF, D), EXPERT_FF ** -0.5),
        'final_norm_w': 1.0 + nrm((D,), 0.02),
    }


def reference(x, c, ctx, c_ctx, w_mod, b_mod, norm_w, attn_w_qkv, attn_q_norm, attn_k_norm, attn_w_o, ret_w_in, ret_decay_logit, ret_w_o, hy_w_in, hy_conv_w, hy_conv_b, hy_f_w1, hy_f_b1, hy_f_freq1, hy_f_w2, hy_f_b2, hy_f_freq2, hy_f_w3, hy_skip, hy_w_out, moe_router, moe_w_gate, moe_w_up, moe_w_down, final_norm_w):
    for i in range(DEPTH):
        kind = i % N_MIXERS
        j = i // N_MIXERS
        with_ctx = i < DEPTH - 1
        mod_lat = (jax.nn.silu(c) @ w_mod[i] + b_mod[i])[:, None, :]
        mod_ctx = jax.nn.silu(c_ctx) @ w_mod[i] + b_mod[i]
        sh1, sc1, g1, sh2, sc2, g2 = jnp.split(mod_lat, N_MOD, axis=-1)
        csh1, csc1, cg1, csh2, csc2, cg2 = jnp.split(mod_ctx, N_MOD, axis=-1)
        h_lat = modulate(rms_norm(x, norm_w[i, 0]), sh1, sc1)
        h_ctx = modulate(rms_norm(ctx, norm_w[i, 0]), csh1, csc1)
        if kind == 0:
            o_ctx, o_lat = attention_mixer(h_lat, h_ctx, attn_w_qkv[j], attn_q_norm[j], attn_k_norm[j], attn_w_o[j], with_ctx)
        elif kind == 1:
            o_ctx, o_lat = retention_mixer(h_lat, h_ctx, ret_w_in[j], ret_decay_logit[j], ret_w_o[j], with_ctx)
        else:
            o_ctx, o_lat = hyena_mixer(h_lat, h_ctx, hy_w_in[j], hy_conv_w[j], hy_conv_b[j], hy_f_w1[j], hy_f_b1[j], hy_f_freq1[j], hy_f_w2[j], hy_f_b2[j], hy_f_freq2[j], hy_f_w3[j], hy_skip[j], hy_w_out[j], with_ctx)
        x = x + g1 * o_lat
        h2 = modulate(rms_norm(x, norm_w[i, 1]), sh2, sc2)
        x = x + g2 * expert_choice_moe(h2, moe_router[i], moe_w_gate[i], moe_w_up[i], moe_w_down[i])
        if with_ctx:
            ctx = ctx + cg1 * o_ctx
            hc2 = modulate(rms_norm(ctx, norm_w[i, 1]), csh2, csc2)
            ctx = ctx + cg2 * expert_choice_moe(hc2, moe_router[i], moe_w_gate[i], moe_w_up[i], moe_w_down[i])
    return rms_norm(x, final_norm_w)
```

```python
import contextlib
import math
import os as _os
import numpy as np
import ml_dtypes
import concourse.bass as bass
import concourse.mybir as mybir
from concourse.bass_utils import run_bass_kernel_spmd

F32 = mybir.dt.float32
BF16 = mybir.dt.bfloat16
AF = mybir.ActivationFunctionType
ALU = mybir.AluOpType
AX = mybir.AxisListType

D = 2048
TL = 2048
TC = 256
TT = TL + TC
NT = TT // 128
KT = D // 128
DEPTH = 4
EPS = 1e-6
NE = 16
CAPL = 256
CAPC = 32
FF = 1024
ENGS = ('pe', 'act', 'dve', 'pool', 'sp')
IMPLEMENTED = {'ret', 'hy'}


class T:
    __slots__ = ('ap', 'w', 'r', 'name')

    def __init__(self, ap, name=''):
        self.ap = ap
        self.w = None
        self.r = {}
        self.name = name


class Sched:
    def __init__(self, nc, n_dma_sems=(48, 48)):
        self.nc = nc
        self.cnt = {e: 0 for e in ENGS}
        self.waited = {e: {} for e in ENGS}
        self.ndma = {'sp': n_dma_sems[0], 'pool': n_dma_sems[1]}
        self.dma_i = {'sp': 0, 'pool': 0}
        self.dma_cnt = {}
        self.sems = {}
        self.ninst = 0
        self.eng = {'pe': nc.tensor, 'act': nc.scalar, 'dve': nc.vector, 'pool': nc.gpsimd, 'sp': nc.sync}

    def alloc_sems(self, st):
        for e in ENGS:
            self.sems[e] = st.enter_context(self.nc.semaphore("s_" + e))
        for q in ('sp', 'pool'):
            for i in range(self.ndma[q]):
                self.sems[(q, i)] = st.enter_context(self.nc.semaphore(f"s_{q}{i}"))

    @staticmethod
    def _need(need, tok):
        if tok is None:
            return
        k, v = tok
        if need.get(k, 0) < v:
            need[k] = v

    def op(self, eng, fn, reads=(), writes=(), dma=False):
        need = {}
        for t in reads:
            self._need(need, t.w)
        for t in writes:
            self._need(need, t.w)
            for k, v in t.r.items():
                self._need(need, (k, v))
        if dma:
            i = self.dma_i[eng]
            self.dma_i[eng] = i + 1
            key = (eng, i % self.ndma[eng])
            prev = self.dma_cnt.get(key, 0)
            if prev:
                self._need(need, (key, prev))
            val = prev + 16
            self.dma_cnt[key] = val
            tok = (key, val)
            inc = 16
        else:
            self.cnt[eng] += 1
            key = eng
            tok = (eng, self.cnt[eng])
            inc = 1
        wd = self.waited[eng]
        eo = self.eng[eng]
        for k, v in need.items():
            if eng == 'pe' and k == 'pe':
                continue
            if wd.get(k, 0) >= v:
                continue
            wd[k] = v
            eo.wait_ge(self.sems[k], v)
        fn(eo).then_inc(self.sems[key], inc)
        self.ninst += 1
        for t in reads:
            if t.r.get(tok[0], 0) < tok[1]:
                t.r[tok[0]] = tok[1]
        for t in writes:
            t.w = tok
            t.r = {}
        return tok

    def barrier(self, engs=ENGS):
        toks = {}
        for e in ENGS:
            if e != 'sp' and self.cnt[e] > 0:
                toks[e] = self.cnt[e]
        for k, v in self.dma_cnt.items():
            toks[k] = v
        for e in engs:
            wd = self.waited[e]
            for k, v in toks.items():
                if wd.get(k, 0) >= v:
                    continue
                wd[k] = v
                self.eng[e].wait_ge(self.sems[k], v)


class Prefetch:
    def __init__(self, ring, loaders, depth):
        self.ring = ring
        self.loaders = loaders
        self.depth = depth
        self.tiles = [None] * len(loaders)
        self.nxt = 0

    def get(self, i):
        while self.nxt < len(self.loaders) and self.nxt <= i + self.depth:
            t = self.ring.next()
            self.loaders[self.nxt](t)
            self.tiles[self.nxt] = t
            self.nxt += 1
        return self.tiles[i]


class Ring:
    def __init__(self, tiles):
        self.tiles = tiles
        self.i = 0

    def next(self):
        t = self.tiles[self.i % len(self.tiles)]
        self.i += 1
        return t


def _bf(a):
    return np.ascontiguousarray(a.astype(ml_dtypes.bfloat16))


def make_consts():
    c = {}
    c['ident_f'] = np.eye(128, dtype=np.float32)
    c['ident_b'] = _bf(np.eye(128, dtype=np.float32))
    c['ones_f'] = np.ones((128, 128), np.float32)
    c['ones_b'] = _bf(np.ones((128, 128), np.float32))
    c['iota_row'] = np.tile(np.arange(256, dtype=np.float32)[None, :], (128, 1))
    ip = np.zeros((128, 4), np.float32)
    ip[:, 0] = np.arange(128)
    ip[:, 1] = np.arange(128) + 128
    c['iota_part'] = ip
    t = np.arange(TL)
    row = (t // 64).astype(np.float64)
    col = (t % 64).astype(np.float64)

    def rope_tables(hd):
        nf = hd // 4
        inv = 10000.0 ** (-np.arange(nf, dtype=np.float64) / nf)
        cosT = np.zeros((hd, TL), np.float32)
        sinT = np.zeros((hd, TL), np.float32)
        perm = np.zeros((hd, hd), np.float32)
        for d in range(hd):
            axis = d // (2 * nf)
            half = (d % (2 * nf)) // nf
            f = d % nf
            pos = row if axis == 0 else col
            ang = (pos.astype(np.float32) * np.float32(inv[f])).astype(np.float32)
            cosT[d] = np.cos(ang)
            sinT[d] = np.sin(ang) * (-1.0 if half == 0 else 1.0)
            partner = d + nf if half == 0 else d - nf
            perm[partner, d] = 1.0
        return cosT, sinT, perm
    ca, sa, pa = rope_tables(128)
    c['cosA'] = ca
    c['sinA'] = sa
    c['permA'] = pa
    cr, sr, pr = rope_tables(256)
    c['cosR0'] = np.ascontiguousarray(cr[0:128])
    c['cosR1'] = np.ascontiguousarray(cr[128:256])
    c['sinR0'] = np.ascontiguousarray(sr[0:128])
    c['sinR1'] = np.ascontiguousarray(sr[128:256])
    c['permR'] = np.ascontiguousarray(pr[0:128, 0:128])
    def hy_consts(L, tag):
        n = 2 * L
        nkt = L // 128
        tl = np.linspace(0.0, 1.0, L, dtype=np.float32)
        bands = 16
        w = (2.0 * math.pi * np.arange(L, dtype=np.float32) / L).astype(np.float32)
        f = np.linspace(1e-4, bands - 1, bands, dtype=np.float32)
        z = np.concatenate([tl[:, None], np.cos(f[None, :] * w[:, None]), -np.sin(f[None, :] * w[:, None])], axis=-1).astype(np.float32)
        c['hyZ' + tag] = np.ascontiguousarray(z.T)
        deltas = np.abs(np.linspace(math.log(1e-2) / 1.5, math.log(1e-2) / 0.3, D, dtype=np.float32))
        c['hyDecay' + tag] = np.exp(-tl[:, None] * deltas[None, :]).astype(np.float32)
        k = np.arange(L, dtype=np.int64)
        ff = np.arange(L, dtype=np.int64)
        m = ((2 * ff[None, :] + 1) * k[:, None]) % (2 * n)
        ang = m.astype(np.float64) * (math.pi / n)
        CT = np.cos(ang)
        ST = np.sin(ang)
        tcw = min(512, L)
        ntc = L // tcw

        def tile_T(M):
            return _bf(M.reshape(nkt, 128, nkt, 128).transpose(2, 1, 0, 3).reshape(nkt, 128, nkt * 128))

        def tile_F(M):
            return _bf(M.reshape(ntc, tcw, nkt, 128).transpose(0, 3, 2, 1).reshape(ntc, 128, nkt * tcw))
        c['hyCT' + tag] = tile_T(CT)
        c['hyST' + tag] = tile_T(ST)
        c['hyCF' + tag] = tile_F(CT)
        c['hySF' + tag] = tile_F(ST)
    hy_consts(TL, 'L')
    hy_consts(TC, 'C')
    p_ = np.arange(128, dtype=np.float32)[:, None]
    c['retDelta'] = np.ascontiguousarray(np.arange(3968, dtype=np.float32)[None, :] - p_ - 1920.0)
    y_ = np.arange(2176, dtype=np.float32)[None, :]
    c['retEf'] = np.ascontiguousarray(y_ - p_ + 128.0)
    c['retEb'] = np.ascontiguousarray(p_ - y_ + 2176.0)
    return c


class KB:
    pass


def build(layers=(0, 1, 2, 3), debug=None, stop=None, small=None):
    nc = bass.Bass("TRN2", target_bir_lowering=False)
    S = Sched(nc)
    consts = make_consts()

    def din(name, shape, dt=F32):
        return nc.dram_tensor(name, list(shape), dt, kind="ExternalInput").ap()

    def dscr(name, shape, dt):
        return nc.dram_tensor(name, list(shape), dt, kind="Internal").ap()

    I = {}
    I['x'] = din('x', [TL, D])
    I['c'] = din('c', [D])
    I['ctx'] = din('ctx', [TC, D])
    I['c_ctx'] = din('c_ctx', [D])
    small = small or {}
    _DD = small.get('depth', DEPTH)
    _NEW = small.get('ne', NE)
    I['w_mod'] = din('w_mod', [_DD, D, 6 * D])
    I['b_mod'] = din('b_mod', [DEPTH, 6 * D])
    I['norm_w'] = din('norm_w', [DEPTH, 2, D])
    I['attn_w_qkv'] = din('attn_w_qkv', [2, D, 3072])
    I['attn_q_norm'] = din('attn_q_norm', [2, 128])
    I['attn_k_norm'] = din('attn_k_norm', [2, 128])
    I['attn_w_o'] = din('attn_w_o', [2, D, D])
    I['ret_w_in'] = din('ret_w_in', [1, D, 12288])
    I['ret_decay_logit'] = din('ret_decay_logit', [1, 2, 8])
    I['ret_w_o'] = din('ret_w_o', [1, 4096, D])
    I['hy_w_in'] = din('hy_w_in', [1, D, 3 * D])
    I['hy_conv_w'] = din('hy_conv_w', [1, 3, 3 * D])
    I['hy_conv_b'] = din('hy_conv_b', [1, 3 * D])
    I['hy_f_w1'] = din('hy_f_w1', [1, 33, 64])
    I['hy_f_b1'] = din('hy_f_b1', [1, 64])
    I['hy_f_freq1'] = din('hy_f_freq1', [1, 64])
    I['hy_f_w2'] = din('hy_f_w2', [1, 64, 64])
    I['hy_f_b2'] = din('hy_f_b2', [1, 64])
    I['hy_f_freq2'] = din('hy_f_freq2', [1, 64])
    I['hy_f_w3'] = din('hy_f_w3', [1, 64, 2 * D])
    I['hy_skip'] = din('hy_skip', [1, D])
    I['hy_w_out'] = din('hy_w_out', [1, D, D])
    I['moe_router'] = din('moe_router', [DEPTH, D, NE])
    I['moe_w_gate'] = din('moe_w_gate', [_DD, _NEW, D, FF])
    I['moe_w_up'] = din('moe_w_up', [_DD, _NEW, D, FF])
    I['moe_w_down'] = din('moe_w_down', [_DD, _NEW, FF, D])
    I['final_norm_w'] = din('final_norm_w', [D])
    CI = {}
    for k, v in consts.items():
        CI[k] = din('k_' + k, v.shape, BF16 if v.dtype == ml_dtypes.bfloat16 else F32)
    OUT = nc.dram_tensor('out', [TL, D], F32, kind="ExternalOutput").ap()

    XR = dscr('XR', [TT, D], F32)
    HT = dscr('HT', [KT, 128, TT], BF16)
    H2TM = dscr('H2TM', [TT, D], BF16)
    MOD = dscr('MOD', [2, 6 * D], F32)
    QT = dscr('QT', [16, 128, TT], BF16)
    KTs = dscr('KTs', [4, 128, TT], BF16)
    Vs = dscr('Vs', [TT, 512], BF16)
    OT = dscr('OT', [32, 128, TT], BF16)
    RK = dscr('RK', [16, 128, TT], BF16)
    RG = dscr('RG', [32, 128, TT], BF16)
    RV = dscr('RV', [TT, 4096], BF16)
    UTM = dscr('UTM', [TT, D], BF16)
    UTF = dscr('UTF', [KT, 128, TT], F32)
    X0F = dscr('X0F', [KT, 128, TT], F32)
    HPM = dscr('HPM', [2, TL, D], BF16)
    HSPEC = {'L': dscr('HSPECL', [2, TL, D], F32), 'C': dscr('HSPECC', [2, TC, D], F32)}
    SKIPT = dscr('SKIPT', [128, KT], F32)
    POS = dscr('POS', [NE, TT], F32)
    YG = dscr('YG', [NE, 288, D], BF16)

    dT = {}

    def dt_(name, ap):
        dT[name] = T(ap, name)
        return dT[name]
    tXR = [dt_(f'XR{i}', XR) for i in range(NT)]
    tHT = dt_('HT', HT)
    tH2 = dt_('H2TM', H2TM)
    tMOD = dt_('MOD', MOD)
    tQT = dt_('QT', QT)
    tKT = dt_('KTs', KTs)
    tV = dt_('Vs', Vs)
    tOT = dt_('OT', OT)
    tRK = dt_('RK', RK)
    tRG = dt_('RG', RG)
    tRV = dt_('RV', RV)
    tUTM = dt_('UTM', UTM)
    tUTF = dt_('UTF', UTF)
    tX0F = dt_('X0F', X0F)
    tHPM = dt_('HPM', HPM)
    tHSPEC = {'L': dt_('HSPECL', HSPEC['L']), 'C': dt_('HSPECC', HSPEC['C'])}
    tSKIPT = dt_('SKIPT', SKIPT)
    tPOS = dt_('POS', POS)
    tYG = dt_('YG', YG)
    tIN = T(None, 'inputs')
    tOUT = dt_('OUT', OUT)

    with contextlib.ExitStack() as top:
        S.alloc_sems(top)

        def sb(stk, name, shape, dt):
            return T(top_or(stk).enter_context(nc.sbuf_tensor(name, list(shape), dt)), name)

        def top_or(stk):
            return stk if stk is not None else top

        uid = [0]

        def nm(p):
            uid[0] += 1
            return f"{p}_{uid[0]}"

        PS = [T(top.enter_context(nc.psum_tensor(f"ps{i}", [128, 512], F32)), f"ps{i}") for i in range(7)]
        PSB = T(top.enter_context(nc.psum_tensor("psb", [128, 1024], BF16)), "psb")
        psring = Ring(PS)

        Csb = {}
        for k in ('ident_f', 'ident_b', 'ones_f', 'ones_b', 'iota_row', 'iota_part', 'permA', 'permR'):
            v = consts[k]
            Csb[k] = sb(None, 'c_' + k, v.shape, BF16 if v.dtype == ml_dtypes.bfloat16 else F32)
            S.op('sp', lambda e, k=k: e.dma_start(out=Csb[k].ap[:], in_=CI[k]), [tIN], [Csb[k]], dma=True)
        ones_col = sb(None, 'ones_col', [128, 1], F32)
        S.op('dve', lambda e: e.memset(ones_col.ap[:], 1.0), [], [ones_col])
        craw = sb(None, 'craw', [128, KT, 2], F32)
        sT = sb(None, 'sT', [128, KT, 2], BF16)
        S.op('sp', lambda e: e.dma_start(out=craw.ap[:, :, 0], in_=I['c'].rearrange("(kt p) -> p kt", p=128),
                                         allow_slow_non_contiguous=True), [tIN], [craw], dma=True)
        S.op('sp', lambda e: e.dma_start(out=craw.ap[:, :, 1], in_=I['c_ctx'].rearrange("(kt p) -> p kt", p=128),
                                         allow_slow_non_contiguous=True), [tIN], [craw], dma=True)
        S.op('act', lambda e: e.activation(out=sT.ap[:], in_=craw.ap[:], func=AF.Silu), [craw], [sT])
        probs_tm = sb(None, 'probs_tm', [128, NT, NE], F32)
        probsT = sb(None, 'probsT', [NE, TT], F32)
        posTM = sb(None, 'posTM', [128, NT, NE], F32)
        GHL = sb(None, 'GHL', [128, NT, NE, 2], BF16)

        for i in range(NT):
            src = I['ctx'][i * 128:(i + 1) * 128, :] if i < 2 else I['x'][(i - 2) * 128:(i - 1) * 128, :]
            S.op('sp', lambda e: e.dma_start(out=XR[i * 128:(i + 1) * 128, :], in_=src), [tIN], [tXR[i]], dma=True)

        def bcast_load(stk, name, vec_ap):
            n = vec_ap.shape[-1]
            t = sb(stk, nm(name), [128, n], F32)
            S.op('sp', lambda e: e.dma_start(out=t.ap[:], in_=vec_ap.partition_broadcast(128)), [tMOD, tIN], [t], dma=True)
            return t

        def phase_mod(li):
            with contextlib.ExitStack() as ph:
                wring = Ring([sb(ph, nm('wm'), [128, KT, 512], BF16) for _ in range(3)])
                modsb = sb(ph, nm('modsb'), [2, 6 * D], F32)
                bsb = sb(ph, nm('bsb'), [2, 6 * D], F32)
                S.op('sp', lambda e: e.dma_start(out=bsb.ap[:], in_=I['b_mod'][li].partition_broadcast(2)), [tIN], [bsb], dma=True)
                wsrc = I['w_mod'][li].rearrange("(kt p) n -> p kt n", p=128)
                for cch in range(24):
                    w = wring.next()
                    S.op('pool', lambda e: e.dma_start(out=w.ap[:], in_=wsrc[:, :, cch * 512:(cch + 1) * 512]), [tIN], [w], dma=True)
                    ps = psring.next()
                    for kt in range(KT):
                        S.op('pe', lambda e: e.matmul(ps.ap[0:2, :], lhsT=sT.ap[:, kt, :], rhs=w.ap[:, kt, :],
                                                      start=(kt == 0), stop=(kt == KT - 1)), [sT, w], [ps])
                    S.op('dve', lambda e: e.tensor_tensor(out=modsb.ap[:, cch * 512:(cch + 1) * 512], in0=ps.ap[0:2, :],
                                                          in1=bsb.ap[:, cch * 512:(cch + 1) * 512], op=ALU.add), [ps, bsb], [modsb])
                S.op('sp', lambda e: e.dma_start(out=MOD[:, :], in_=modsb.ap[:]), [modsb], [tMOD], dma=True)
                S.barrier()

        def phase_norm(li, k, want_tm=False, router=False, final=False):
            with contextlib.ExitStack() as ph:
                if final:
                    nw = bcast_load(ph, 'nw', I['final_norm_w'])
                    A_bc = [nw, nw]
                    B_bc = [None, None]
                else:
                    nw = bcast_load(ph, 'nw', I['norm_w'][li, k])
                    A_bc, B_bc = [], []
                    for which in (0, 1):
                        sc = bcast_load(ph, 'sc', MOD[which, (3 * k + 1) * D:(3 * k + 2) * D])
                        S.op('dve', lambda e: e.scalar_tensor_tensor(out=sc.ap[:], in0=sc.ap[:], scalar=1.0, in1=nw.ap[:],
                                                                     op0=ALU.add, op1=ALU.mult), [sc, nw], [sc])
                        A_bc.append(sc)
                        B_bc.append(bcast_load(ph, 'sh', MOD[which, (3 * k) * D:(3 * k + 1) * D]))
                if router:
                    wr = sb(ph, nm('wr'), [128, KT, NE], F32)
                    for kt in range(KT):
                        S.op('sp', lambda e: e.dma_start(out=wr.ap[:, kt, :], in_=I['moe_router'][li, kt * 128:(kt + 1) * 128, :]),
                             [tIN], [wr], dma=True)
                ptile = sb(ph, nm('ptile'), [128, 128], F32)
                S.op('dve', lambda e: e.memset(ptile.ap[:], 0.0), [], [ptile])
                xring = Ring([sb(ph, nm('xt'), [128, D], F32) for _ in range(2)])
                hring = Ring([sb(ph, nm('h'), [128, D], F32) for _ in range(2)])
                hbring = Ring([sb(ph, nm('hb'), [128, D], BF16) for _ in range(2)])
                junk = sb(ph, nm('junk'), [128, D], BF16)
                htring = Ring([sb(ph, nm('htb'), [128, 4, 128], BF16) for _ in range(4)])
                hfring = Ring([sb(ph, nm('htf'), [128, KT, 128], F32) for _ in range(2)])
                stat = Ring([sb(ph, nm('st'), [128, 4], F32) for _ in range(3)])
                tiles = range(2, NT) if final else range(NT)
                for i in tiles:
                    which = 1 if i < 2 else 0
                    xt = xring.next()
                    S.op('sp', lambda e: e.dma_start(out=xt.ap[:], in_=XR[i * 128:(i + 1) * 128, :]), [tXR[i]], [xt], dma=True)
                    s4 = stat.next()
                    S.op('act', lambda e: e.activation(out=junk.ap[:], in_=xt.ap[:], func=AF.Square, accum_out=s4.ap[:, 0:1]),
                         [xt], [junk, s4])
                    S.op('dve', lambda e: e.tensor_scalar(out=s4.ap[:, 1:2], in0=s4.ap[:, 0:1], scalar1=1.0 / D, scalar2=EPS,
                                                          op0=ALU.mult, op1=ALU.add), [s4], [s4])
                    S.op('act', lambda e: e.activation(out=s4.ap[:, 2:3], in_=s4.ap[:, 1:2], func=AF.Sqrt), [s4], [s4])
                    S.op('dve', lambda e: e.reciprocal(out=s4.ap[:, 3:4], in_=s4.ap[:, 2:3]), [s4], [s4])
                    h = hring.next()
                    S.op('dve', lambda e: e.scalar_tensor_tensor(out=h.ap[:], in0=xt.ap[:], scalar=s4.ap[:, 3:4], in1=A_bc[which].ap[:],
                                                                 op0=ALU.mult, op1=ALU.mult), [xt, s4, A_bc[which]], [h])
                    if final:
                        S.op('sp', lambda e: e.dma_start(out=OUT[(i - 2) * 128:(i - 1) * 128, :], in_=h.ap[:]), [h], [tOUT], dma=True)
                        continue
                    S.op('pool', lambda e: e.tensor_tensor(out=h.ap[:], in0=h.ap[:], in1=B_bc[which].ap[:], op=ALU.add),
                         [h, B_bc[which]], [h])
                    if want_tm:
                        hb = hbring.next()
                        S.op('act', lambda e: e.copy(out=hb.ap[:], in_=h.ap[:]), [h], [hb])
                        S.op('sp', lambda e: e.dma_start(out=H2TM[i * 128:(i + 1) * 128, :], in_=hb.ap[:]), [hb], [tH2], dma=True)
                    hf = hfring.next() if router else None
                    for g in range(4):
                        ps = psring.next()
                        for j in range(4):
                            kt = g * 4 + j
                            S.op('pe', lambda e: e.transpose(out=ps.ap[:, j * 128:(j + 1) * 128], in_=h.ap[:, kt * 128:(kt + 1) * 128],
                                                             identity=Csb['ident_f'].ap[:]), [h, Csb['ident_f']], [ps])
                        hb4 = htring.next()
                        if router:
                            S.op('act', lambda e: e.copy(out=hf.ap[:, g * 4:(g + 1) * 4, :], in_=ps.ap[:].rearrange("p (a b) -> p a b", b=128)),
                                 [ps], [hf])
                            S.op('dve', lambda e: e.tensor_copy(out=hb4.ap[:], in_=hf.ap[:, g * 4:(g + 1) * 4, :]), [hf], [hb4])
                        else:
                            S.op('dve', lambda e: e.tensor_copy(out=hb4.ap[:].rearrange("p a b -> p (a b)"), in_=ps.ap[:]), [ps], [hb4])
                        S.op('sp', lambda e: e.dma_start(out=HT[g * 4:(g + 1) * 4, :, i * 128:(i + 1) * 128].rearrange("k p t -> p k t"),
                                                         in_=hb4.ap[:]), [hb4], [tHT], dma=True)
                    if router and _os.environ.get("KRT3", "0") != "1":
                        ps = psring.next()
                        for kt in range(KT):
                            S.op('pe', lambda e: e.matmul(ps.ap[:, 0:NE], lhsT=hf.ap[:, kt, :], rhs=wr.ap[:, kt, :],
                                                          start=(kt == 0), stop=(kt == KT - 1)), [hf, wr], [ps])
                        s5 = stat.next()
                        S.op('dve', lambda e: e.reduce_max(out=s5.ap[:, 0:1], in_=ps.ap[:, 0:NE], axis=AX.X), [ps], [s5])
                        S.op('dve', lambda e: e.tensor_scalar(out=s5.ap[:, 1:2], in0=s5.ap[:, 0:1], scalar1=-1.0, scalar2=None,
                                                              op0=ALU.mult), [s5], [s5])
                        S.op('act', lambda e: e.activation(out=probs_tm.ap[:, i, :], in_=ps.ap[:, 0:NE], func=AF.Exp,
                                                           bias=s5.ap[:, 1:2], scale=1.0, accum_out=s5.ap[:, 2:3]),
                             [ps, s5], [probs_tm, s5])
                        S.op('dve', lambda e: e.reciprocal(out=s5.ap[:, 3:4], in_=s5.ap[:, 2:3]), [s5], [s5])
                        S.op('dve', lambda e: e.tensor_scalar(out=probs_tm.ap[:, i, :], in0=probs_tm.ap[:, i, :], scalar1=s5.ap[:, 3:4],
                                                              scalar2=None, op0=ALU.mult), [probs_tm, s5], [probs_tm])
                        ps2 = psring.next()
                        if _os.environ.get("KRT2", "0") == "1":
                            continue
                        S.op('dve', lambda e: e.tensor_copy(out=ptile.ap[:, 0:NE], in_=probs_tm.ap[:, i, :]), [probs_tm], [ptile])
                        S.op('pe', lambda e: e.transpose(out=ps2.ap[:, 0:128], in_=ptile.ap[:], identity=Csb['ident_f'].ap[:]),
                             [ptile, Csb['ident_f']], [ps2])
                        S.op('act', lambda e: e.copy(out=probsT.ap[:, i * 128:(i + 1) * 128], in_=ps2.ap[0:NE, 0:128]), [ps2], [probsT])
                S.barrier()

        def load_fm(stk, name, src, tsrc, nkt, t0, n):
            t = sb(stk, nm(name), [128, nkt, n], BF16)
            for kt in range(nkt):
                S.op('sp', lambda e: e.dma_start(out=t.ap[:, kt, :], in_=src[kt, :, t0:t0 + n]), [tsrc], [t], dma=True)
            return t

        def phase_proj_residual(li, src, tsrc, nkt, W, gate_k, with_ctx):
            wsrc = W.rearrange("(kt p) n -> p kt n", p=128)
            ngrp = 1 if nkt <= 16 else 2
            tile_lo = 0 if with_ctx else 2
            per = (NT - tile_lo + ngrp - 1) // ngrp
            for gi in range(ngrp):
                tl = list(range(tile_lo + gi * per, min(NT, tile_lo + (gi + 1) * per)))
                with contextlib.ExitStack() as ph:
                    g_bc = [bcast_load(ph, 'g', MOD[which, (3 * gate_k + 2) * D:(3 * gate_k + 3) * D]) for which in (0, 1)]
                    t0 = tl[0] * 128
                    a = load_fm(ph, 'a', src, tsrc, nkt, t0, len(tl) * 128)
                    nwb = 3 if nkt <= 16 else 2
                    wring = Ring([sb(ph, nm('wo'), [128, nkt, 512], BF16) for _ in range(nwb)])
                    xring = Ring([sb(ph, nm('xo'), [128, 512], F32) for _ in range(4)])
                    tring = Ring([sb(ph, nm('to'), [128, 512], F32) for _ in range(3)])
                    wpf = Prefetch(wring, [(lambda t, cch=cch: S.op('pool', lambda e: e.dma_start(out=t.ap[:], in_=wsrc[:, :, cch * 512:(cch + 1) * 512]),
                                                                     [tIN], [t], dma=True)) for cch in range(4)], nwb - 1)
                    units = [(cch, i) for cch in range(4) for i in tl]
                    xpf = Prefetch(xring, [(lambda t, cch=cch, i=i: S.op('sp', lambda e: e.dma_start(out=t.ap[:], in_=XR[i * 128:(i + 1) * 128, cch * 512:(cch + 1) * 512]),
                                                                         [tXR[i]], [t], dma=True)) for (cch, i) in units], 2)
                    for cch in range(4):
                        w = wpf.get(cch)
                        for i in tl:
                            which = 1 if i < 2 else 0
                            ps = psring.next()
                            c0 = i * 128 - t0
                            for kt in range(nkt):
                                S.op('pe', lambda e: e.matmul(ps.ap[:], lhsT=a.ap[:, kt, c0:c0 + 128], rhs=w.ap[:, kt, :],
                                                              start=(kt == 0), stop=(kt == nkt - 1)), [a, w], [ps])
                            xt = xpf.get(units.index((cch, i)))
                            tm = tring.next()
                            S.op('dve', lambda e: e.tensor_tensor(out=tm.ap[:], in0=ps.ap[:], in1=g_bc[which].ap[:, cch * 512:(cch + 1) * 512],
                                                                  op=ALU.mult), [ps, g_bc[which]], [tm])
                            S.op('pool', lambda e: e.tensor_tensor(out=tm.ap[:], in0=tm.ap[:], in1=xt.ap[:], op=ALU.add), [tm, xt], [tm])
                            S.op('sp', lambda e: e.dma_start(out=XR[i * 128:(i + 1) * 128, cch * 512:(cch + 1) * 512], in_=tm.ap[:]),
                                 [tm], [tXR[i]], dma=True)
                    S.barrier()

        def phase_attn(li, j, with_ctx):
            Wqkv = I['attn_w_qkv'][j]
            wsrc = Wqkv.rearrange("(kt p) n -> p kt n", p=128)
            with contextlib.ExitStack() as ph:
                hT = load_fm(ph, 'hT', HT, tHT, KT, 0, TT)
                cosA = sb(ph, nm('cosA'), [128, TL], F32)
                sinA = sb(ph, nm('sinA'), [128, TL], F32)
                S.op('sp', lambda e: e.dma_start(out=cosA.ap[:], in_=CI['cosA']), [tIN], [cosA], dma=True)
                S.op('sp', lambda e: e.dma_start(out=sinA.ap[:], in_=CI['sinA']), [tIN], [sinA], dma=True)
                nq = sb(ph, nm('nq'), [128, 2], F32)
                S.op('sp', lambda e: e.dma_start(out=nq.ap[:, 0:1], in_=I['attn_q_norm'][j].rearrange("(p o) -> p o", o=1)), [tIN], [nq], dma=True)
                S.op('sp', lambda e: e.dma_start(out=nq.ap[:, 1:2], in_=I['attn_k_norm'][j].rearrange("(p o) -> p o", o=1)), [tIN], [nq], dma=True)
                wring = Ring([sb(ph, nm('wq'), [128, KT, 128], BF16) for _ in range(3)])
                sqr = Ring([sb(ph, nm('sq'), [128, 512], F32) for _ in range(3)])
                rsr = Ring([sb(ph, nm('rs'), [128, 512], F32) for _ in range(3)])
                qnr = Ring([sb(ph, nm('qn'), [128, 512], F32) for _ in range(4)])
                epsc = sb(ph, nm('epsc'), [128, 1], F32)
                S.op('dve', lambda e: e.memset(epsc.ap[:], EPS), [], [epsc])
                t1r = Ring([sb(ph, nm('t1'), [128, 512], F32) for _ in range(2)])
                t2r = Ring([sb(ph, nm('t2'), [128, 512], F32) for _ in range(2)])
                obr = Ring([sb(ph, nm('ob'), [128, 512], BF16) for _ in range(3)])
                chunks = [(0, 256, False)] + [(256 + c * 512, 512, True) for c in range(4)]
                q1, q2 = [], []

                def a2_tail1(psA, sq, n, col, lat, t0, cb):
                    psB = psring.next()
                    S.op('pe', lambda e: e.matmul(psB.ap[:, 0:n], lhsT=Csb['ones_f'].ap[:], rhs=sq.ap[:, 0:n], start=True, stop=True),
                         [sq, Csb['ones_f']], [psB])
                    rs = rsr.next()
                    S.op('act', lambda e: e.activation(out=rs.ap[:, 0:n], in_=psB.ap[:, 0:n], func=AF.Sqrt, bias=epsc.ap[:, 0:1], scale=1.0 / 128),
                         [psB, epsc], [rs])
                    S.op('dve', lambda e: e.reciprocal(out=rs.ap[:, 0:n], in_=rs.ap[:, 0:n]), [rs], [rs])
                    qn = qnr.next()
                    S.op('dve', lambda e: e.scalar_tensor_tensor(out=qn.ap[:, 0:n], in0=psA.ap[:, 0:n], scalar=nq.ap[:, col:col + 1],
                                                                 in1=rs.ap[:, 0:n], op0=ALU.mult, op1=ALU.mult), [psA, nq, rs], [qn])
                    return qn

                def a2_tail2(qn, n, lat, t0, cb):
                    ob = obr.next()
                    if lat:
                        psC = psring.next()
                        S.op('pe', lambda e: e.matmul(psC.ap[:, 0:n], lhsT=Csb['permA'].ap[:], rhs=qn.ap[:, 0:n], start=True, stop=True),
                             [qn, Csb['permA']], [psC])
                        l0 = t0 - TC
                        t1 = t1r.next()
                        t2 = t2r.next()
                        S.op('pool', lambda e: e.tensor_tensor(out=t1.ap[:, 0:n], in0=qn.ap[:, 0:n], in1=cosA.ap[:, l0:l0 + n], op=ALU.mult),
                             [qn, cosA], [t1])
                        S.op('dve', lambda e: e.tensor_tensor(out=t2.ap[:, 0:n], in0=psC.ap[:, 0:n], in1=sinA.ap[:, l0:l0 + n], op=ALU.mult),
                             [psC, sinA], [t2])
                        S.op('pool', lambda e: e.tensor_tensor(out=ob.ap[:, 0:n], in0=t1.ap[:, 0:n], in1=t2.ap[:, 0:n], op=ALU.add),
                             [t1, t2], [ob])
                    else:
                        S.op('act', lambda e: e.copy(out=ob.ap[:, 0:n], in_=qn.ap[:, 0:n]), [qn], [ob])
                    if cb < 16:
                        S.op('sp', lambda e: e.dma_start(out=QT[cb, :, t0:t0 + n], in_=ob.ap[:, 0:n]), [ob], [tQT], dma=True)
                    else:
                        S.op('sp', lambda e: e.dma_start(out=KTs[cb - 16, :, t0:t0 + n], in_=ob.ap[:, 0:n]), [ob], [tKT], dma=True)

                def a2_advance(flush=False):
                    while len(q1) > (0 if flush else 1):
                        (psA, sq, n, col, lat, t0, cb) = q1.pop(0)
                        qn = a2_tail1(psA, sq, n, col, lat, t0, cb)
                        q2.append((qn, n, lat, t0, cb))
                    while len(q2) > (0 if flush else 1):
                        a2_tail2(*q2.pop(0))

                wpf = Prefetch(wring, [(lambda t, cb=cb: S.op('pool', lambda e: e.dma_start(out=t.ap[:], in_=wsrc[:, :, cb * 128:(cb + 1) * 128]),
                                                                 [tIN], [t], dma=True)) for cb in range(20)], 2)
                for cb in range(20):
                    is_q = cb < 16
                    w = wpf.get(cb)
                    for (t0, n, lat) in chunks:
                        if is_q and (not lat) and (not with_ctx):
                            continue
                        psA = psring.next()
                        for kt in range(KT):
                            S.op('pe', lambda e: e.matmul(psA.ap[:, 0:n], lhsT=w.ap[:, kt, :], rhs=hT.ap[:, kt, t0:t0 + n],
                                                          start=(kt == 0), stop=(kt == KT - 1)), [w, hT], [psA])
                        sq = sqr.next()
                        S.op('act', lambda e: e.activation(out=sq.ap[:, 0:n], in_=psA.ap[:, 0:n], func=AF.Square), [psA], [sq])
                        q1.append((psA, sq, n, 0 if is_q else 1, lat, t0, cb))
                        a2_advance()
                a2_advance(flush=True)
                wv = sb(ph, nm('wv'), [128, KT, 512], BF16)
                S.op('pool', lambda e: e.dma_start(out=wv.ap[:], in_=wsrc[:, :, 2560:3072]), [tIN], [wv], dma=True)
                for i in range(NT):
                    ps = psring.next()
                    for kt in range(KT):
                        S.op('pe', lambda e: e.matmul(ps.ap[:], lhsT=hT.ap[:, kt, i * 128:(i + 1) * 128], rhs=wv.ap[:, kt, :],
                                                      start=(kt == 0), stop=(kt == KT - 1)), [hT, wv], [ps])
                    ob = obr.next()
                    S.op('act', lambda e: e.copy(out=ob.ap[:], in_=ps.ap[:]), [ps], [ob])
                    S.op('sp', lambda e: e.dma_start(out=Vs[i * 128:(i + 1) * 128, :], in_=ob.ap[:]), [ob], [tV], dma=True)
                S.barrier()
            with contextlib.ExitStack() as ph:
                scale = 128 ** -0.5
                ktr = Ring([sb(ph, nm('kt'), [128, TT], BF16) for _ in range(2)])
                vr = Ring([sb(ph, nm('v'), [128, NT, 128], BF16) for _ in range(2)])
                qr = Ring([sb(ph, nm('q'), [128, TT], BF16) for _ in range(2)])
                er = Ring([sb(ph, nm('e'), [128, 512], BF16) for _ in range(5)])
                rdr = Ring([sb(ph, nm('rd'), [128, 512], F32) for _ in range(2)])
                oor = Ring([sb(ph, nm('oo'), [128, 512], BF16) for _ in range(2)])
                psS = Ring(PS[0:3])
                psO = Ring(PS[3:5])
                psD = Ring(PS[5:7])
                LOOK = 2
                pend = []

                def emit_pv(ee, n, jt, pO, pD, idx, nk, v, t0, h_):
                    S.op('pe', lambda e: e.matmul(pO.ap[:, 0:n], lhsT=v.ap[:, jt, :], rhs=ee.ap[:, 0:n],
                                                  start=(idx == 0), stop=(idx == nk - 1)), [v, ee], [pO])
                    S.op('pe', lambda e: e.matmul(pD.ap[:, 0:n], lhsT=Csb['ones_b'].ap[:], rhs=ee.ap[:, 0:n],
                                                  start=(idx == 0), stop=(idx == nk - 1)), [Csb['ones_b'], ee], [pD])
                    if idx == nk - 1:
                        rd = rdr.next()
                        S.op('dve', lambda e: e.reciprocal(out=rd.ap[:, 0:n], in_=pD.ap[:, 0:n]), [pD], [rd])
                        oo = oor.next()
                        S.op('dve', lambda e: e.tensor_tensor(out=oo.ap[:, 0:n], in0=pO.ap[:, 0:n], in1=rd.ap[:, 0:n], op=ALU.mult),
                             [pO, rd], [oo])
                        S.op('sp', lambda e: e.dma_start(out=OT[h_, :, t0:t0 + n], in_=oo.ap[:, 0:n]), [oo], [tOT], dma=True)

                for kv in range(4):
                    kt_ = ktr.next()
                    S.op('sp', lambda e: e.dma_start(out=kt_.ap[:], in_=KTs[kv]), [tKT], [kt_], dma=True)
                    v = vr.next()
                    for i in range(NT):
                        S.op('sp', lambda e: e.dma_start(out=v.ap[:, i, :], in_=Vs[i * 128:(i + 1) * 128, kv * 128:(kv + 1) * 128]),
                             [tV], [v], dma=True)
                    for hh in range(4):
                        h_ = kv * 4 + hh
                        q = qr.next()
                        S.op('sp', lambda e: e.dma_start(out=q.ap[:], in_=QT[h_]), [tQT], [q], dma=True)
                        qchunks = [(256 + c * 512, 512, list(range(NT))) for c in range(4)]
                        if with_ctx:
                            qchunks = [(0, 256, [0, 1])] + qchunks
                        for (t0, n, ktiles) in qchunks:
                            pO = psO.next()
                            pD = psD.next()
                            for idx, jt in enumerate(ktiles):
                                pS = psS.next()
                                S.op('pe', lambda e: e.matmul(pS.ap[:, 0:n], lhsT=kt_.ap[:, jt * 128:(jt + 1) * 128], rhs=q.ap[:, t0:t0 + n],
                                                              start=True, stop=True), [kt_, q], [pS])
                                ee = er.next()
                                S.op('act', lambda e: e.activation(out=ee.ap[:, 0:n], in_=pS.ap[:, 0:n], func=AF.Exp, scale=scale), [pS], [ee])
                                pend.append((ee, n, jt, pO, pD, idx, len(ktiles), v, t0, h_))
                                if len(pend) > LOOK:
                                    emit_pv(*pend.pop(0))
                while pend:
                    emit_pv(*pend.pop(0))
                S.barrier()
            phase_proj_residual(li, OT, tOT, 16, I['attn_w_o'][j], 0, with_ctx)


        def phase_ret(li, j, with_ctx):
            Win = I['ret_w_in'][j]
            wsrc = Win.rearrange("(kt p) n -> p kt n", p=128)
            chunks = [(0, 256, False)] + [(256 + c * 512, 512, True) for c in range(4)]
            with contextlib.ExitStack() as ph:
                hT = load_fm(ph, 'hT', HT, tHT, KT, 0, TT)
                cs = {}
                for k in ('cosR0', 'sinR0', 'cosR1', 'sinR1'):
                    cs[k] = sb(ph, nm(k), [128, TL], F32)
                    S.op('sp', lambda e: e.dma_start(out=cs[k].ap[:], in_=CI[k]), [tIN], [cs[k]], dma=True)
                wring = Ring([sb(ph, nm('wq'), [128, KT, 128], BF16) for _ in range(3)])
                qnr = Ring([sb(ph, nm('qn'), [128, 512], F32) for _ in range(2)])
                t1r = Ring([sb(ph, nm('t1'), [128, 512], F32) for _ in range(2)])
                t2r = Ring([sb(ph, nm('t2'), [128, 512], F32) for _ in range(2)])
                obr = Ring([sb(ph, nm('ob'), [128, 512], BF16) for _ in range(3)])
                rq = []

                def r2_store(ob, n, t0, cb):
                    if cb < 16:
                        S.op('sp', lambda e: e.dma_start(out=QT[cb, :, t0:t0 + n], in_=ob.ap[:, 0:n]), [ob], [tQT], dma=True)
                    else:
                        S.op('sp', lambda e: e.dma_start(out=RK[cb - 16, :, t0:t0 + n], in_=ob.ap[:, 0:n]), [ob], [tRK], dma=True)

                def r2_tail(qn, n, t0, a, cb):
                    ob = obr.next()
                    psC = psring.next()
                    S.op('pe', lambda e: e.matmul(psC.ap[:, 0:n], lhsT=Csb['permR'].ap[:], rhs=qn.ap[:, 0:n], start=True, stop=True),
                         [qn, Csb['permR']], [psC])
                    l0 = t0 - TC
                    t1 = t1r.next()
                    t2 = t2r.next()
                    ck = cs['cosR%d' % a]
                    sk = cs['sinR%d' % a]
                    S.op('pool', lambda e: e.tensor_tensor(out=t1.ap[:, 0:n], in0=qn.ap[:, 0:n], in1=ck.ap[:, l0:l0 + n], op=ALU.mult),
                         [qn, ck], [t1])
                    S.op('dve', lambda e: e.tensor_tensor(out=t2.ap[:, 0:n], in0=psC.ap[:, 0:n], in1=sk.ap[:, l0:l0 + n], op=ALU.mult),
                         [psC, sk], [t2])
                    S.op('pool', lambda e: e.tensor_tensor(out=ob.ap[:, 0:n], in0=t1.ap[:, 0:n], in1=t2.ap[:, 0:n], op=ALU.add),
                         [t1, t2], [ob])
                    r2_store(ob, n, t0, cb)

                rcols = [cb * 128 for cb in range(32)] + [8192 + gb * 128 for gb in range(32)]
                wpf = Prefetch(wring, [(lambda t, c0=c0: S.op('pool', lambda e: e.dma_start(out=t.ap[:], in_=wsrc[:, :, c0:c0 + 128]),
                                                                 [tIN], [t], dma=True)) for c0 in rcols], 2)
                for cb in range(32):
                    is_q = cb < 16
                    a = cb % 2
                    col0 = cb * 128 if is_q else 2048 + (cb - 16) * 128
                    scl = 1.0 if is_q else 1.0 / 16.0
                    w = wpf.get(cb)
                    for (t0, n, lat) in chunks:
                        psA = psring.next()
                        for kt in range(KT):
                            S.op('pe', lambda e: e.matmul(psA.ap[:, 0:n], lhsT=w.ap[:, kt, :], rhs=hT.ap[:, kt, t0:t0 + n],
                                                          start=(kt == 0), stop=(kt == KT - 1)), [w, hT], [psA])
                        if lat:
                            qn = qnr.next()
                            S.op('act', lambda e: e.mul(out=qn.ap[:, 0:n], in_=psA.ap[:, 0:n], mul=scl), [psA], [qn])
                            rq.append((qn, n, t0, a, cb))
                            if len(rq) > 1:
                                r2_tail(*rq.pop(0))
                        else:
                            ob = obr.next()
                            S.op('act', lambda e: e.mul(out=ob.ap[:, 0:n], in_=psA.ap[:, 0:n], mul=scl), [psA], [ob])
                            r2_store(ob, n, t0, cb)
                while rq:
                    r2_tail(*rq.pop(0))
                for gb in range(32):
                    col0 = 8192 + gb * 128
                    w = wpf.get(32 + gb)
                    for (t0, n, lat) in chunks:
                        psA = psring.next()
                        for kt in range(KT):
                            S.op('pe', lambda e: e.matmul(psA.ap[:, 0:n], lhsT=w.ap[:, kt, :], rhs=hT.ap[:, kt, t0:t0 + n],
                                                          start=(kt == 0), stop=(kt == KT - 1)), [w, hT], [psA])
                        ob = obr.next()
                        S.op('act', lambda e: e.activation(out=ob.ap[:, 0:n], in_=psA.ap[:, 0:n], func=AF.Silu), [psA], [ob])
                        S.op('sp', lambda e: e.dma_start(out=RG[gb, :, t0:t0 + n], in_=ob.ap[:, 0:n]), [ob], [tRG], dma=True)
                wvr = Ring([sb(ph, nm('wv'), [128, KT, 512], BF16) for _ in range(2)])
                for vc in range(8):
                    wv = wvr.next()
                    S.op('pool', lambda e: e.dma_start(out=wv.ap[:], in_=wsrc[:, :, 4096 + vc * 512:4096 + (vc + 1) * 512]), [tIN], [wv], dma=True)
                    for i in range(NT):
                        ps = psring.next()
                        for kt in range(KT):
                            S.op('pe', lambda e: e.matmul(ps.ap[:], lhsT=hT.ap[:, kt, i * 128:(i + 1) * 128], rhs=wv.ap[:, kt, :],
                                                          start=(kt == 0), stop=(kt == KT - 1)), [hT, wv], [ps])
                        ob = obr.next()
                        if i % 2:
                            S.op('act', lambda e: e.copy(out=ob.ap[:], in_=ps.ap[:]), [ps], [ob])
                        else:
                            S.op('dve', lambda e: e.tensor_copy(out=ob.ap[:], in_=ps.ap[:]), [ps], [ob])
                        S.op('sp', lambda e: e.dma_start(out=RV[i * 128:(i + 1) * 128, vc * 512:(vc + 1) * 512], in_=ob.ap[:]), [ob], [tRV], dma=True)
                S.barrier()
            with contextlib.ExitStack() as ph:
                OFF = 1920
                SW = 3968
                CW = 2176
                ip = sb(ph, nm('ip'), [128, SW], F32)
                rp = sb(ph, nm('rp'), [128, SW], F32)
                rn = sb(ph, nm('rn'), [128, SW], F32)
                strip = sb(ph, nm('strip'), [128, SW], F32)
                tmpB = sb(ph, nm('tmpB'), [128, SW], F32)
                Ef = sb(ph, nm('Ef'), [128, CW], F32)
                Eb = sb(ph, nm('Eb'), [128, CW], F32)
                Cs = sb(ph, nm('Cs'), [128, CW], F32)
                tmpC = sb(ph, nm('tmpC'), [128, CW], F32)
                S.op('sp', lambda e: e.dma_start(out=ip.ap[:], in_=CI['retDelta']), [tIN], [ip], dma=True)
                S.op('sp', lambda e: e.dma_start(out=Ef.ap[:], in_=CI['retEf']), [tIN], [Ef], dma=True)
                S.op('sp', lambda e: e.dma_start(out=Eb.ap[:], in_=CI['retEb']), [tIN], [Eb], dma=True)
                S.op('dve', lambda e: e.tensor_scalar(out=rp.ap[:], in0=ip.ap[:], scalar1=0.0, scalar2=None, op0=ALU.max), [ip], [rp])
                S.op('dve', lambda e: e.tensor_tensor(out=rn.ap[:], in0=rp.ap[:], in1=ip.ap[:], op=ALU.subtract), [rp, ip], [rn])
                S.op('dve', lambda e: e.tensor_scalar(out=ip.ap[:], in0=ip.ap[:], scalar1=0.0, scalar2=None, op0=ALU.is_ge), [ip], [ip])
                lg = sb(ph, nm('lg'), [128, 16], F32)
                S.op('sp', lambda e: e.dma_start(out=lg.ap[:], in_=I['ret_decay_logit'][j].rearrange("a h -> (a h)").partition_broadcast(128)),
                     [tIN], [lg], dma=True)
                S.op('act', lambda e: e.activation(out=lg.ap[:], in_=lg.ap[:], func=AF.Exp, scale=-1.0), [lg], [lg])
                S.op('dve', lambda e: e.tensor_scalar(out=lg.ap[:], in0=lg.ap[:], scalar1=1.0, scalar2=None, op0=ALU.add), [lg], [lg])
                S.op('act', lambda e: e.activation(out=lg.ap[:], in_=lg.ap[:], func=AF.Ln), [lg], [lg])
                S.op('dve', lambda e: e.tensor_scalar(out=lg.ap[:], in0=lg.ap[:], scalar1=-1.0, scalar2=None, op0=ALU.mult), [lg], [lg])
                q2r = Ring([sb(ph, nm('q2'), [128, 2, TT], BF16) for _ in range(1)])
                k2r = Ring([sb(ph, nm('k2'), [128, 2, TT], BF16) for _ in range(1)])
                vhr = Ring([sb(ph, nm('vh'), [128, NT, 512], BF16) for _ in range(1)])
                smr = Ring([sb(ph, nm('sm'), [128, 512], BF16) for _ in range(3)])
                sqr = Ring([sb(ph, nm('sq'), [128, 512], F32) for _ in range(2)])
                rsr = Ring([sb(ph, nm('rs'), [128, 512], F32) for _ in range(1)])
                gr = Ring([sb(ph, nm('g'), [128, 512], BF16) for _ in range(2)])
                tmr = Ring([sb(ph, nm('tm'), [128, 512], F32) for _ in range(2)])
                obr = Ring([sb(ph, nm('ob'), [128, 512], BF16) for _ in range(2)])
                O = PS[0:4]
                pSr = Ring(PS[4:6])
                pN = PS[6]
                rpend = []

                def emit_rpv(sm, n, jt, idx, nsrc, vh):
                    for vt in range(4):
                        S.op('pe', lambda e: e.matmul(O[vt].ap[:, 0:n], lhsT=vh.ap[:, jt, vt * 128:(vt + 1) * 128], rhs=sm.ap[:, 0:n],
                                                      start=(idx == 0), stop=(idx == nsrc - 1)), [vh, sm], [O[vt]])

                for h_ in range(8):
                    S.op('act', lambda e: e.activation(out=strip.ap[:], in_=rp.ap[:], func=AF.Exp, scale=lg.ap[:, h_:h_ + 1]), [rp, lg], [strip])
                    S.op('act', lambda e: e.activation(out=tmpB.ap[:], in_=rn.ap[:], func=AF.Exp, scale=lg.ap[:, 8 + h_:9 + h_]), [rn, lg], [tmpB])
                    S.op('pool', lambda e: e.tensor_tensor(out=strip.ap[:], in0=strip.ap[:], in1=tmpB.ap[:], op=ALU.subtract), [strip, tmpB], [strip])
                    S.op('dve', lambda e: e.tensor_tensor(out=strip.ap[:], in0=strip.ap[:], in1=ip.ap[:], op=ALU.mult), [strip, ip], [strip])
                    S.op('pool', lambda e: e.tensor_tensor(out=strip.ap[:], in0=strip.ap[:], in1=tmpB.ap[:], op=ALU.add), [strip, tmpB], [strip])
                    S.op('act', lambda e: e.activation(out=Cs.ap[:], in_=Ef.ap[:], func=AF.Exp, scale=lg.ap[:, h_:h_ + 1]), [Ef, lg], [Cs])
                    S.op('act', lambda e: e.activation(out=tmpC.ap[:], in_=Eb.ap[:], func=AF.Exp, scale=lg.ap[:, 8 + h_:9 + h_]), [Eb, lg], [tmpC])
                    S.op('pool', lambda e: e.tensor_tensor(out=Cs.ap[:], in0=Cs.ap[:], in1=tmpC.ap[:], op=ALU.add), [Cs, tmpC], [Cs])
                    q2 = q2r.next()
                    k2 = k2r.next()
                    vh = vhr.next()
                    for a in range(2):
                        S.op('sp', lambda e: e.dma_start(out=q2.ap[:, a, :], in_=QT[2 * h_ + a]), [tQT], [q2], dma=True)
                        S.op('sp', lambda e: e.dma_start(out=k2.ap[:, a, :], in_=RK[2 * h_ + a]), [tRK], [k2], dma=True)
                    for i in range(NT):
                        S.op('sp', lambda e: e.dma_start(out=vh.ap[:, i, :], in_=RV[i * 128:(i + 1) * 128, h_ * 512:(h_ + 1) * 512]), [tRV], [vh], dma=True)
                    for (t0, n, lat) in chunks:
                        srcs = list(range(NT)) if lat else [0, 1]
                        for idx, jt in enumerate(srcs):
                            pS = pSr.next()
                            for a in range(2):
                                S.op('pe', lambda e: e.matmul(pS.ap[:, 0:n], lhsT=k2.ap[:, a, jt * 128:(jt + 1) * 128], rhs=q2.ap[:, a, t0:t0 + n],
                                                              start=(a == 0), stop=(a == 1)), [k2, q2], [pS])
                            if not lat:
                                x0 = 0 - 128 * jt + OFF
                                mk, mt = strip.ap[:, x0:x0 + n], strip
                            elif jt < 2:
                                y0 = (t0 - TC) + (128 if jt == 0 else 0)
                                mk, mt = Cs.ap[:, y0:y0 + n], Cs
                            else:
                                x0 = (t0 - TC) - 128 * (jt - 2) + OFF
                                mk, mt = strip.ap[:, x0:x0 + n], strip
                            sm = smr.next()
                            S.op('dve', lambda e: e.tensor_tensor(out=sm.ap[:, 0:n], in0=pS.ap[:, 0:n], in1=mk, op=ALU.mult), [pS, mt], [sm])
                            rpend.append((sm, n, jt, idx, len(srcs), vh))
                            if len(rpend) > 1:
                                emit_rpv(*rpend.pop(0))
                        while rpend:
                            emit_rpv(*rpend.pop(0))
                        for vt in range(4):
                            sq = sqr.next()
                            S.op('act', lambda e: e.activation(out=sq.ap[:, 0:n], in_=O[vt].ap[:, 0:n], func=AF.Square), [O[vt]], [sq])
                            S.op('pe', lambda e: e.matmul(pN.ap[:, 0:n], lhsT=Csb['ones_f'].ap[:], rhs=sq.ap[:, 0:n], start=(vt == 0), stop=(vt == 3)),
                                 [sq, Csb['ones_f']], [pN])
                        rs = rsr.next()
                        S.op('dve', lambda e: e.tensor_scalar(out=rs.ap[:, 0:n], in0=pN.ap[:, 0:n], scalar1=1.0 / 512, scalar2=EPS,
                                                              op0=ALU.mult, op1=ALU.add), [pN], [rs])
                        S.op('act', lambda e: e.activation(out=rs.ap[:, 0:n], in_=rs.ap[:, 0:n], func=AF.Sqrt), [rs], [rs])
                        S.op('dve', lambda e: e.reciprocal(out=rs.ap[:, 0:n], in_=rs.ap[:, 0:n]), [rs], [rs])
                        for vt in range(4):
                            g = gr.next()
                            S.op('sp', lambda e: e.dma_start(out=g.ap[:, 0:n], in_=RG[h_ * 4 + vt, :, t0:t0 + n]), [tRG], [g], dma=True)
                            tm = tmr.next()
                            S.op('dve', lambda e: e.tensor_tensor(out=tm.ap[:, 0:n], in0=O[vt].ap[:, 0:n], in1=rs.ap[:, 0:n], op=ALU.mult), [O[vt], rs], [tm])
                            ob = obr.next()
                            S.op('pool', lambda e: e.tensor_tensor(out=ob.ap[:, 0:n], in0=tm.ap[:, 0:n], in1=g.ap[:, 0:n], op=ALU.mult), [tm, g], [ob])
                            S.op('sp', lambda e: e.dma_start(out=OT[h_ * 4 + vt, :, t0:t0 + n], in_=ob.ap[:, 0:n]), [ob], [tOT], dma=True)
                S.barrier()
            phase_proj_residual(li, OT, tOT, 32, I['ret_w_o'][j], 0, with_ctx)


        def phase_hyena(li, j, with_ctx):
            wsrc = I['hy_w_in'][j].rearrange("(kt p) n -> p kt n", p=128)
            chunks = [(0, 256, False)] + [(256 + c * 512, 512, True) for c in range(4)]
            ZW = 2308
            with contextlib.ExitStack() as ph:
                hT = load_fm(ph, 'hT', HT, tHT, KT, 0, TT)
                A1 = sb(ph, nm('A1'), [128, 128], F32)
                A2 = sb(ph, nm('A2'), [80, 128], F32)
                cwT = sb(ph, nm('cwT'), [128, 208], F32)
                cwv = I['hy_conv_w'][j].rearrange("k (cb p) -> (k cb) p", p=128)
                S.op('sp', lambda e: e.dma_start(out=A1.ap[:], in_=cwv[0:128, :]), [tIN], [A1], dma=True)
                S.op('sp', lambda e: e.dma_start(out=A2.ap[0:16, :], in_=cwv[128:144, :]), [tIN], [A2], dma=True)
                S.op('sp', lambda e: e.dma_start(out=A2.ap[16:64, :], in_=I['hy_conv_b'][j].rearrange("(cb p) -> cb p", p=128)), [tIN], [A2], dma=True)
                S.op('sp', lambda e: e.dma_start(out=A2.ap[64:80, :], in_=I['hy_skip'][j].rearrange("(cb p) -> cb p", p=128)), [tIN], [A2], dma=True)
                ps = psring.next()
                S.op('pe', lambda e: e.transpose(out=ps.ap[:, 0:128], in_=A1.ap[:], identity=Csb['ident_f'].ap[:]), [A1, Csb['ident_f']], [ps])
                S.op('pe', lambda e: e.transpose(out=ps.ap[:, 128:208], in_=A2.ap[:], identity=Csb['ident_f'].ap[0:80, 0:80]), [A2, Csb['ident_f']], [ps])
                S.op('dve', lambda e: e.tensor_copy(out=cwT.ap[:], in_=ps.ap[:, 0:208]), [ps], [cwT])
                S.op('sp', lambda e: e.dma_start(out=SKIPT[:, :], in_=cwT.ap[:, 192:208]), [cwT], [tSKIPT], dma=True)
                wring = Ring([sb(ph, nm('wq'), [128, KT, 128], BF16) for _ in range(3)])
                zbs = [sb(ph, nm('zb'), [128, ZW], F32) for _ in range(3)]
                for zb in zbs:
                    S.op('pool', lambda e: e.memset(zb.ap[:], 0.0), [], [zb])
                zcs = [sb(ph, nm('zc'), [128, TT], F32) for _ in range(3)]
                ub = sb(ph, nm('ub'), [128, TT], BF16)
                ust = Ring([sb(ph, nm('ust'), [128, 6, 128], BF16) for _ in range(2)])
                hblocks = [cb for ct in range(KT) for cb in (16 + ct, 32 + ct, ct)]
                wpf = Prefetch(wring, [(lambda t, cb=cb: S.op('pool', lambda e: e.dma_start(out=t.ap[:], in_=wsrc[:, :, cb * 128:(cb + 1) * 128]),
                                                                 [tIN], [t], dma=True)) for cb in hblocks], 2)
                for ct in range(KT):
                    for bi, cb in enumerate((16 + ct, 32 + ct, ct)):
                        w = wpf.get(ct * 3 + bi)
                        zb = zbs[bi]
                        for (t0, n, lat) in chunks:
                            psA = psring.next()
                            for kt in range(KT):
                                S.op('pe', lambda e: e.matmul(psA.ap[:, 0:n], lhsT=w.ap[:, kt, :], rhs=hT.ap[:, kt, t0:t0 + n],
                                                              start=(kt == 0), stop=(kt == KT - 1)), [w, hT], [psA])
                            z0 = (1 + t0) if not lat else (259 + t0 - TC)
                            S.op('act', lambda e: e.copy(out=zb.ap[:, z0:z0 + n], in_=psA.ap[:, 0:n]), [psA], [zb])
                        zc = zcs[bi]
                        for (zoff, t0, n) in ((0, 0, TC), (258, TC, TL)):
                            S.op('dve', lambda e: e.tensor_scalar(out=zc.ap[:, t0:t0 + n], in0=zb.ap[:, zoff + 1:zoff + 1 + n],
                                                                  scalar1=cwT.ap[:, 48 + cb:49 + cb], scalar2=cwT.ap[:, 144 + cb:145 + cb],
                                                                  op0=ALU.mult, op1=ALU.add), [zb, cwT], [zc])
                            S.op('dve', lambda e: e.scalar_tensor_tensor(out=zc.ap[:, t0:t0 + n], in0=zb.ap[:, zoff:zoff + n],
                                                                         scalar=cwT.ap[:, cb:cb + 1], in1=zc.ap[:, t0:t0 + n],
                                                                         op0=ALU.mult, op1=ALU.add), [zb, cwT, zc], [zc])
                            S.op('dve', lambda e: e.scalar_tensor_tensor(out=zc.ap[:, t0:t0 + n], in0=zb.ap[:, zoff + 2:zoff + 2 + n],
                                                                         scalar=cwT.ap[:, 96 + cb:97 + cb], in1=zc.ap[:, t0:t0 + n],
                                                                         op0=ALU.mult, op1=ALU.add), [zb, cwT, zc], [zc])
                    S.op('pool', lambda e: e.tensor_tensor(out=zcs[0].ap[:], in0=zcs[0].ap[:], in1=zcs[1].ap[:], op=ALU.mult), [zcs[0], zcs[1]], [zcs[0]])
                    S.op('sp', lambda e: e.dma_start(out=UTF[ct], in_=zcs[0].ap[:]), [zcs[0]], [tUTF], dma=True)
                    S.op('sp', lambda e: e.dma_start(out=X0F[ct], in_=zcs[2].ap[:]), [zcs[2]], [tX0F], dma=True)
                    S.op('act', lambda e: e.copy(out=ub.ap[:], in_=zcs[0].ap[:]), [zcs[0]], [ub])
                    for g in range(3):
                        for jj in range(6):
                            i = g * 6 + jj
                            S.op('pe', lambda e: e.transpose(out=PSB.ap[:, jj * 128:(jj + 1) * 128], in_=ub.ap[:, i * 128:(i + 1) * 128],
                                                             identity=Csb['ident_b'].ap[:]), [ub, Csb['ident_b']], [PSB])
                        us = ust.next()
                        S.op('dve', lambda e: e.tensor_copy(out=us.ap[:].rearrange("p a b -> p (a b)"), in_=PSB.ap[:, 0:768]), [PSB], [us])
                        for hh in range(2):
                            i0 = g * 6 + hh * 3
                            S.op('sp', lambda e: e.dma_start(out=UTM[i0 * 128:(i0 + 3) * 128, ct * 128:(ct + 1) * 128].rearrange("(i p) c -> p i c", p=128),
                                                             in_=us.ap[:, hh * 3:(hh + 1) * 3, :]), [us], [tUTM], dma=True)
                S.barrier()
            for (tag, L, tok0) in ((('C', TC, 0),) if with_ctx else ()) + (('L', TL, TC),):
                hy_filter(j, tag, L)
                hy_conv(j, tag, L, tok0)
            phase_proj_residual(li, OT, tOT, 16, I['hy_w_out'][j], 0, with_ctx)

        def hy_filter(j, tag, L):
            with contextlib.ExitStack() as ph0:
                rnorm = sb(ph0, nm('rnorm'), [128, D], F32)
                hy_filter_inner(j, tag, L, rnorm)

        def hy_filter_inner(j, tag, L, rnorm):
            nkt = L // 128
            cw = min(512, L)
            with contextlib.ExitStack() as ph:
                zT = sb(ph, nm('zT'), [33, L], F32)
                S.op('sp', lambda e: e.dma_start(out=zT.ap[:], in_=CI['hyZ' + tag]), [tIN], [zT], dma=True)
                w1 = sb(ph, nm('w1'), [33, 128], F32)
                w2 = sb(ph, nm('w2'), [64, 128], F32)
                w3 = sb(ph, nm('w3'), [64, 2 * D], F32)
                S.op('dve', lambda e: e.memset(w1.ap[:], 0.0), [], [w1])
                S.op('dve', lambda e: e.memset(w2.ap[:], 0.0), [], [w2])
                S.op('sp', lambda e: e.dma_start(out=w1.ap[:, 0:64], in_=I['hy_f_w1'][j]), [tIN], [w1], dma=True)
                S.op('sp', lambda e: e.dma_start(out=w2.ap[:, 0:64], in_=I['hy_f_w2'][j]), [tIN], [w2], dma=True)
                S.op('sp', lambda e: e.dma_start(out=w3.ap[:], in_=I['hy_f_w3'][j]), [tIN], [w3], dma=True)
                pv = sb(ph, nm('pv'), [64, 4], F32)
                for ci, k in enumerate(('hy_f_b1', 'hy_f_freq1', 'hy_f_b2', 'hy_f_freq2')):
                    S.op('sp', lambda e: e.dma_start(out=pv.ap[:, ci:ci + 1], in_=I[k][j].rearrange("(p o) -> p o", o=1)), [tIN], [pv], dma=True)
                S.op('dve', lambda e: e.tensor_scalar(out=pv.ap[:, 1:2], in0=pv.ap[:, 1:2], scalar1=1.0 / (2 * math.pi), scalar2=None, op0=ALU.mult), [pv], [pv])
                S.op('dve', lambda e: e.tensor_scalar(out=pv.ap[:, 3:4], in0=pv.ap[:, 3:4], scalar1=1.0 / (2 * math.pi), scalar2=None, op0=ALU.mult), [pv], [pv])
                h1T = sb(ph, nm('h1T'), [64, L], F32)
                h2T = sb(ph, nm('h2T'), [64, L], F32)
                r = sb(ph, nm('r'), [64, 512], F32)
                ri = sb(ph, nm('ri'), [64, 512], mybir.dt.int32)
                rf = sb(ph, nm('rf'), [64, 512], F32)
                msk = sb(ph, nm('msk'), [64, 512], F32)

                def sin_layer(wt, kdim, src, dst, bcol):
                    for c0 in range(0, L, cw):
                        ps = psring.next()
                        S.op('pe', lambda e: e.matmul(ps.ap[:, 0:cw], lhsT=wt.ap[0:kdim, :], rhs=src.ap[0:kdim, c0:c0 + cw], start=True, stop=True),
                             [wt, src], [ps])
                        S.op('dve', lambda e: e.tensor_scalar(out=r.ap[:, 0:cw], in0=ps.ap[0:64, 0:cw], scalar1=pv.ap[:, bcol:bcol + 1],
                                                              scalar2=pv.ap[:, bcol + 1:bcol + 2], op0=ALU.add, op1=ALU.mult), [ps, pv], [r])
                        S.op('dve', lambda e: e.tensor_copy(out=ri.ap[:, 0:cw], in_=r.ap[:, 0:cw]), [r], [ri])
                        S.op('dve', lambda e: e.tensor_copy(out=rf.ap[:, 0:cw], in_=ri.ap[:, 0:cw]), [ri], [rf])
                        S.op('dve', lambda e: e.tensor_tensor(out=r.ap[:, 0:cw], in0=r.ap[:, 0:cw], in1=rf.ap[:, 0:cw], op=ALU.subtract), [r, rf], [r])
                        S.op('dve', lambda e: e.tensor_scalar(out=msk.ap[:, 0:cw], in0=r.ap[:, 0:cw], scalar1=0.5, scalar2=None, op0=ALU.is_gt), [r], [msk])
                        S.op('dve', lambda e: e.tensor_tensor(out=r.ap[:, 0:cw], in0=r.ap[:, 0:cw], in1=msk.ap[:, 0:cw], op=ALU.subtract), [r, msk], [r])
                        S.op('dve', lambda e: e.tensor_scalar(out=msk.ap[:, 0:cw], in0=r.ap[:, 0:cw], scalar1=-0.5, scalar2=None, op0=ALU.is_lt), [r], [msk])
                        S.op('dve', lambda e: e.tensor_tensor(out=r.ap[:, 0:cw], in0=r.ap[:, 0:cw], in1=msk.ap[:, 0:cw], op=ALU.add), [r, msk], [r])
                        S.op('act', lambda e: e.activation(out=dst.ap[:, c0:c0 + cw], in_=r.ap[:, 0:cw], func=AF.Sin, scale=6.28318), [r], [dst])
                sin_layer(w1, 33, zT, h1T, 0)
                sin_layer(w2, 64, h1T, h2T, 2)
                dring = Ring([sb(ph, nm('dec'), [128, 512], F32) for _ in range(3)])
                hfr = Ring([sb(ph, nm('hf'), [128, 512], F32) for _ in range(3)])
                hbr = Ring([sb(ph, nm('hb'), [128, 512], F32) for _ in range(3)])
                abr = Ring([sb(ph, nm('ab'), [128, 512], F32) for _ in range(3)])
                aacc = [sb(ph, nm('aacc'), [128, 512], F32) for _ in range(4)]
                for c4 in range(4):
                    S.op('pool', lambda e: e.memset(aacc[c4].ap[:], 0.0), [], [aacc[c4]])
                hpr = Ring([sb(ph, nm('hp'), [128, 512], BF16) for _ in range(3)])
                NP = PS[0:4]
                pr = Ring(PS[4:7])
                dpf = Prefetch(dring, [(lambda t, kt=kt, c4=c4: S.op('sp', lambda e: e.dma_start(out=t.ap[:], in_=CI['hyDecay' + tag][kt * 128:(kt + 1) * 128, c4 * 512:(c4 + 1) * 512]),
                                                                      [tIN], [t], dma=True)) for kt in range(nkt) for c4 in range(4)], 2)
                for kt in range(nkt):
                    for c4 in range(4):
                        dec = dpf.get(kt * 4 + c4)
                        hh = []
                        for dr, rg in ((0, hfr), (1, hbr)):
                            ps = pr.next()
                            S.op('pe', lambda e: e.matmul(ps.ap[:], lhsT=h2T.ap[:, kt * 128:(kt + 1) * 128], rhs=w3.ap[:, dr * D + c4 * 512:dr * D + (c4 + 1) * 512],
                                                          start=True, stop=True), [h2T, w3], [ps])
                            ht = rg.next()
                            S.op('dve', lambda e: e.tensor_tensor(out=ht.ap[:], in0=ps.ap[:], in1=dec.ap[:], op=ALU.mult), [ps, dec], [ht])
                            if dr == 1 and kt == 0:
                                S.op('dve', lambda e: e.memset(ht.ap[0:1, :], 0.0), [], [ht])
                            ab = abr.next()
                            S.op('act', lambda e: e.activation(out=ab.ap[:], in_=ht.ap[:], func=AF.Abs), [ht], [ab])
                            S.op('dve', lambda e: e.tensor_tensor(out=aacc[c4].ap[:], in0=aacc[c4].ap[:], in1=ab.ap[:], op=ALU.add), [ab, aacc[c4]], [aacc[c4]])
                            hh.append(ht)
                        hp = hpr.next()
                        S.op('pool', lambda e: e.tensor_tensor(out=hp.ap[:], in0=hh[0].ap[:], in1=hh[1].ap[:], op=ALU.add), [hh[0], hh[1]], [hp])
                        S.op('sp', lambda e: e.dma_start(out=HPM[0, kt * 128:(kt + 1) * 128, c4 * 512:(c4 + 1) * 512], in_=hp.ap[:]), [hp], [tHPM], dma=True)
                        hm = hpr.next()
                        S.op('pool', lambda e: e.tensor_tensor(out=hm.ap[:], in0=hh[0].ap[:], in1=hh[1].ap[:], op=ALU.subtract), [hh[0], hh[1]], [hm])
                        S.op('sp', lambda e: e.dma_start(out=HPM[1, kt * 128:(kt + 1) * 128, c4 * 512:(c4 + 1) * 512], in_=hm.ap[:]), [hm], [tHPM], dma=True)
                for c4 in range(4):
                    S.op('pe', lambda e: e.matmul(NP[c4].ap[:], lhsT=Csb['ones_f'].ap[:], rhs=aacc[c4].ap[:], start=True, stop=True),
                         [aacc[c4], Csb['ones_f']], [NP[c4]])
                    S.op('dve', lambda e: e.reciprocal(out=rnorm.ap[:, c4 * 512:(c4 + 1) * 512], in_=NP[c4].ap[:]), [NP[c4]], [rnorm])
                S.barrier()
            with contextlib.ExitStack() as ph:
                hpr = Ring([sb(ph, nm('hpc'), [128, nkt, 512], BF16) for _ in range(2)])
                hmr = Ring([sb(ph, nm('hmc'), [128, nkt, 512], BF16) for _ in range(2)])
                ctr = Ring([sb(ph, nm('ct'), [128, nkt, 128], BF16) for _ in range(3)])
                strr = Ring([sb(ph, nm('st'), [128, nkt, 128], BF16) for _ in range(3)])
                hor = Ring([sb(ph, nm('ho'), [128, 512], F32) for _ in range(4)])
                cpf = Prefetch(ctr, [(lambda t, ft=ft: S.op('sp', lambda e: e.dma_start(out=t.ap[:].rearrange("p a b -> p (a b)"), in_=CI['hyCT' + tag][ft]),
                                                             [tIN], [t], dma=True)) for _c in range(4) for ft in range(nkt)], 2)
                spf = Prefetch(strr, [(lambda t, ft=ft: S.op('sp', lambda e: e.dma_start(out=t.ap[:].rearrange("p a b -> p (a b)"), in_=CI['hyST' + tag][ft]),
                                                              [tIN], [t], dma=True)) for _c in range(4) for ft in range(nkt)], 2)
                for c4 in range(4):
                    hp = hpr.next()
                    hm = hmr.next()
                    for kt in range(nkt):
                        S.op('sp', lambda e: e.dma_start(out=hp.ap[:, kt, :], in_=HPM[0, kt * 128:(kt + 1) * 128, c4 * 512:(c4 + 1) * 512]), [tHPM], [hp], dma=True)
                        S.op('sp', lambda e: e.dma_start(out=hm.ap[:, kt, :], in_=HPM[1, kt * 128:(kt + 1) * 128, c4 * 512:(c4 + 1) * 512]), [tHPM], [hm], dma=True)
                    for ft in range(nkt):
                        ctt = cpf.get(c4 * nkt + ft)
                        stt = spf.get(c4 * nkt + ft)
                        for si, (mt, hx) in enumerate(((ctt, hp), (stt, hm))):
                            ps = psring.next()
                            for kt in range(nkt):
                                S.op('pe', lambda e: e.matmul(ps.ap[:], lhsT=mt.ap[:, kt, :], rhs=hx.ap[:, kt, :], start=(kt == 0), stop=(kt == nkt - 1)),
                                     [mt, hx], [ps])
                            ho = hor.next()
                            S.op('dve', lambda e: e.tensor_tensor(out=ho.ap[:], in0=ps.ap[:], in1=rnorm.ap[:, c4 * 512:(c4 + 1) * 512], op=ALU.mult), [ps, rnorm], [ho])
                            S.op('sp', lambda e: e.dma_start(out=HSPEC[tag][si, ft * 128:(ft + 1) * 128, c4 * 512:(c4 + 1) * 512], in_=ho.ap[:]), [ho], [tHSPEC[tag]], dma=True)
                S.barrier()

        def hy_conv(j, tag, L, tok0):
            nkt = L // 128
            tcw = min(512, L)
            ntc = L // tcw
            nfft = 2 * L
            with contextlib.ExitStack() as ph:
                skT = sb(ph, nm('skT'), [128, KT], F32)
                S.op('sp', lambda e: e.dma_start(out=skT.ap[:], in_=SKIPT[:, :]), [tSKIPT], [skT], dma=True)
                ur = Ring([sb(ph, nm('u'), [128, nkt, 512], BF16) for _ in range(1)])
                Yr = Ring([sb(ph, nm('Y'), [128, 2 * nkt, 512], BF16) for _ in range(1)])
                ctr = Ring([sb(ph, nm('ct'), [128, nkt, 128], BF16) for _ in range(3)])
                strr = Ring([sb(ph, nm('st'), [128, nkt, 128], BF16) for _ in range(3)])
                hcr = Ring([sb(ph, nm('hc'), [128, 512], F32) for _ in range(3)])
                hsr = Ring([sb(ph, nm('hs'), [128, 512], F32) for _ in range(3)])
                ucr = Ring([sb(ph, nm('uc'), [128, 512], F32) for _ in range(2)])
                usr = Ring([sb(ph, nm('us'), [128, 512], F32) for _ in range(2)])
                tr_ = Ring([sb(ph, nm('tt'), [128, 512], F32) for _ in range(4)])
                cfr = Ring([sb(ph, nm('cf'), [128, nkt, tcw], BF16) for _ in range(2)])
                sfr = Ring([sb(ph, nm('sf'), [128, nkt, tcw], BF16) for _ in range(2)])
                utr = Ring([sb(ph, nm('ut'), [128, 512], F32) for _ in range(2)])
                x0r = Ring([sb(ph, nm('x0'), [128, 512], F32) for _ in range(2)])
                obr = Ring([sb(ph, nm('ob'), [128, 512], BF16) for _ in range(2)])
                cpf = Prefetch(ctr, [(lambda t, ft=ft: S.op('sp', lambda e: e.dma_start(out=t.ap[:].rearrange("p a b -> p (a b)"), in_=CI['hyCT' + tag][ft]),
                                                             [tIN], [t], dma=True)) for _c in range(4) for ft in range(nkt)], 2)
                spf = Prefetch(strr, [(lambda t, ft=ft: S.op('sp', lambda e: e.dma_start(out=t.ap[:].rearrange("p a b -> p (a b)"), in_=CI['hyST' + tag][ft]),
                                                              [tIN], [t], dma=True)) for _c in range(4) for ft in range(nkt)], 2)
                hcpf = Prefetch(hcr, [(lambda t, ft=ft, c4=c4: S.op('sp', lambda e: e.dma_start(out=t.ap[:], in_=HSPEC[tag][0, ft * 128:(ft + 1) * 128, c4 * 512:(c4 + 1) * 512]),
                                                                     [tHSPEC[tag]], [t], dma=True)) for c4 in range(4) for ft in range(nkt)], 2)
                hspf = Prefetch(hsr, [(lambda t, ft=ft, c4=c4: S.op('sp', lambda e: e.dma_start(out=t.ap[:], in_=HSPEC[tag][1, ft * 128:(ft + 1) * 128, c4 * 512:(c4 + 1) * 512]),
                                                                     [tHSPEC[tag]], [t], dma=True)) for c4 in range(4) for ft in range(nkt)], 2)
                cfpf = Prefetch(cfr, [(lambda t, tc=tc: S.op('sp', lambda e: e.dma_start(out=t.ap[:].rearrange("p a b -> p (a b)"), in_=CI['hyCF' + tag][tc]),
                                                              [tIN], [t], dma=True)) for _c in range(4) for tc in range(ntc)], 1)
                sfpf = Prefetch(sfr, [(lambda t, tc=tc: S.op('sp', lambda e: e.dma_start(out=t.ap[:].rearrange("p a b -> p (a b)"), in_=CI['hySF' + tag][tc]),
                                                              [tIN], [t], dma=True)) for _c in range(4) for tc in range(ntc)], 1)
                for c4 in range(4):
                    u = ur.next()
                    for kt in range(nkt):
                        S.op('sp', lambda e: e.dma_start(out=u.ap[:, kt, :], in_=UTM[tok0 + kt * 128:tok0 + (kt + 1) * 128, c4 * 512:(c4 + 1) * 512]), [tUTM], [u], dma=True)
                    Y = Yr.next()
                    for ft in range(nkt):
                        ctt = cpf.get(c4 * nkt + ft)
                        stt = spf.get(c4 * nkt + ft)
                        hc = hcpf.get(c4 * nkt + ft)
                        hs = hspf.get(c4 * nkt + ft)
                        pc = psring.next()
                        pss = psring.next()
                        for kt in range(nkt):
                            S.op('pe', lambda e: e.matmul(pc.ap[:], lhsT=ctt.ap[:, kt, :], rhs=u.ap[:, kt, :], start=(kt == 0), stop=(kt == nkt - 1)), [ctt, u], [pc])
                        for kt in range(nkt):
                            S.op('pe', lambda e: e.matmul(pss.ap[:], lhsT=stt.ap[:, kt, :], rhs=u.ap[:, kt, :], start=(kt == 0), stop=(kt == nkt - 1)), [stt, u], [pss])
                        uc = ucr.next()
                        us = usr.next()
                        S.op('act', lambda e: e.copy(out=uc.ap[:], in_=pc.ap[:]), [pc], [uc])
                        S.op('act', lambda e: e.copy(out=us.ap[:], in_=pss.ap[:]), [pss], [us])
                        t1 = tr_.next(); t2 = tr_.next(); t3 = tr_.next(); t4 = tr_.next()
                        S.op('dve', lambda e: e.tensor_tensor(out=t1.ap[:], in0=uc.ap[:], in1=hc.ap[:], op=ALU.mult), [uc, hc], [t1])
                        S.op('pool', lambda e: e.tensor_tensor(out=t2.ap[:], in0=us.ap[:], in1=hs.ap[:], op=ALU.mult), [us, hs], [t2])
                        S.op('dve', lambda e: e.tensor_tensor(out=Y.ap[:, ft, :], in0=t1.ap[:], in1=t2.ap[:], op=ALU.subtract), [t1, t2], [Y])
                        S.op('pool', lambda e: e.tensor_tensor(out=t3.ap[:], in0=uc.ap[:], in1=hs.ap[:], op=ALU.mult), [uc, hs], [t3])
                        S.op('dve', lambda e: e.tensor_tensor(out=t4.ap[:], in0=us.ap[:], in1=hc.ap[:], op=ALU.mult), [us, hc], [t4])
                        S.op('pool', lambda e: e.tensor_tensor(out=Y.ap[:, nkt + ft, :], in0=t3.ap[:], in1=t4.ap[:], op=ALU.add), [t3, t4], [Y])
                    for tc in range(ntc):
                        cf = cfpf.get(c4 * ntc + tc)
                        sf = sfpf.get(c4 * ntc + tc)
                        for ctl in range(4):
                            cg = c4 * 4 + ctl
                            ps = psring.next()
                            for ft in range(nkt):
                                S.op('pe', lambda e: e.matmul(ps.ap[:, 0:tcw], lhsT=Y.ap[:, ft, ctl * 128:(ctl + 1) * 128], rhs=cf.ap[:, ft, :],
                                                              start=(ft == 0), stop=False), [Y, cf], [ps])
                                S.op('pe', lambda e: e.matmul(ps.ap[:, 0:tcw], lhsT=Y.ap[:, nkt + ft, ctl * 128:(ctl + 1) * 128], rhs=sf.ap[:, ft, :],
                                                              start=False, stop=(ft == nkt - 1)), [Y, sf], [ps])
                            g0 = tok0 + tc * tcw
                            ut = utr.next()
                            x0 = x0r.next()
                            S.op('sp', lambda e: e.dma_start(out=ut.ap[:, 0:tcw], in_=UTF[cg, :, g0:g0 + tcw]), [tUTF], [ut], dma=True)
                            S.op('sp', lambda e: e.dma_start(out=x0.ap[:, 0:tcw], in_=X0F[cg, :, g0:g0 + tcw]), [tX0F], [x0], dma=True)
                            S.op('pool', lambda e: e.tensor_scalar(out=ut.ap[:, 0:tcw], in0=ut.ap[:, 0:tcw], scalar1=skT.ap[:, cg:cg + 1], scalar2=None, op0=ALU.mult),
                                 [ut, skT], [ut])
                            S.op('dve', lambda e: e.scalar_tensor_tensor(out=ut.ap[:, 0:tcw], in0=ps.ap[:, 0:tcw], scalar=2.0 / nfft, in1=ut.ap[:, 0:tcw],
                                                                         op0=ALU.mult, op1=ALU.add), [ps, ut], [ut])
                            ob = obr.next()
                            S.op('pool', lambda e: e.tensor_tensor(out=ob.ap[:, 0:tcw], in0=ut.ap[:, 0:tcw], in1=x0.ap[:, 0:tcw], op=ALU.mult), [ut, x0], [ob])
                            S.op('sp', lambda e: e.dma_start(out=OT[cg, :, g0:g0 + tcw], in_=ob.ap[:, 0:tcw]), [ob], [tOT], dma=True)
                S.barrier()

        def phase_moe(li, with_ctx):
            with contextlib.ExitStack() as ph:
                work = sb(ph, nm('work'), [NE, TL], F32)
                m8 = sb(ph, nm('m8'), [NE, 8], F32)
                ones_r = sb(ph, nm('ones_r'), [NE, TL], F32)
                maskT = sb(ph, nm('maskT'), [NE, TT], F32)
                cum = sb(ph, nm('cum'), [NE, TT], F32)
                S.op('pool', lambda e: e.memset(ones_r.ap[:], 1.0), [], [ones_r])
                segs = [(TC, TL, CAPL)] + ([(0, TC, CAPC)] if with_ctx else [])
                for (t0, n, cap) in segs:
                    S.op('dve', lambda e: e.tensor_copy(out=work.ap[:, 0:n], in_=probsT.ap[:, t0:t0 + n]), [probsT], [work])
                    for it in range(cap // 8):
                        S.op('dve', lambda e: e.max(out=m8.ap[:], in_=work.ap[:, 0:n]), [work], [m8])
                        if it < cap // 8 - 1:
                            S.op('dve', lambda e: e.match_replace(out=work.ap[:, 0:n], in_to_replace=m8.ap[:], in_values=work.ap[:, 0:n],
                                                                  imm_value=-1.0), [m8, work], [work])
                    S.op('dve', lambda e: e.tensor_scalar(out=maskT.ap[:, t0:t0 + n], in0=probsT.ap[:, t0:t0 + n], scalar1=m8.ap[:, 7:8],
                                                          scalar2=None, op0=ALU.is_ge), [probsT, m8], [maskT])
                    S.op('dve', lambda e: e.tensor_tensor_scan(out=cum.ap[:, t0:t0 + n], data0=ones_r.ap[:, 0:n], data1=maskT.ap[:, t0:t0 + n],
                                                               initial=0.0, op0=ALU.mult, op1=ALU.add), [ones_r, maskT], [cum])
                    S.op('dve', lambda e: e.tensor_tensor(out=cum.ap[:, t0:t0 + n], in0=cum.ap[:, t0:t0 + n], in1=maskT.ap[:, t0:t0 + n],
                                                          op=ALU.mult), [cum, maskT], [cum])
                    S.op('dve', lambda e: e.tensor_scalar(out=cum.ap[:, t0:t0 + n], in0=cum.ap[:, t0:t0 + n], scalar1=-1.0, scalar2=None,
                                                          op0=ALU.add), [cum], [cum])
                if not with_ctx:
                    S.op('dve', lambda e: e.memset(cum.ap[:, 0:TC], -1.0), [], [cum])
                S.op('sp', lambda e: e.dma_start(out=POS[:, :], in_=cum.ap[:]), [cum], [tPOS], dma=True)
                gt = sb(ph, nm('gt'), [128, NE], F32)
                gh = sb(ph, nm('gh'), [128, NE], F32)
                for i in range(NT):
                    ps = psring.next()
                    S.op('pe', lambda e: e.transpose(out=ps.ap[:, 0:NE], in_=cum.ap[:, i * 128:(i + 1) * 128],
                                                     identity=Csb['ident_f'].ap[0:NE, 0:NE]), [cum, Csb['ident_f']], [ps])
                    S.op('act', lambda e: e.copy(out=posTM.ap[:, i, :], in_=ps.ap[:, 0:NE]), [ps], [posTM])
                    S.op('dve', lambda e: e.scalar_tensor_tensor(out=gt.ap[:], in0=posTM.ap[:, i, :], scalar=0.0, in1=probs_tm.ap[:, i, :],
                                                                 op0=ALU.is_ge, op1=ALU.mult), [posTM, probs_tm], [gt])
                    S.op('dve', lambda e: e.tensor_copy(out=GHL.ap[:, i, :, 0], in_=gt.ap[:]), [gt], [GHL])
                    S.op('dve', lambda e: e.tensor_copy(out=gh.ap[:], in_=GHL.ap[:, i, :, 0]), [GHL], [gh])
                    S.op('dve', lambda e: e.tensor_tensor(out=GHL.ap[:, i, :, 1], in0=gt.ap[:], in1=gh.ap[:], op=ALU.subtract), [gt, gh], [GHL])
                S.barrier()
            if stop == 'm2':
                return
            jts = [(0, 128), (128, 128)] + ([(256, 32)] if with_ctx else [])
            NJ = 288 if with_ctx else 256
            with contextlib.ExitStack() as ph:
                h2 = sb(ph, nm('h2'), [128, NT, D], BF16)
                for i in range(0 if with_ctx else 2, NT):
                    S.op('sp', lambda e: e.dma_start(out=h2.ap[:, i, :], in_=H2TM[i * 128:(i + 1) * 128, :]), [tH2], [h2], dma=True)
                wring = Ring([sb(ph, nm('we'), [128, KT, 512], BF16) for _ in range(4)])
                selr = Ring([sb(ph, nm('sel'), [128, NT, 256], BF16) for _ in range(2)])
                xgr = Ring([sb(ph, nm('xg'), [128, KT, 288], BF16) for _ in range(1)])
                actr = Ring([sb(ph, nm('act'), [128, 3, FF], BF16) for _ in range(1)])
                actTr = Ring([sb(ph, nm('actT'), [128, 8, 288], BF16) for _ in range(1)])
                sar = Ring([sb(ph, nm('sa'), [128, 512], F32) for _ in range(2)])
                ygr = Ring([sb(ph, nm('yg'), [128, 3, D], BF16) for _ in range(1)])
                gsr = Ring([sb(ph, nm('gs'), [128, 4], F32) for _ in range(2)])
                gsfr = Ring([sb(ph, nm('gsf'), [128, 8], F32) for _ in range(2)])
                def build_sel(ex):
                    sel = selr.next()
                    for i in range(0 if with_ctx else 2, NT):
                        n = 32 if i < 2 else 256
                        S.op('dve',
                             lambda e: e.tensor_scalar(out=sel.ap[:, i, 0:n], in0=Csb['iota_row'].ap[:, 0:n], scalar1=posTM.ap[:, i, ex:ex + 1],
                                                       scalar2=None, op0=ALU.is_equal), [Csb['iota_row'], posTM], [sel])
                    return sel
                sel_next = build_sel(0)
                for ex in range(NE):
                    sel = sel_next
                    gs = gsr.next()
                    psg = psring.next()
                    for ji, (j0, jn) in enumerate(jts):
                        tl = [0, 1] if j0 == 256 else list(range(2, NT))
                        for idx, i in enumerate(tl):
                            lo = 0 if j0 == 256 else j0
                            S.op('pe', lambda e: e.matmul(psg.ap[0:jn, ji * 2:ji * 2 + 2], lhsT=sel.ap[:, i, lo:lo + jn], rhs=GHL.ap[:, i, ex, :],
                                                          start=(idx == 0), stop=(idx == len(tl) - 1)), [sel, GHL], [psg])
                    gsf = gsfr.next()
                    for ji, (j0, jn) in enumerate(jts):
                        S.op('act', lambda e: e.copy(out=gsf.ap[0:jn, ji * 2:ji * 2 + 2], in_=psg.ap[0:jn, ji * 2:ji * 2 + 2]), [psg], [gsf])
                        S.op('dve', lambda e: e.tensor_tensor(out=gs.ap[0:jn, ji:ji + 1], in0=gsf.ap[0:jn, ji * 2:ji * 2 + 1],
                                                              in1=gsf.ap[0:jn, ji * 2 + 1:ji * 2 + 2], op=ALU.add), [gsf], [gs])
                    xg = xgr.next()
                    for dtile in range(KT):
                        ps = psring.next()
                        for i in range(2, NT):
                            S.op('pe', lambda e: e.matmul(ps.ap[:, 0:256], lhsT=h2.ap[:, i, dtile * 128:(dtile + 1) * 128], rhs=sel.ap[:, i, 0:256],
                                                          start=(i == 2), stop=(i == NT - 1)), [h2, sel], [ps])
                        if with_ctx:
                            for i in range(2):
                                S.op('pe', lambda e: e.matmul(ps.ap[:, 256:288], lhsT=h2.ap[:, i, dtile * 128:(dtile + 1) * 128], rhs=sel.ap[:, i, 0:32],
                                                              start=(i == 0), stop=(i == 1)), [h2, sel], [ps])
                        S.op('dve' if dtile % 2 else 'act',
                             (lambda e: e.tensor_copy(out=xg.ap[:, dtile, 0:NJ], in_=ps.ap[:, 0:NJ])) if dtile % 2 else
                             (lambda e: e.copy(out=xg.ap[:, dtile, 0:NJ], in_=ps.ap[:, 0:NJ])), [ps], [xg])
                    if ex + 1 < NE:
                        sel_next = build_sel(ex + 1)
                    act = actr.next()
                    for fc in range(2):
                        wg = wring.next()
                        S.op('pool', lambda e: e.dma_start(out=wg.ap[:], in_=I['moe_w_gate'][li, ex].rearrange("(kt p) f -> p kt f", p=128)[:, :, fc * 512:(fc + 1) * 512]),
                             [tIN], [wg], dma=True)
                        wu = wring.next()
                        S.op('pool', lambda e: e.dma_start(out=wu.ap[:], in_=I['moe_w_up'][li, ex].rearrange("(kt p) f -> p kt f", p=128)[:, :, fc * 512:(fc + 1) * 512]),
                             [tIN], [wu], dma=True)
                        for ji, (j0, jn) in enumerate(jts):
                            pA = psring.next()
                            pU = psring.next()
                            for kt in range(KT):
                                S.op('pe', lambda e: e.matmul(pA.ap[0:jn, :], lhsT=xg.ap[:, kt, j0:j0 + jn], rhs=wg.ap[:, kt, :],
                                                              start=(kt == 0), stop=(kt == KT - 1)), [xg, wg], [pA])
                            for kt in range(KT):
                                S.op('pe', lambda e: e.matmul(pU.ap[0:jn, :], lhsT=xg.ap[:, kt, j0:j0 + jn], rhs=wu.ap[:, kt, :],
                                                              start=(kt == 0), stop=(kt == KT - 1)), [xg, wu], [pU])
                            sa = sar.next()
                            S.op('act', lambda e: e.activation(out=sa.ap[0:jn, :], in_=pA.ap[0:jn, :], func=AF.Silu), [pA], [sa])
                            S.op('dve', lambda e: e.tensor_tensor(out=act.ap[0:jn, ji, fc * 512:(fc + 1) * 512], in0=pU.ap[0:jn, :], in1=sa.ap[0:jn, :],
                                                                  op=ALU.mult), [pU, sa], [act])
                    actT = actTr.next()
                    for ji, (j0, jn) in enumerate(jts):
                        for ft in range(8):
                            S.op('pe', lambda e: e.transpose(out=PSB.ap[:, ft * 128:ft * 128 + jn], in_=act.ap[0:jn, ji, ft * 128:(ft + 1) * 128],
                                                             identity=Csb['ident_b'].ap[0:jn, 0:jn]), [act, Csb['ident_b']], [PSB])
                        S.op('dve', lambda e: e.tensor_copy(out=actT.ap[:, :, j0:j0 + jn],
                                                            in_=PSB.ap[:].rearrange("p (a b) -> p a b", b=128)[:, :, 0:jn]), [PSB], [actT])
                    yg = ygr.next()
                    for dc in range(4):
                        wd = wring.next()
                        S.op('pool', lambda e: e.dma_start(out=wd.ap[:, 0:8, :], in_=I['moe_w_down'][li, ex].rearrange("(kt p) f -> p kt f", p=128)[:, :, dc * 512:(dc + 1) * 512]),
                             [tIN], [wd], dma=True)
                        for ji, (j0, jn) in enumerate(jts):
                            pY = psring.next()
                            for ft in range(8):
                                S.op('pe', lambda e: e.matmul(pY.ap[0:jn, :], lhsT=actT.ap[:, ft, j0:j0 + jn], rhs=wd.ap[:, ft, :],
                                                              start=(ft == 0), stop=(ft == 7)), [actT, wd], [pY])
                            if (dc + ji) % 2:
                                S.op('act', lambda e: e.mul(out=yg.ap[0:jn, ji, dc * 512:(dc + 1) * 512], in_=pY.ap[0:jn, :],
                                                            mul=gs.ap[0:jn, ji:ji + 1]), [pY, gs], [yg])
                            else:
                                S.op('dve', lambda e: e.tensor_scalar(out=yg.ap[0:jn, ji, dc * 512:(dc + 1) * 512], in0=pY.ap[0:jn, :],
                                                                      scalar1=gs.ap[0:jn, ji:ji + 1], scalar2=None, op0=ALU.mult), [pY, gs], [yg])
                    for ji, (j0, jn) in enumerate(jts):
                        S.op('sp', lambda e: e.dma_start(out=YG[ex, j0:j0 + jn, :], in_=yg.ap[0:jn, ji, :]), [yg], [tYG], dma=True)
                S.barrier()
            if stop == 'm3':
                return
            with contextlib.ExitStack() as ph:
                g_bc = [bcast_load(ph, 'g2', MOD[which, 5 * D:6 * D]) for which in (0, 1)]
                posr = Ring([sb(ph, nm('posb'), [128, NE, 512], F32) for _ in range(1)])
                selTs = [sb(ph, nm('selT'), [128, 2, 512], BF16) for _ in range(NE)]
                ygr = Ring([sb(ph, nm('ygc'), [128, NE, 2, 512], BF16) for _ in range(2)])
                xring = Ring([sb(ph, nm('xo'), [128, 512], F32) for _ in range(4)])
                tring = Ring([sb(ph, nm('to'), [128, 512], F32) for _ in range(3)])
                tchunks = [(256 + c * 512, 512, False) for c in range(4)]
                if with_ctx:
                    tchunks = [(0, 256, True)] + tchunks
                def yg_loader(t, dc, isctx):
                    if isctx:
                        S.op('sp', lambda e: e.dma_start(out=t.ap[0:32, :, 0, :], in_=YG[:, 256:288, dc * 512:(dc + 1) * 512].rearrange("e j d -> j e d")),
                             [tYG], [t], dma=True)
                    else:
                        for jt in range(2):
                            S.op('sp', lambda e: e.dma_start(out=t.ap[:, :, jt, :],
                                                             in_=YG[:, jt * 128:(jt + 1) * 128, dc * 512:(dc + 1) * 512].rearrange("e j d -> j e d")),
                                 [tYG], [t], dma=True)
                ygpf = Prefetch(ygr, [(lambda t, dc=dc, isctx=isctx: yg_loader(t, dc, isctx)) for (_t0, _n, isctx) in tchunks for dc in range(4)], 1)
                m4units = [(t0 // 128 + ti, dc) for (t0, n, isctx) in tchunks for dc in range(4) for ti in range(n // 128)]
                x4pf = Prefetch(xring, [(lambda t, i=i, dc=dc: S.op('sp', lambda e: e.dma_start(out=t.ap[:], in_=XR[i * 128:(i + 1) * 128, dc * 512:(dc + 1) * 512]),
                                                                     [tXR[i]], [t], dma=True)) for (i, dc) in m4units], 2)
                for (t0, n, isctx) in tchunks:
                    posb = posr.next()
                    S.op('sp', lambda e: e.dma_start(out=posb.ap[:, :, 0:n], in_=POS[:, t0:t0 + n].partition_broadcast(128)), [tPOS], [posb], dma=True)
                    np_ = 32 if isctx else 128
                    for ex in range(NE):
                        for jt in range(1 if isctx else 2):
                            S.op('dve',
                                 lambda e: e.tensor_scalar(out=selTs[ex].ap[0:np_, jt, 0:n], in0=posb.ap[0:np_, ex, 0:n],
                                                           scalar1=Csb['iota_part'].ap[0:np_, jt:jt + 1], scalar2=None, op0=ALU.is_equal),
                                 [posb, Csb['iota_part']], [selTs[ex]])
                    for dc in range(4):
                        ygc = ygpf.get(tchunks.index((t0, n, isctx)) * 4 + dc)
                        for ti in range(n // 128):
                            i = t0 // 128 + ti
                            which = 1 if isctx else 0
                            ps = psring.next()
                            pairs = [(ex, jt) for ex in range(NE) for jt in range(1 if isctx else 2)]
                            for idx, (ex, jt) in enumerate(pairs):
                                S.op('pe', lambda e: e.matmul(ps.ap[:], lhsT=selTs[ex].ap[0:np_, jt, ti * 128:(ti + 1) * 128], rhs=ygc.ap[0:np_, ex, jt, :],
                                                              start=(idx == 0), stop=(idx == len(pairs) - 1)), [selTs[ex], ygc], [ps])
                            xt = x4pf.get(m4units.index((i, dc)))
                            tm = tring.next()
                            S.op('dve', lambda e: e.tensor_tensor(out=tm.ap[:], in0=ps.ap[:], in1=g_bc[which].ap[:, dc * 512:(dc + 1) * 512], op=ALU.mult),
                                 [ps, g_bc[which]], [tm])
                            S.op('pool', lambda e: e.tensor_tensor(out=tm.ap[:], in0=tm.ap[:], in1=xt.ap[:], op=ALU.add), [tm, xt], [tm])
                            S.op('sp', lambda e: e.dma_start(out=XR[i * 128:(i + 1) * 128, dc * 512:(dc + 1) * 512], in_=tm.ap[:]), [tm], [tXR[i]], dma=True)
                S.barrier()

        S.barrier()
        for li in layers:
            kind = li % 3
            j = li // 3
            with_ctx = li < DEPTH - 1
            if stop == 'setup':
                break
            phase_mod(li)
            if stop == 'mod':
                break
            if debug != 'mixer_skip':
                phase_norm(li, 0)
                if stop == 'norm1':
                    break
                if kind == 0:
                    phase_attn(li, j, with_ctx)
                elif kind == 1 and 'ret' in IMPLEMENTED:
                    phase_ret(li, j, with_ctx)
                elif kind == 2 and 'hy' in IMPLEMENTED:
                    phase_hyena(li, j, with_ctx)
                else:
                    pass
            if debug != 'moe_skip':
                import os as _os
                phase_norm(li, 1, want_tm=_os.environ.get("KTM", "1") == "1", router=_os.environ.get("KRT", "1") == "1")
                if stop == 'norm2':
                    break
                phase_moe(li, with_ctx)
        phase_norm(0, 0, final=True)
        S.barrier()
    print("instructions:", S.ninst)
    return nc, consts


_W_KEYS = ['c_ctx', 'w_mod', 'b_mod', 'norm_w', 'attn_w_qkv', 'attn_q_norm', 'attn_k_norm', 'attn_w_o',
           'ret_w_in', 'ret_decay_logit', 'ret_w_o',
           'hy_w_in', 'hy_conv_w', 'hy_conv_b', 'hy_f_w1', 'hy_f_b1', 'hy_f_freq1', 'hy_f_w2', 'hy_f_b2', 'hy_f_freq2',
           'hy_f_w3', 'hy_skip', 'hy_w_out',
           'moe_router', 'moe_w_gate', 'moe_w_up', 'moe_w_down', 'final_norm_w']


def run(inputs, layers=(0, 1, 2, 3), debug=None, cores=8, stop=None, small=None):
    nc, consts = build(layers, debug, stop, small)
    in_maps = []
    shared = {k: np.ascontiguousarray(np.asarray(inputs[k], dtype=np.float32)) for k in _W_KEYS}
    if small:
        dd = small.get('depth', DEPTH)
        ne = small.get('ne', NE)
        shared['w_mod'] = np.ascontiguousarray(shared['w_mod'][:dd])
        for k in ('moe_w_gate', 'moe_w_up', 'moe_w_down'):
            shared[k] = np.ascontiguousarray(shared[k][:dd, :ne])
    for k, v in consts.items():
        shared['k_' + k] = v
    for b in range(cores):
        m = dict(shared)
        m['x'] = np.ascontiguousarray(inputs['x'][b])
        m['c'] = np.ascontiguousarray(inputs['c'][b])
        m['ctx'] = np.ascontiguousarray(inputs['ctx'][b])
        in_maps.append(m)
    res = run_bass_kernel_spmd(nc, in_maps, core_ids=list(range(cores)))
    return np.stack([r['out'] for r in res.results], axis=0)


def kernel(**inputs):
    return run(inputs).astype(np.float32)
```

```python
import contextlib
import math
import os as _os
import numpy as np
import ml_dtypes
import concourse.bass as bass
import concourse.mybir as mybir
from concourse.bass_utils import run_bass_kernel_spmd

F32 = mybir.dt.float32
BF16 = mybir.dt.bfloat16
AF = mybir.ActivationFunctionType
ALU = mybir.AluOpType
AX = mybir.AxisListType

D = 2048
TL = 2048
TC = 256
TT = TL + TC
NT = TT // 128
KT = D // 128
DEPTH = 4
EPS = 1e-6
NE = 16
CAPL = 256
CAPC = 32
FF = 1024
ENGS = ('pe', 'act', 'dve', 'pool', 'sp')
IMPLEMENTED = {'ret', 'hy'}
MOD_OVERLAP = True


class T:
    __slots__ = ('ap', 'w', 'r', 'name')

    def __init__(self, ap, name=''):
        self.ap = ap
        self.w = None
        self.r = {}
        self.name = name


class Sched:
    def __init__(self, nc, n_dma_sems=(48, 48)):
        self.nc = nc
        self.cnt = {e: 0 for e in ENGS}
        self.waited = {e: {} for e in ENGS}
        self.ndma = {'sp': n_dma_sems[0], 'pool': n_dma_sems[1]}
        self.dma_i = {'sp': 0, 'pool': 0}
        self.dma_cnt = {}
        self.sems = {}
        self.ninst = 0
        self.eng = {'pe': nc.tensor, 'act': nc.scalar, 'dve': nc.vector, 'pool': nc.gpsimd, 'sp': nc.sync}

    def alloc_sems(self, st):
        for e in ENGS:
            self.sems[e] = st.enter_context(self.nc.semaphore("s_" + e))
        for q in ('sp', 'pool'):
            for i in range(self.ndma[q]):
                self.sems[(q, i)] = st.enter_context(self.nc.semaphore(f"s_{q}{i}"))

    @staticmethod
    def _need(need, tok):
        if tok is None:
            return
        k, v = tok
        if need.get(k, 0) < v:
            need[k] = v

    def op(self, eng, fn, reads=(), writes=(), dma=False):
        need = {}
        for t in reads:
            self._need(need, t.w)
        for t in writes:
            self._need(need, t.w)
            for k, v in t.r.items():
                self._need(need, (k, v))
        if dma:
            i = self.dma_i[eng]
            self.dma_i[eng] = i + 1
            key = (eng, i % self.ndma[eng])
            prev = self.dma_cnt.get(key, 0)
            if prev:
                self._need(need, (key, prev))
            val = prev + 16
            self.dma_cnt[key] = val
            tok = (key, val)
            inc = 16
        else:
            self.cnt[eng] += 1
            key = eng
            tok = (eng, self.cnt[eng])
            inc = 1
        wd = self.waited[eng]
        eo = self.eng[eng]
        for k, v in need.items():
            if eng == 'pe' and k == 'pe':
                continue
            if wd.get(k, 0) >= v:
                continue
            wd[k] = v
            eo.wait_ge(self.sems[k], v)
        fn(eo).then_inc(self.sems[key], inc)
        self.ninst += 1
        for t in reads:
            if t.r.get(tok[0], 0) < tok[1]:
                t.r[tok[0]] = tok[1]
        for t in writes:
            t.w = tok
            t.r = {}
        return tok

    def barrier(self, engs=ENGS):
        toks = {}
        for e in ENGS:
            if e != 'sp' and self.cnt[e] > 0:
                toks[e] = self.cnt[e]
        for k, v in self.dma_cnt.items():
            toks[k] = v
        for e in engs:
            wd = self.waited[e]
            for k, v in toks.items():
                if wd.get(k, 0) >= v:
                    continue
                wd[k] = v
                self.eng[e].wait_ge(self.sems[k], v)


class Prefetch:
    def __init__(self, ring, loaders, depth):
        self.ring = ring
        self.loaders = loaders
        self.depth = depth
        self.tiles = [None] * len(loaders)
        self.nxt = 0

    def get(self, i):
        while self.nxt < len(self.loaders) and self.nxt <= i + self.depth:
            t = self.ring.next()
            self.loaders[self.nxt](t)
            self.tiles[self.nxt] = t
            self.nxt += 1
        return self.tiles[i]


class Ring:
    def __init__(self, tiles):
        self.tiles = tiles
        self.i = 0

    def next(self):
        t = self.tiles[self.i % len(self.tiles)]
        self.i += 1
        return t


def _bf(a):
    return np.ascontiguousarray(a.astype(ml_dtypes.bfloat16))


def make_consts():
    c = {}
    c['ident_f'] = np.eye(128, dtype=np.float32)
    c['ident_b'] = _bf(np.eye(128, dtype=np.float32))
    c['ones_f'] = np.ones((128, 128), np.float32)
    c['ones_b'] = _bf(np.ones((128, 128), np.float32))
    c['iota_row'] = np.tile(np.arange(256, dtype=np.float32)[None, :], (128, 1))
    ip = np.zeros((128, 4), np.float32)
    ip[:, 0] = np.arange(128)
    ip[:, 1] = np.arange(128) + 128
    c['iota_part'] = ip
    t = np.arange(TL)
    row = (t // 64).astype(np.float64)
    col = (t % 64).astype(np.float64)

    def rope_tables(hd):
        nf = hd // 4
        inv = 10000.0 ** (-np.arange(nf, dtype=np.float64) / nf)
        cosT = np.zeros((hd, TL), np.float32)
        sinT = np.zeros((hd, TL), np.float32)
        perm = np.zeros((hd, hd), np.float32)
        for d in range(hd):
            axis = d // (2 * nf)
            half = (d % (2 * nf)) // nf
            f = d % nf
            pos = row if axis == 0 else col
            ang = (pos.astype(np.float32) * np.float32(inv[f])).astype(np.float32)
            cosT[d] = np.cos(ang)
            sinT[d] = np.sin(ang) * (-1.0 if half == 0 else 1.0)
            partner = d + nf if half == 0 else d - nf
            perm[partner, d] = 1.0
        return cosT, sinT, perm
    ca, sa, pa = rope_tables(128)
    c['cosA'] = ca
    c['sinA'] = sa
    c['permA'] = pa
    cr, sr, pr = rope_tables(256)
    c['cosR0'] = np.ascontiguousarray(cr[0:128])
    c['cosR1'] = np.ascontiguousarray(cr[128:256])
    c['sinR0'] = np.ascontiguousarray(sr[0:128])
    c['sinR1'] = np.ascontiguousarray(sr[128:256])
    c['permR'] = np.ascontiguousarray(pr[0:128, 0:128])
    def hy_consts(L, tag):
        n = 2 * L
        nkt = L // 128
        tl = np.linspace(0.0, 1.0, L, dtype=np.float32)
        bands = 16
        w = (2.0 * math.pi * np.arange(L, dtype=np.float32) / L).astype(np.float32)
        f = np.linspace(1e-4, bands - 1, bands, dtype=np.float32)
        z = np.concatenate([tl[:, None], np.cos(f[None, :] * w[:, None]), -np.sin(f[None, :] * w[:, None])], axis=-1).astype(np.float32)
        c['hyZ' + tag] = np.ascontiguousarray(z.T)
        deltas = np.abs(np.linspace(math.log(1e-2) / 1.5, math.log(1e-2) / 0.3, D, dtype=np.float32))
        c['hyDecay' + tag] = np.exp(-tl[:, None] * deltas[None, :]).astype(np.float32)
        k = np.arange(L, dtype=np.int64)
        ff = np.arange(L, dtype=np.int64)
        m = ((2 * ff[None, :] + 1) * k[:, None]) % (2 * n)
        ang = m.astype(np.float64) * (math.pi / n)
        CT = np.cos(ang)
        ST = np.sin(ang)
        tcw = min(512, L)
        ntc = L // tcw

        def tile_T(M):
            return _bf(M.reshape(nkt, 128, nkt, 128).transpose(2, 1, 0, 3).reshape(nkt, 128, nkt * 128))

        def tile_F(M):
            return _bf(M.reshape(ntc, tcw, nkt, 128).transpose(0, 3, 2, 1).reshape(ntc, 128, nkt * tcw))
        c['hyCT' + tag] = tile_T(CT)
        c['hyST' + tag] = tile_T(ST)
        c['hyCF' + tag] = tile_F(CT)
        c['hySF' + tag] = tile_F(ST)
    hy_consts(TL, 'L')
    hy_consts(TC, 'C')
    p_ = np.arange(128, dtype=np.float32)[:, None]
    c['retDelta'] = np.ascontiguousarray(np.arange(3968, dtype=np.float32)[None, :] - p_ - 1920.0)
    y_ = np.arange(2176, dtype=np.float32)[None, :]
    c['retEf'] = np.ascontiguousarray(y_ - p_ + 128.0)
    c['retEb'] = np.ascontiguousarray(p_ - y_ + 2176.0)
    return c


class KB:
    pass


def build(layers=(0, 1, 2, 3), debug=None, stop=None, small=None):
    nc = bass.Bass("TRN2", target_bir_lowering=False)
    S = Sched(nc)
    consts = make_consts()

    def din(name, shape, dt=F32):
        return nc.dram_tensor(name, list(shape), dt, kind="ExternalInput").ap()

    def dscr(name, shape, dt):
        return nc.dram_tensor(name, list(shape), dt, kind="Internal").ap()

    I = {}
    I['x'] = din('x', [TL, D])
    I['c'] = din('c', [D])
    I['ctx'] = din('ctx', [TC, D])
    I['c_ctx'] = din('c_ctx', [D])
    small = small or {}
    _DD = small.get('depth', DEPTH)
    _NEW = small.get('ne', NE)
    I['w_mod'] = din('w_mod', [_DD, D, 6 * D])
    I['b_mod'] = din('b_mod', [DEPTH, 6 * D])
    I['norm_w'] = din('norm_w', [DEPTH, 2, D])
    I['attn_w_qkv'] = din('attn_w_qkv', [2, D, 3072])
    I['attn_q_norm'] = din('attn_q_norm', [2, 128])
    I['attn_k_norm'] = din('attn_k_norm', [2, 128])
    I['attn_w_o'] = din('attn_w_o', [2, D, D])
    I['ret_w_in'] = din('ret_w_in', [1, D, 12288])
    I['ret_decay_logit'] = din('ret_decay_logit', [1, 2, 8])
    I['ret_w_o'] = din('ret_w_o', [1, 4096, D])
    I['hy_w_in'] = din('hy_w_in', [1, D, 3 * D])
    I['hy_conv_w'] = din('hy_conv_w', [1, 3, 3 * D])
    I['hy_conv_b'] = din('hy_conv_b', [1, 3 * D])
    I['hy_f_w1'] = din('hy_f_w1', [1, 33, 64])
    I['hy_f_b1'] = din('hy_f_b1', [1, 64])
    I['hy_f_freq1'] = din('hy_f_freq1', [1, 64])
    I['hy_f_w2'] = din('hy_f_w2', [1, 64, 64])
    I['hy_f_b2'] = din('hy_f_b2', [1, 64])
    I['hy_f_freq2'] = din('hy_f_freq2', [1, 64])
    I['hy_f_w3'] = din('hy_f_w3', [1, 64, 2 * D])
    I['hy_skip'] = din('hy_skip', [1, D])
    I['hy_w_out'] = din('hy_w_out', [1, D, D])
    I['moe_router'] = din('moe_router', [DEPTH, D, NE])
    I['moe_w_gate'] = din('moe_w_gate', [_DD, _NEW, D, FF])
    I['moe_w_up'] = din('moe_w_up', [_DD, _NEW, D, FF])
    I['moe_w_down'] = din('moe_w_down', [_DD, _NEW, FF, D])
    I['final_norm_w'] = din('final_norm_w', [D])
    CI = {}
    for k, v in consts.items():
        CI[k] = din('k_' + k, v.shape, BF16 if v.dtype == ml_dtypes.bfloat16 else F32)
    OUT = nc.dram_tensor('out', [TL, D], F32, kind="ExternalOutput").ap()

    XR = dscr('XR', [TT, D], F32)
    HT = dscr('HT', [KT, 128, TT], BF16)
    H2TM = dscr('H2TM', [TT, D], BF16)
    MODS = [dscr('MOD0', [2, 6 * D], F32), dscr('MOD1', [2, 6 * D], F32)]
    QT = dscr('QT', [16, 128, TT], BF16)
    KTs = dscr('KTs', [4, 128, TT], BF16)
    Vs = dscr('Vs', [TT, 512], BF16)
    OT = dscr('OT', [32, 128, TT], BF16)
    RK = dscr('RK', [16, 128, TT], BF16)
    RG = dscr('RG', [32, 128, TT], BF16)
    RV = dscr('RV', [TT, 4096], BF16)
    UTM = dscr('UTM', [TT, D], BF16)
    UTF = dscr('UTF', [KT, 128, TT], F32)
    X0F = dscr('X0F', [KT, 128, TT], F32)
    HPM = dscr('HPM', [2, TL, D], BF16)
    HSPEC = {'L': dscr('HSPECL', [2, TL, D], F32), 'C': dscr('HSPECC', [2, TC, D], F32)}
    SKIPT = dscr('SKIPT', [128, KT], F32)
    POS = dscr('POS', [NE, TT], F32)
    YG = dscr('YG', [NE, 288, D], BF16)

    dT = {}

    def dt_(name, ap):
        dT[name] = T(ap, name)
        return dT[name]
    tXR = [dt_(f'XR{i}', XR) for i in range(NT)]
    tHT = dt_('HT', HT)
    tH2 = dt_('H2TM', H2TM)
    tMODS = [dt_('MOD0', MODS[0]), dt_('MOD1', MODS[1])]
    tQT = dt_('QT', QT)
    tKT = dt_('KTs', KTs)
    tV = dt_('Vs', Vs)
    tOT = dt_('OT', OT)
    tRK = dt_('RK', RK)
    tRG = dt_('RG', RG)
    tRV = dt_('RV', RV)
    tUTM = dt_('UTM', UTM)
    tUTF = dt_('UTF', UTF)
    tX0F = dt_('X0F', X0F)
    tHPM = dt_('HPM', HPM)
    tHSPEC = {'L': dt_('HSPECL', HSPEC['L']), 'C': dt_('HSPECC', HSPEC['C'])}
    tSKIPT = dt_('SKIPT', SKIPT)
    tPOS = dt_('POS', POS)
    tYG = dt_('YG', YG)
    tIN = T(None, 'inputs')
    tOUT = dt_('OUT', OUT)

    with contextlib.ExitStack() as top:
        S.alloc_sems(top)

        def sb(stk, name, shape, dt):
            return T(top_or(stk).enter_context(nc.sbuf_tensor(name, list(shape), dt)), name)

        def top_or(stk):
            return stk if stk is not None else top

        uid = [0]

        def nm(p):
            uid[0] += 1
            return f"{p}_{uid[0]}"

        PS = [T(top.enter_context(nc.psum_tensor(f"ps{i}", [128, 512], F32)), f"ps{i}") for i in range(7)]
        PSB = T(top.enter_context(nc.psum_tensor("psb", [128, 1024], BF16)), "psb")
        psring = Ring(PS)

        Csb = {}
        for k in ('ident_f', 'ident_b', 'ones_f', 'ones_b', 'iota_row', 'iota_part', 'permA', 'permR'):
            v = consts[k]
            Csb[k] = sb(None, 'c_' + k, v.shape, BF16 if v.dtype == ml_dtypes.bfloat16 else F32)
            S.op('sp', lambda e, k=k: e.dma_start(out=Csb[k].ap[:], in_=CI[k]), [tIN], [Csb[k]], dma=True)
        ones_col = sb(None, 'ones_col', [128, 1], F32)
        S.op('dve', lambda e: e.memset(ones_col.ap[:], 1.0), [], [ones_col])
        craw = sb(None, 'craw', [128, KT, 2], F32)
        sT = sb(None, 'sT', [128, KT, 2], BF16)
        S.op('sp', lambda e: e.dma_start(out=craw.ap[:, :, 0], in_=I['c'].rearrange("(kt p) -> p kt", p=128),
                                         allow_slow_non_contiguous=True), [tIN], [craw], dma=True)
        S.op('sp', lambda e: e.dma_start(out=craw.ap[:, :, 1], in_=I['c_ctx'].rearrange("(kt p) -> p kt", p=128),
                                         allow_slow_non_contiguous=True), [tIN], [craw], dma=True)
        S.op('act', lambda e: e.activation(out=sT.ap[:], in_=craw.ap[:], func=AF.Silu), [craw], [sT])
        probs_tm = sb(None, 'probs_tm', [128, NT, NE], F32)
        probsT = sb(None, 'probsT', [NE, TT], F32)
        posTM = sb(None, 'posTM', [128, NT, NE], F32)
        GHL = sb(None, 'GHL', [128, NT, NE, 2], BF16)

        for i in range(NT):
            src = I['ctx'][i * 128:(i + 1) * 128, :] if i < 2 else I['x'][(i - 2) * 128:(i - 1) * 128, :]
            S.op('sp', lambda e: e.dma_start(out=XR[i * 128:(i + 1) * 128, :], in_=src), [tIN], [tXR[i]], dma=True)

        def bcast_load(stk, name, vec_ap):
            n = vec_ap.shape[-1]
            t = sb(stk, nm(name), [128, n], F32)
            S.op('sp', lambda e: e.dma_start(out=t.ap[:], in_=vec_ap.partition_broadcast(128)), [tMODS[0], tMODS[1], tIN], [t], dma=True)
            return t

        mod_done = set()

        def phase_mod(li, stk=None):
            mod_done.add(li)
            MOD = MODS[li % 2]
            tM = tMODS[li % 2]
            own = stk is None
            ph = contextlib.ExitStack() if own else stk
            wring = Ring([sb(ph, nm('wm'), [128, KT, 512], BF16) for _ in range(3)])
            bring = Ring([sb(ph, nm('bm'), [1, 512], F32) for _ in range(3)])
            oring = Ring([sb(ph, nm('om'), [2, 512], F32) for _ in range(3)])
            wsrc = I['w_mod'][li].rearrange("(kt p) n -> p kt n", p=128)
            wpf = Prefetch(wring, [(lambda t, cch=cch: S.op('pool', lambda e: e.dma_start(out=t.ap[:], in_=wsrc[:, :, cch * 512:(cch + 1) * 512]),
                                                             [tIN], [t], dma=True)) for cch in range(24)], 2)
            for cch in range(24):
                w = wpf.get(cch)
                br = bring.next()
                S.op('pool', lambda e: e.dma_start(out=br.ap[:], in_=I['b_mod'][li, cch * 512:(cch + 1) * 512].rearrange("(o n) -> o n", o=1)),
                     [tIN], [br], dma=True)
                ps = psring.next()
                S.op('pe', lambda e: e.matmul(ps.ap[:, :], lhsT=Csb['ones_f'].ap[0:1, :], rhs=br.ap[0:1, :], start=True, stop=False),
                     [Csb['ones_f'], br], [ps])
                for kt in range(KT):
                    S.op('pe', lambda e: e.matmul(ps.ap[0:2, :], lhsT=sT.ap[:, kt, :], rhs=w.ap[:, kt, :],
                                                  start=False, stop=(kt == KT - 1)), [sT, w], [ps])
                om = oring.next()
                S.op('act', lambda e: e.copy(out=om.ap[:], in_=ps.ap[0:2, :]), [ps], [om])
                S.op('pool', lambda e: e.dma_start(out=MOD[:, cch * 512:(cch + 1) * 512], in_=om.ap[:]), [om], [tM], dma=True)
            if own:
                S.barrier()
                ph.close()

        def phase_norm(li, k, want_tm=False, router=False, final=False):
            MOD = MODS[li % 2]
            with contextlib.ExitStack() as ph:
                if final:
                    nw = bcast_load(ph, 'nw', I['final_norm_w'])
                    A_bc = [nw, nw]
                    B_bc = [None, None]
                else:
                    nw = bcast_load(ph, 'nw', I['norm_w'][li, k])
                    A_bc, B_bc = [], []
                    for which in (0, 1):
                        sc = bcast_load(ph, 'sc', MOD[which, (3 * k + 1) * D:(3 * k + 2) * D])
                        S.op('dve', lambda e: e.scalar_tensor_tensor(out=sc.ap[:], in0=sc.ap[:], scalar=1.0, in1=nw.ap[:],
                                                                     op0=ALU.add, op1=ALU.mult), [sc, nw], [sc])
                        A_bc.append(sc)
                        B_bc.append(bcast_load(ph, 'sh', MOD[which, (3 * k) * D:(3 * k + 1) * D]))
                if router:
                    wr = sb(ph, nm('wr'), [128, KT, NE], F32)
                    for kt in range(KT):
                        S.op('sp', lambda e: e.dma_start(out=wr.ap[:, kt, :], in_=I['moe_router'][li, kt * 128:(kt + 1) * 128, :]),
                             [tIN], [wr], dma=True)
                ptile = sb(ph, nm('ptile'), [128, 128], F32)
                S.op('dve', lambda e: e.memset(ptile.ap[:], 0.0), [], [ptile])
                xring = Ring([sb(ph, nm('xt'), [128, D], F32) for _ in range(2)])
                hring = Ring([sb(ph, nm('h'), [128, D], F32) for _ in range(2)])
                hbring = Ring([sb(ph, nm('hb'), [128, D], BF16) for _ in range(2)])
                junk = sb(ph, nm('junk'), [128, D], BF16)
                htring = Ring([sb(ph, nm('htb'), [128, 4, 128], BF16) for _ in range(4)])
                hfring = Ring([sb(ph, nm('htf'), [128, KT, 128], F32) for _ in range(2)])
                stat = Ring([sb(ph, nm('st'), [128, 4], F32) for _ in range(3)])
                tiles = range(2, NT) if final else range(NT)
                for i in tiles:
                    which = 1 if i < 2 else 0
                    xt = xring.next()
                    S.op('sp', lambda e: e.dma_start(out=xt.ap[:], in_=XR[i * 128:(i + 1) * 128, :]), [tXR[i]], [xt], dma=True)
                    s4 = stat.next()
                    S.op('act', lambda e: e.activation(out=junk.ap[:], in_=xt.ap[:], func=AF.Square, accum_out=s4.ap[:, 0:1]),
                         [xt], [junk, s4])
                    S.op('dve', lambda e: e.tensor_scalar(out=s4.ap[:, 1:2], in0=s4.ap[:, 0:1], scalar1=1.0 / D, scalar2=EPS,
                                                          op0=ALU.mult, op1=ALU.add), [s4], [s4])
                    S.op('act', lambda e: e.activation(out=s4.ap[:, 2:3], in_=s4.ap[:, 1:2], func=AF.Sqrt), [s4], [s4])
                    S.op('dve', lambda e: e.reciprocal(out=s4.ap[:, 3:4], in_=s4.ap[:, 2:3]), [s4], [s4])
                    h = hring.next()
                    S.op('dve', lambda e: e.scalar_tensor_tensor(out=h.ap[:], in0=xt.ap[:], scalar=s4.ap[:, 3:4], in1=A_bc[which].ap[:],
                                                                 op0=ALU.mult, op1=ALU.mult), [xt, s4, A_bc[which]], [h])
                    if final:
                        S.op('sp', lambda e: e.dma_start(out=OUT[(i - 2) * 128:(i - 1) * 128, :], in_=h.ap[:]), [h], [tOUT], dma=True)
                        continue
                    S.op('pool', lambda e: e.tensor_tensor(out=h.ap[:], in0=h.ap[:], in1=B_bc[which].ap[:], op=ALU.add),
                         [h, B_bc[which]], [h])
                    if want_tm:
                        hb = hbring.next()
                        S.op('act', lambda e: e.copy(out=hb.ap[:], in_=h.ap[:]), [h], [hb])
                        S.op('sp', lambda e: e.dma_start(out=H2TM[i * 128:(i + 1) * 128, :], in_=hb.ap[:]), [hb], [tH2], dma=True)
                    hf = hfring.next() if router else None
                    for g in range(4):
                        ps = psring.next()
                        for j in range(4):
                            kt = g * 4 + j
                            S.op('pe', lambda e: e.transpose(out=ps.ap[:, j * 128:(j + 1) * 128], in_=h.ap[:, kt * 128:(kt + 1) * 128],
                                                             identity=Csb['ident_f'].ap[:]), [h, Csb['ident_f']], [ps])
                        hb4 = htring.next()
                        if router:
                            S.op('act', lambda e: e.copy(out=hf.ap[:, g * 4:(g + 1) * 4, :], in_=ps.ap[:].rearrange("p (a b) -> p a b", b=128)),
                                 [ps], [hf])
                            S.op('dve', lambda e: e.tensor_copy(out=hb4.ap[:], in_=hf.ap[:, g * 4:(g + 1) * 4, :]), [hf], [hb4])
                        else:
                            S.op('dve', lambda e: e.tensor_copy(out=hb4.ap[:].rearrange("p a b -> p (a b)"), in_=ps.ap[:]), [ps], [hb4])
                        S.op('sp', lambda e: e.dma_start(out=HT[g * 4:(g + 1) * 4, :, i * 128:(i + 1) * 128].rearrange("k p t -> p k t"),
                                                         in_=hb4.ap[:]), [hb4], [tHT], dma=True)
                    if router and _os.environ.get("KRT3", "0") != "1":
                        ps = psring.next()
                        for kt in range(KT):
                            S.op('pe', lambda e: e.matmul(ps.ap[:, 0:NE], lhsT=hf.ap[:, kt, :], rhs=wr.ap[:, kt, :],
                                                          start=(kt == 0), stop=(kt == KT - 1)), [hf, wr], [ps])
                        s5 = stat.next()
                        S.op('dve', lambda e: e.reduce_max(out=s5.ap[:, 0:1], in_=ps.ap[:, 0:NE], axis=AX.X), [ps], [s5])
                        S.op('dve', lambda e: e.tensor_scalar(out=s5.ap[:, 1:2], in0=s5.ap[:, 0:1], scalar1=-1.0, scalar2=None,
                                                              op0=ALU.mult), [s5], [s5])
                        S.op('act', lambda e: e.activation(out=probs_tm.ap[:, i, :], in_=ps.ap[:, 0:NE], func=AF.Exp,
                                                           bias=s5.ap[:, 1:2], scale=1.0, accum_out=s5.ap[:, 2:3]),
                             [ps, s5], [probs_tm, s5])
                        S.op('dve', lambda e: e.reciprocal(out=s5.ap[:, 3:4], in_=s5.ap[:, 2:3]), [s5], [s5])
                        S.op('dve', lambda e: e.tensor_scalar(out=probs_tm.ap[:, i, :], in0=probs_tm.ap[:, i, :], scalar1=s5.ap[:, 3:4],
                                                              scalar2=None, op0=ALU.mult), [probs_tm, s5], [probs_tm])
                        ps2 = psring.next()
                        if _os.environ.get("KRT2", "0") == "1":
                            continue
                        S.op('dve', lambda e: e.tensor_copy(out=ptile.ap[:, 0:NE], in_=probs_tm.ap[:, i, :]), [probs_tm], [ptile])
                        S.op('pe', lambda e: e.transpose(out=ps2.ap[:, 0:128], in_=ptile.ap[:], identity=Csb['ident_f'].ap[:]),
                             [ptile, Csb['ident_f']], [ps2])
                        S.op('act', lambda e: e.copy(out=probsT.ap[:, i * 128:(i + 1) * 128], in_=ps2.ap[0:NE, 0:128]), [ps2], [probsT])
                S.barrier()

        def load_fm(stk, name, src, tsrc, nkt, t0, n):
            t = sb(stk, nm(name), [128, nkt, n], BF16)
            for kt in range(nkt):
                S.op('sp', lambda e: e.dma_start(out=t.ap[:, kt, :], in_=src[kt, :, t0:t0 + n]), [tsrc], [t], dma=True)
            return t

        def phase_proj_residual(li, src, tsrc, nkt, W, gate_k, with_ctx):
            MOD = MODS[li % 2]
            wsrc = W.rearrange("(kt p) n -> p kt n", p=128)
            ngrp = 1 if nkt <= 16 else 2
            tile_lo = 0 if with_ctx else 2
            per = (NT - tile_lo + ngrp - 1) // ngrp
            for gi in range(ngrp):
                tl = list(range(tile_lo + gi * per, min(NT, tile_lo + (gi + 1) * per)))
                with contextlib.ExitStack() as ph:
                    g_bc = [bcast_load(ph, 'g', MOD[which, (3 * gate_k + 2) * D:(3 * gate_k + 3) * D]) for which in (0, 1)]
                    t0 = tl[0] * 128
                    a = load_fm(ph, 'a', src, tsrc, nkt, t0, len(tl) * 128)
                    nwb = 3 if nkt <= 16 else 2
                    wring = Ring([sb(ph, nm('wo'), [128, nkt, 512], BF16) for _ in range(nwb)])
                    xring = Ring([sb(ph, nm('xo'), [128, 512], F32) for _ in range(4)])
                    tring = Ring([sb(ph, nm('to'), [128, 512], F32) for _ in range(3)])
                    wpf = Prefetch(wring, [(lambda t, cch=cch: S.op('pool', lambda e: e.dma_start(out=t.ap[:], in_=wsrc[:, :, cch * 512:(cch + 1) * 512]),
                                                                     [tIN], [t], dma=True)) for cch in range(4)], nwb - 1)
                    units = [(cch, i) for cch in range(4) for i in tl]
                    xpf = Prefetch(xring, [(lambda t, cch=cch, i=i: S.op('sp', lambda e: e.dma_start(out=t.ap[:], in_=XR[i * 128:(i + 1) * 128, cch * 512:(cch + 1) * 512]),
                                                                         [tXR[i]], [t], dma=True)) for (cch, i) in units], 2)
                    for cch in range(4):
                        w = wpf.get(cch)
                        for i in tl:
                            which = 1 if i < 2 else 0
                            ps = psring.next()
                            c0 = i * 128 - t0
                            for kt in range(nkt):
                                S.op('pe', lambda e: e.matmul(ps.ap[:], lhsT=a.ap[:, kt, c0:c0 + 128], rhs=w.ap[:, kt, :],
                                                              start=(kt == 0), stop=(kt == nkt - 1)), [a, w], [ps])
                            xt = xpf.get(units.index((cch, i)))
                            tm = tring.next()
                            S.op('dve', lambda e: e.tensor_tensor(out=tm.ap[:], in0=ps.ap[:], in1=g_bc[which].ap[:, cch * 512:(cch + 1) * 512],
                                                                  op=ALU.mult), [ps, g_bc[which]], [tm])
                            S.op('pool', lambda e: e.tensor_tensor(out=tm.ap[:], in0=tm.ap[:], in1=xt.ap[:], op=ALU.add), [tm, xt], [tm])
                            S.op('sp', lambda e: e.dma_start(out=XR[i * 128:(i + 1) * 128, cch * 512:(cch + 1) * 512], in_=tm.ap[:]),
                                 [tm], [tXR[i]], dma=True)
                    S.barrier()

        def phase_attn(li, j, with_ctx):
            Wqkv = I['attn_w_qkv'][j]
            wsrc = Wqkv.rearrange("(kt p) n -> p kt n", p=128)
            with contextlib.ExitStack() as ph:
                hT = load_fm(ph, 'hT', HT, tHT, KT, 0, TT)
                cosA = sb(ph, nm('cosA'), [128, TL], F32)
                sinA = sb(ph, nm('sinA'), [128, TL], F32)
                S.op('sp', lambda e: e.dma_start(out=cosA.ap[:], in_=CI['cosA']), [tIN], [cosA], dma=True)
                S.op('sp', lambda e: e.dma_start(out=sinA.ap[:], in_=CI['sinA']), [tIN], [sinA], dma=True)
                nq = sb(ph, nm('nq'), [128, 2], F32)
                S.op('sp', lambda e: e.dma_start(out=nq.ap[:, 0:1], in_=I['attn_q_norm'][j].rearrange("(p o) -> p o", o=1)), [tIN], [nq], dma=True)
                S.op('sp', lambda e: e.dma_start(out=nq.ap[:, 1:2], in_=I['attn_k_norm'][j].rearrange("(p o) -> p o", o=1)), [tIN], [nq], dma=True)
                wring = Ring([sb(ph, nm('wq'), [128, KT, 128], BF16) for _ in range(3)])
                sqr = Ring([sb(ph, nm('sq'), [128, 512], F32) for _ in range(3)])
                rsr = Ring([sb(ph, nm('rs'), [128, 512], F32) for _ in range(3)])
                qnr = Ring([sb(ph, nm('qn'), [128, 512], F32) for _ in range(4)])
                epsc = sb(ph, nm('epsc'), [128, 1], F32)
                S.op('dve', lambda e: e.memset(epsc.ap[:], EPS), [], [epsc])
                t1r = Ring([sb(ph, nm('t1'), [128, 512], F32) for _ in range(2)])
                t2r = Ring([sb(ph, nm('t2'), [128, 512], F32) for _ in range(2)])
                obr = Ring([sb(ph, nm('ob'), [128, 512], BF16) for _ in range(3)])
                chunks = [(0, 256, False)] + [(256 + c * 512, 512, True) for c in range(4)]
                q1, q2 = [], []

                def a2_tail1(psA, sq, n, col, lat, t0, cb):
                    psB = psring.next()
                    S.op('pe', lambda e: e.matmul(psB.ap[:, 0:n], lhsT=Csb['ones_f'].ap[:], rhs=sq.ap[:, 0:n], start=True, stop=True),
                         [sq, Csb['ones_f']], [psB])
                    rs = rsr.next()
                    S.op('act', lambda e: e.activation(out=rs.ap[:, 0:n], in_=psB.ap[:, 0:n], func=AF.Sqrt, bias=epsc.ap[:, 0:1], scale=1.0 / 128),
                         [psB, epsc], [rs])
                    S.op('dve', lambda e: e.reciprocal(out=rs.ap[:, 0:n], in_=rs.ap[:, 0:n]), [rs], [rs])
                    qn = qnr.next()
                    S.op('dve', lambda e: e.scalar_tensor_tensor(out=qn.ap[:, 0:n], in0=psA.ap[:, 0:n], scalar=nq.ap[:, col:col + 1],
                                                                 in1=rs.ap[:, 0:n], op0=ALU.mult, op1=ALU.mult), [psA, nq, rs], [qn])
                    return qn

                def a2_tail2(qn, n, lat, t0, cb):
                    ob = obr.next()
                    if lat:
                        psC = psring.next()
                        S.op('pe', lambda e: e.matmul(psC.ap[:, 0:n], lhsT=Csb['permA'].ap[:], rhs=qn.ap[:, 0:n], start=True, stop=True),
                             [qn, Csb['permA']], [psC])
                        l0 = t0 - TC
                        t1 = t1r.next()
                        t2 = t2r.next()
                        S.op('pool', lambda e: e.tensor_tensor(out=t1.ap[:, 0:n], in0=qn.ap[:, 0:n], in1=cosA.ap[:, l0:l0 + n], op=ALU.mult),
                             [qn, cosA], [t1])
                        S.op('dve', lambda e: e.tensor_tensor(out=t2.ap[:, 0:n], in0=psC.ap[:, 0:n], in1=sinA.ap[:, l0:l0 + n], op=ALU.mult),
                             [psC, sinA], [t2])
                        S.op('pool', lambda e: e.tensor_tensor(out=ob.ap[:, 0:n], in0=t1.ap[:, 0:n], in1=t2.ap[:, 0:n], op=ALU.add),
                             [t1, t2], [ob])
                    else:
                        S.op('act', lambda e: e.copy(out=ob.ap[:, 0:n], in_=qn.ap[:, 0:n]), [qn], [ob])
                    if cb < 16:
                        S.op('sp', lambda e: e.dma_start(out=QT[cb, :, t0:t0 + n], in_=ob.ap[:, 0:n]), [ob], [tQT], dma=True)
                    else:
                        S.op('sp', lambda e: e.dma_start(out=KTs[cb - 16, :, t0:t0 + n], in_=ob.ap[:, 0:n]), [ob], [tKT], dma=True)

                def a2_advance(flush=False):
                    while len(q1) > (0 if flush else 1):
                        (psA, sq, n, col, lat, t0, cb) = q1.pop(0)
                        qn = a2_tail1(psA, sq, n, col, lat, t0, cb)
                        q2.append((qn, n, lat, t0, cb))
                    while len(q2) > (0 if flush else 1):
                        a2_tail2(*q2.pop(0))

                wpf = Prefetch(wring, [(lambda t, cb=cb: S.op('pool', lambda e: e.dma_start(out=t.ap[:], in_=wsrc[:, :, cb * 128:(cb + 1) * 128]),
                                                                 [tIN], [t], dma=True)) for cb in range(20)], 2)
                for cb in range(20):
                    is_q = cb < 16
                    w = wpf.get(cb)
                    for (t0, n, lat) in chunks:
                        if is_q and (not lat) and (not with_ctx):
                            continue
                        psA = psring.next()
                        for kt in range(KT):
                            S.op('pe', lambda e: e.matmul(psA.ap[:, 0:n], lhsT=w.ap[:, kt, :], rhs=hT.ap[:, kt, t0:t0 + n],
                                                          start=(kt == 0), stop=(kt == KT - 1)), [w, hT], [psA])
                        sq = sqr.next()
                        S.op('act', lambda e: e.activation(out=sq.ap[:, 0:n], in_=psA.ap[:, 0:n], func=AF.Square), [psA], [sq])
                        q1.append((psA, sq, n, 0 if is_q else 1, lat, t0, cb))
                        a2_advance()
                a2_advance(flush=True)
                wv = sb(ph, nm('wv'), [128, KT, 512], BF16)
                S.op('pool', lambda e: e.dma_start(out=wv.ap[:], in_=wsrc[:, :, 2560:3072]), [tIN], [wv], dma=True)
                for i in range(NT):
                    ps = psring.next()
                    for kt in range(KT):
                        S.op('pe', lambda e: e.matmul(ps.ap[:], lhsT=hT.ap[:, kt, i * 128:(i + 1) * 128], rhs=wv.ap[:, kt, :],
                                                      start=(kt == 0), stop=(kt == KT - 1)), [hT, wv], [ps])
                    ob = obr.next()
                    S.op('act', lambda e: e.copy(out=ob.ap[:], in_=ps.ap[:]), [ps], [ob])
                    S.op('sp', lambda e: e.dma_start(out=Vs[i * 128:(i + 1) * 128, :], in_=ob.ap[:]), [ob], [tV], dma=True)
                S.barrier()
            with contextlib.ExitStack() as ph:
                scale = 128 ** -0.5
                ktr = Ring([sb(ph, nm('kt'), [128, TT], BF16) for _ in range(2)])
                vr = Ring([sb(ph, nm('v'), [128, NT, 128], BF16) for _ in range(2)])
                qr = Ring([sb(ph, nm('q'), [128, TT], BF16) for _ in range(2)])
                er = Ring([sb(ph, nm('e'), [128, 512], BF16) for _ in range(5)])
                rdr = Ring([sb(ph, nm('rd'), [128, 512], F32) for _ in range(2)])
                oor = Ring([sb(ph, nm('oo'), [128, 512], BF16) for _ in range(2)])
                psS = Ring(PS[0:3])
                psO = Ring(PS[3:5])
                psD = Ring(PS[5:7])
                LOOK = 2
                pend = []

                def emit_pv(ee, n, jt, pO, pD, idx, nk, v, t0, h_):
                    S.op('pe', lambda e: e.matmul(pO.ap[:, 0:n], lhsT=v.ap[:, jt, :], rhs=ee.ap[:, 0:n],
                                                  start=(idx == 0), stop=(idx == nk - 1)), [v, ee], [pO])
                    S.op('pe', lambda e: e.matmul(pD.ap[:, 0:n], lhsT=Csb['ones_b'].ap[:], rhs=ee.ap[:, 0:n],
                                                  start=(idx == 0), stop=(idx == nk - 1)), [Csb['ones_b'], ee], [pD])
                    if idx == nk - 1:
                        rd = rdr.next()
                        S.op('dve', lambda e: e.reciprocal(out=rd.ap[:, 0:n], in_=pD.ap[:, 0:n]), [pD], [rd])
                        oo = oor.next()
                        S.op('dve', lambda e: e.tensor_tensor(out=oo.ap[:, 0:n], in0=pO.ap[:, 0:n], in1=rd.ap[:, 0:n], op=ALU.mult),
                             [pO, rd], [oo])
                        S.op('sp', lambda e: e.dma_start(out=OT[h_, :, t0:t0 + n], in_=oo.ap[:, 0:n]), [oo], [tOT], dma=True)

                for kv in range(4):
                    kt_ = ktr.next()
                    S.op('sp', lambda e: e.dma_start(out=kt_.ap[:], in_=KTs[kv]), [tKT], [kt_], dma=True)
                    v = vr.next()
                    for i in range(NT):
                        S.op('sp', lambda e: e.dma_start(out=v.ap[:, i, :], in_=Vs[i * 128:(i + 1) * 128, kv * 128:(kv + 1) * 128]),
                             [tV], [v], dma=True)
                    for hh in range(4):
                        h_ = kv * 4 + hh
                        q = qr.next()
                        S.op('sp', lambda e: e.dma_start(out=q.ap[:], in_=QT[h_]), [tQT], [q], dma=True)
                        qchunks = [(256 + c * 512, 512, list(range(NT))) for c in range(4)]
                        if with_ctx:
                            qchunks = [(0, 256, [0, 1])] + qchunks
                        for (t0, n, ktiles) in qchunks:
                            pO = psO.next()
                            pD = psD.next()
                            for idx, jt in enumerate(ktiles):
                                pS = psS.next()
                                S.op('pe', lambda e: e.matmul(pS.ap[:, 0:n], lhsT=kt_.ap[:, jt * 128:(jt + 1) * 128], rhs=q.ap[:, t0:t0 + n],
                                                              start=True, stop=True), [kt_, q], [pS])
                                ee = er.next()
                                S.op('act', lambda e: e.activation(out=ee.ap[:, 0:n], in_=pS.ap[:, 0:n], func=AF.Exp, scale=scale), [pS], [ee])
                                pend.append((ee, n, jt, pO, pD, idx, len(ktiles), v, t0, h_))
                                if len(pend) > LOOK:
                                    emit_pv(*pend.pop(0))
                while pend:
                    emit_pv(*pend.pop(0))
                S.barrier()
            phase_proj_residual(li, OT, tOT, 16, I['attn_w_o'][j], 0, with_ctx)


        def phase_ret(li, j, with_ctx):
            Win = I['ret_w_in'][j]
            wsrc = Win.rearrange("(kt p) n -> p kt n", p=128)
            chunks = [(0, 256, False)] + [(256 + c * 512, 512, True) for c in range(4)]
            with contextlib.ExitStack() as ph:
                hT = load_fm(ph, 'hT', HT, tHT, KT, 0, TT)
                cs = {}
                for k in ('cosR0', 'sinR0', 'cosR1', 'sinR1'):
                    cs[k] = sb(ph, nm(k), [128, TL], F32)
                    S.op('sp', lambda e: e.dma_start(out=cs[k].ap[:], in_=CI[k]), [tIN], [cs[k]], dma=True)
                wring = Ring([sb(ph, nm('wq'), [128, KT, 128], BF16) for _ in range(3)])
                qnr = Ring([sb(ph, nm('qn'), [128, 512], F32) for _ in range(2)])
                t1r = Ring([sb(ph, nm('t1'), [128, 512], F32) for _ in range(2)])
                t2r = Ring([sb(ph, nm('t2'), [128, 512], F32) for _ in range(2)])
                obr = Ring([sb(ph, nm('ob'), [128, 512], BF16) for _ in range(3)])
                rq = []

                def r2_store(ob, n, t0, cb):
                    if cb < 16:
                        S.op('sp', lambda e: e.dma_start(out=QT[cb, :, t0:t0 + n], in_=ob.ap[:, 0:n]), [ob], [tQT], dma=True)
                    else:
                        S.op('sp', lambda e: e.dma_start(out=RK[cb - 16, :, t0:t0 + n], in_=ob.ap[:, 0:n]), [ob], [tRK], dma=True)

                def r2_tail(qn, n, t0, a, cb):
                    ob = obr.next()
                    psC = psring.next()
                    S.op('pe', lambda e: e.matmul(psC.ap[:, 0:n], lhsT=Csb['permR'].ap[:], rhs=qn.ap[:, 0:n], start=True, stop=True),
                         [qn, Csb['permR']], [psC])
                    l0 = t0 - TC
                    t1 = t1r.next()
                    t2 = t2r.next()
                    ck = cs['cosR%d' % a]
                    sk = cs['sinR%d' % a]
                    S.op('pool', lambda e: e.tensor_tensor(out=t1.ap[:, 0:n], in0=qn.ap[:, 0:n], in1=ck.ap[:, l0:l0 + n], op=ALU.mult),
                         [qn, ck], [t1])
                    S.op('dve', lambda e: e.tensor_tensor(out=t2.ap[:, 0:n], in0=psC.ap[:, 0:n], in1=sk.ap[:, l0:l0 + n], op=ALU.mult),
                         [psC, sk], [t2])
                    S.op('pool', lambda e: e.tensor_tensor(out=ob.ap[:, 0:n], in0=t1.ap[:, 0:n], in1=t2.ap[:, 0:n], op=ALU.add),
                         [t1, t2], [ob])
                    r2_store(ob, n, t0, cb)

                rcols = [cb * 128 for cb in range(32)] + [8192 + gb * 128 for gb in range(32)]
                wpf = Prefetch(wring, [(lambda t, c0=c0: S.op('pool', lambda e: e.dma_start(out=t.ap[:], in_=wsrc[:, :, c0:c0 + 128]),
                                                                 [tIN], [t], dma=True)) for c0 in rcols], 2)
                for cb in range(32):
                    is_q = cb < 16
                    a = cb % 2
                    col0 = cb * 128 if is_q else 2048 + (cb - 16) * 128
                    scl = 1.0 if is_q else 1.0 / 16.0
                    w = wpf.get(cb)
                    for (t0, n, lat) in chunks:
                        psA = psring.next()
                        for kt in range(KT):
                            S.op('pe', lambda e: e.matmul(psA.ap[:, 0:n], lhsT=w.ap[:, kt, :], rhs=hT.ap[:, kt, t0:t0 + n],
                                                          start=(kt == 0), stop=(kt == KT - 1)), [w, hT], [psA])
                        if lat:
                            qn = qnr.next()
                            S.op('act', lambda e: e.mul(out=qn.ap[:, 0:n], in_=psA.ap[:, 0:n], mul=scl), [psA], [qn])
                            rq.append((qn, n, t0, a, cb))
                            if len(rq) > 1:
                                r2_tail(*rq.pop(0))
                        else:
                            ob = obr.next()
                            S.op('act', lambda e: e.mul(out=ob.ap[:, 0:n], in_=psA.ap[:, 0:n], mul=scl), [psA], [ob])
                            r2_store(ob, n, t0, cb)
                while rq:
                    r2_tail(*rq.pop(0))
                for gb in range(32):
                    col0 = 8192 + gb * 128
                    w = wpf.get(32 + gb)
                    for (t0, n, lat) in chunks:
                        psA = psring.next()
                        for kt in range(KT):
                            S.op('pe', lambda e: e.matmul(psA.ap[:, 0:n], lhsT=w.ap[:, kt, :], rhs=hT.ap[:, kt, t0:t0 + n],
                                                          start=(kt == 0), stop=(kt == KT - 1)), [w, hT], [psA])
                        ob = obr.next()
                        S.op('act', lambda e: e.activation(out=ob.ap[:, 0:n], in_=psA.ap[:, 0:n], func=AF.Silu), [psA], [ob])
                        S.op('sp', lambda e: e.dma_start(out=RG[gb, :, t0:t0 + n], in_=ob.ap[:, 0:n]), [ob], [tRG], dma=True)
                wvr = Ring([sb(ph, nm('wv'), [128, KT, 512], BF16) for _ in range(2)])
                for vc in range(8):
                    wv = wvr.next()
                    S.op('pool', lambda e: e.dma_start(out=wv.ap[:], in_=wsrc[:, :, 4096 + vc * 512:4096 + (vc + 1) * 512]), [tIN], [wv], dma=True)
                    for i in range(NT):
                        ps = psring.next()
                        for kt in range(KT):
                            S.op('pe', lambda e: e.matmul(ps.ap[:], lhsT=hT.ap[:, kt, i * 128:(i + 1) * 128], rhs=wv.ap[:, kt, :],
                                                          start=(kt == 0), stop=(kt == KT - 1)), [hT, wv], [ps])
                        ob = obr.next()
                        if i % 2:
                            S.op('act', lambda e: e.copy(out=ob.ap[:], in_=ps.ap[:]), [ps], [ob])
                        else:
                            S.op('dve', lambda e: e.tensor_copy(out=ob.ap[:], in_=ps.ap[:]), [ps], [ob])
                        S.op('sp', lambda e: e.dma_start(out=RV[i * 128:(i + 1) * 128, vc * 512:(vc + 1) * 512], in_=ob.ap[:]), [ob], [tRV], dma=True)
                S.barrier()
            with contextlib.ExitStack() as ph:
                OFF = 1920
                SW = 3968
                CW = 2176
                ip = sb(ph, nm('ip'), [128, SW], F32)
                rp = sb(ph, nm('rp'), [128, SW], F32)
                rn = sb(ph, nm('rn'), [128, SW], F32)
                strip = sb(ph, nm('strip'), [128, SW], F32)
                tmpB = sb(ph, nm('tmpB'), [128, SW], F32)
                Ef = sb(ph, nm('Ef'), [128, CW], F32)
                Eb = sb(ph, nm('Eb'), [128, CW], F32)
                Cs = sb(ph, nm('Cs'), [128, CW], F32)
                tmpC = sb(ph, nm('tmpC'), [128, CW], F32)
                S.op('sp', lambda e: e.dma_start(out=ip.ap[:], in_=CI['retDelta']), [tIN], [ip], dma=True)
                S.op('sp', lambda e: e.dma_start(out=Ef.ap[:], in_=CI['retEf']), [tIN], [Ef], dma=True)
                S.op('sp', lambda e: e.dma_start(out=Eb.ap[:], in_=CI['retEb']), [tIN], [Eb], dma=True)
                S.op('dve', lambda e: e.tensor_scalar(out=rp.ap[:], in0=ip.ap[:], scalar1=0.0, scalar2=None, op0=ALU.max), [ip], [rp])
                S.op('dve', lambda e: e.tensor_tensor(out=rn.ap[:], in0=rp.ap[:], in1=ip.ap[:], op=ALU.subtract), [rp, ip], [rn])
                S.op('dve', lambda e: e.tensor_scalar(out=ip.ap[:], in0=ip.ap[:], scalar1=0.0, scalar2=None, op0=ALU.is_ge), [ip], [ip])
                lg = sb(ph, nm('lg'), [128, 16], F32)
                S.op('sp', lambda e: e.dma_start(out=lg.ap[:], in_=I['ret_decay_logit'][j].rearrange("a h -> (a h)").partition_broadcast(128)),
                     [tIN], [lg], dma=True)
                S.op('act', lambda e: e.activation(out=lg.ap[:], in_=lg.ap[:], func=AF.Exp, scale=-1.0), [lg], [lg])
                S.op('dve', lambda e: e.tensor_scalar(out=lg.ap[:], in0=lg.ap[:], scalar1=1.0, scalar2=None, op0=ALU.add), [lg], [lg])
                S.op('act', lambda e: e.activation(out=lg.ap[:], in_=lg.ap[:], func=AF.Ln), [lg], [lg])
                S.op('dve', lambda e: e.tensor_scalar(out=lg.ap[:], in0=lg.ap[:], scalar1=-1.0, scalar2=None, op0=ALU.mult), [lg], [lg])
                q2r = Ring([sb(ph, nm('q2'), [128, 2, TT], BF16) for _ in range(1)])
                k2r = Ring([sb(ph, nm('k2'), [128, 2, TT], BF16) for _ in range(1)])
                vhr = Ring([sb(ph, nm('vh'), [128, NT, 512], BF16) for _ in range(1)])
                smr = Ring([sb(ph, nm('sm'), [128, 512], BF16) for _ in range(3)])
                sqr = Ring([sb(ph, nm('sq'), [128, 512], F32) for _ in range(2)])
                rsr = Ring([sb(ph, nm('rs'), [128, 512], F32) for _ in range(1)])
                gr = Ring([sb(ph, nm('g'), [128, 512], BF16) for _ in range(2)])
                tmr = Ring([sb(ph, nm('tm'), [128, 512], F32) for _ in range(2)])
                obr = Ring([sb(ph, nm('ob'), [128, 512], BF16) for _ in range(2)])
                O = PS[0:4]
                pSr = Ring(PS[4:6])
                pN = PS[6]
                rpend = []

                def emit_rpv(sm, n, jt, idx, nsrc, vh):
                    for vt in range(4):
                        S.op('pe', lambda e: e.matmul(O[vt].ap[:, 0:n], lhsT=vh.ap[:, jt, vt * 128:(vt + 1) * 128], rhs=sm.ap[:, 0:n],
                                                      start=(idx == 0), stop=(idx == nsrc - 1)), [vh, sm], [O[vt]])

                for h_ in range(8):
                    S.op('act', lambda e: e.activation(out=strip.ap[:], in_=rp.ap[:], func=AF.Exp, scale=lg.ap[:, h_:h_ + 1]), [rp, lg], [strip])
                    S.op('act', lambda e: e.activation(out=tmpB.ap[:], in_=rn.ap[:], func=AF.Exp, scale=lg.ap[:, 8 + h_:9 + h_]), [rn, lg], [tmpB])
                    S.op('pool', lambda e: e.tensor_tensor(out=strip.ap[:], in0=strip.ap[:], in1=tmpB.ap[:], op=ALU.subtract), [strip, tmpB], [strip])
                    S.op('dve', lambda e: e.tensor_tensor(out=strip.ap[:], in0=strip.ap[:], in1=ip.ap[:], op=ALU.mult), [strip, ip], [strip])
                    S.op('pool', lambda e: e.tensor_tensor(out=strip.ap[:], in0=strip.ap[:], in1=tmpB.ap[:], op=ALU.add), [strip, tmpB], [strip])
                    S.op('act', lambda e: e.activation(out=Cs.ap[:], in_=Ef.ap[:], func=AF.Exp, scale=lg.ap[:, h_:h_ + 1]), [Ef, lg], [Cs])
                    S.op('act', lambda e: e.activation(out=tmpC.ap[:], in_=Eb.ap[:], func=AF.Exp, scale=lg.ap[:, 8 + h_:9 + h_]), [Eb, lg], [tmpC])
                    S.op('pool', lambda e: e.tensor_tensor(out=Cs.ap[:], in0=Cs.ap[:], in1=tmpC.ap[:], op=ALU.add), [Cs, tmpC], [Cs])
                    q2 = q2r.next()
                    k2 = k2r.next()
                    vh = vhr.next()
                    for a in range(2):
                        S.op('sp', lambda e: e.dma_start(out=q2.ap[:, a, :], in_=QT[2 * h_ + a]), [tQT], [q2], dma=True)
                        S.op('sp', lambda e: e.dma_start(out=k2.ap[:, a, :], in_=RK[2 * h_ + a]), [tRK], [k2], dma=True)
                    for i in range(NT):
                        S.op('sp', lambda e: e.dma_start(out=vh.ap[:, i, :], in_=RV[i * 128:(i + 1) * 128, h_ * 512:(h_ + 1) * 512]), [tRV], [vh], dma=True)
                    for (t0, n, lat) in chunks:
                        srcs = list(range(NT)) if lat else [0, 1]
                        for idx, jt in enumerate(srcs):
                            pS = pSr.next()
                            for a in range(2):
                                S.op('pe', lambda e: e.matmul(pS.ap[:, 0:n], lhsT=k2.ap[:, a, jt * 128:(jt + 1) * 128], rhs=q2.ap[:, a, t0:t0 + n],
                                                              start=(a == 0), stop=(a == 1)), [k2, q2], [pS])
                            if not lat:
                                x0 = 0 - 128 * jt + OFF
                                mk, mt = strip.ap[:, x0:x0 + n], strip
                            elif jt < 2:
                                y0 = (t0 - TC) + (128 if jt == 0 else 0)
                                mk, mt = Cs.ap[:, y0:y0 + n], Cs
                            else:
                                x0 = (t0 - TC) - 128 * (jt - 2) + OFF
                                mk, mt = strip.ap[:, x0:x0 + n], strip
                            sm = smr.next()
                            S.op('dve', lambda e: e.tensor_tensor(out=sm.ap[:, 0:n], in0=pS.ap[:, 0:n], in1=mk, op=ALU.mult), [pS, mt], [sm])
                            rpend.append((sm, n, jt, idx, len(srcs), vh))
                            if len(rpend) > 1:
                                emit_rpv(*rpend.pop(0))
                        while rpend:
                            emit_rpv(*rpend.pop(0))
                        for vt in range(4):
                            sq = sqr.next()
                            S.op('act', lambda e: e.activation(out=sq.ap[:, 0:n], in_=O[vt].ap[:, 0:n], func=AF.Square), [O[vt]], [sq])
                            S.op('pe', lambda e: e.matmul(pN.ap[:, 0:n], lhsT=Csb['ones_f'].ap[:], rhs=sq.ap[:, 0:n], start=(vt == 0), stop=(vt == 3)),
                                 [sq, Csb['ones_f']], [pN])
                        rs = rsr.next()
                        S.op('dve', lambda e: e.tensor_scalar(out=rs.ap[:, 0:n], in0=pN.ap[:, 0:n], scalar1=1.0 / 512, scalar2=EPS,
                                                              op0=ALU.mult, op1=ALU.add), [pN], [rs])
                        S.op('act', lambda e: e.activation(out=rs.ap[:, 0:n], in_=rs.ap[:, 0:n], func=AF.Sqrt), [rs], [rs])
                        S.op('dve', lambda e: e.reciprocal(out=rs.ap[:, 0:n], in_=rs.ap[:, 0:n]), [rs], [rs])
                        for vt in range(4):
                            g = gr.next()
                            S.op('sp', lambda e: e.dma_start(out=g.ap[:, 0:n], in_=RG[h_ * 4 + vt, :, t0:t0 + n]), [tRG], [g], dma=True)
                            tm = tmr.next()
                            S.op('dve', lambda e: e.tensor_tensor(out=tm.ap[:, 0:n], in0=O[vt].ap[:, 0:n], in1=rs.ap[:, 0:n], op=ALU.mult), [O[vt], rs], [tm])
                            ob = obr.next()
                            S.op('pool', lambda e: e.tensor_tensor(out=ob.ap[:, 0:n], in0=tm.ap[:, 0:n], in1=g.ap[:, 0:n], op=ALU.mult), [tm, g], [ob])
                            S.op('sp', lambda e: e.dma_start(out=OT[h_ * 4 + vt, :, t0:t0 + n], in_=ob.ap[:, 0:n]), [ob], [tOT], dma=True)
                S.barrier()
            phase_proj_residual(li, OT, tOT, 32, I['ret_w_o'][j], 0, with_ctx)


        def phase_hyena(li, j, with_ctx):
            wsrc = I['hy_w_in'][j].rearrange("(kt p) n -> p kt n", p=128)
            chunks = [(0, 256, False)] + [(256 + c * 512, 512, True) for c in range(4)]
            ZW = 2308
            with contextlib.ExitStack() as ph:
                hT = load_fm(ph, 'hT', HT, tHT, KT, 0, TT)
                A1 = sb(ph, nm('A1'), [128, 128], F32)
                A2 = sb(ph, nm('A2'), [80, 128], F32)
                cwT = sb(ph, nm('cwT'), [128, 208], F32)
                cwv = I['hy_conv_w'][j].rearrange("k (cb p) -> (k cb) p", p=128)
                S.op('sp', lambda e: e.dma_start(out=A1.ap[:], in_=cwv[0:128, :]), [tIN], [A1], dma=True)
                S.op('sp', lambda e: e.dma_start(out=A2.ap[0:16, :], in_=cwv[128:144, :]), [tIN], [A2], dma=True)
                S.op('sp', lambda e: e.dma_start(out=A2.ap[16:64, :], in_=I['hy_conv_b'][j].rearrange("(cb p) -> cb p", p=128)), [tIN], [A2], dma=True)
                S.op('sp', lambda e: e.dma_start(out=A2.ap[64:80, :], in_=I['hy_skip'][j].rearrange("(cb p) -> cb p", p=128)), [tIN], [A2], dma=True)
                ps = psring.next()
                S.op('pe', lambda e: e.transpose(out=ps.ap[:, 0:128], in_=A1.ap[:], identity=Csb['ident_f'].ap[:]), [A1, Csb['ident_f']], [ps])
                S.op('pe', lambda e: e.transpose(out=ps.ap[:, 128:208], in_=A2.ap[:], identity=Csb['ident_f'].ap[0:80, 0:80]), [A2, Csb['ident_f']], [ps])
                S.op('dve', lambda e: e.tensor_copy(out=cwT.ap[:], in_=ps.ap[:, 0:208]), [ps], [cwT])
                S.op('sp', lambda e: e.dma_start(out=SKIPT[:, :], in_=cwT.ap[:, 192:208]), [cwT], [tSKIPT], dma=True)
                wring = Ring([sb(ph, nm('wq'), [128, KT, 128], BF16) for _ in range(3)])
                zbs = [sb(ph, nm('zb'), [128, ZW], F32) for _ in range(3)]
                for zb in zbs:
                    S.op('pool', lambda e: e.memset(zb.ap[:], 0.0), [], [zb])
                zcs = [sb(ph, nm('zc'), [128, TT], F32) for _ in range(3)]
                ub = sb(ph, nm('ub'), [128, TT], BF16)
                ust = Ring([sb(ph, nm('ust'), [128, 6, 128], BF16) for _ in range(2)])
                hblocks = [cb for ct in range(KT) for cb in (16 + ct, 32 + ct, ct)]
                wpf = Prefetch(wring, [(lambda t, cb=cb: S.op('pool', lambda e: e.dma_start(out=t.ap[:], in_=wsrc[:, :, cb * 128:(cb + 1) * 128]),
                                                                 [tIN], [t], dma=True)) for cb in hblocks], 2)
                for ct in range(KT):
                    for bi, cb in enumerate((16 + ct, 32 + ct, ct)):
                        w = wpf.get(ct * 3 + bi)
                        zb = zbs[bi]
                        for (t0, n, lat) in chunks:
                            psA = psring.next()
                            for kt in range(KT):
                                S.op('pe', lambda e: e.matmul(psA.ap[:, 0:n], lhsT=w.ap[:, kt, :], rhs=hT.ap[:, kt, t0:t0 + n],
                                                              start=(kt == 0), stop=(kt == KT - 1)), [w, hT], [psA])
                            z0 = (1 + t0) if not lat else (259 + t0 - TC)
                            S.op('act', lambda e: e.copy(out=zb.ap[:, z0:z0 + n], in_=psA.ap[:, 0:n]), [psA], [zb])
                        zc = zcs[bi]
                        for (zoff, t0, n) in ((0, 0, TC), (258, TC, TL)):
                            S.op('dve', lambda e: e.tensor_scalar(out=zc.ap[:, t0:t0 + n], in0=zb.ap[:, zoff + 1:zoff + 1 + n],
                                                                  scalar1=cwT.ap[:, 48 + cb:49 + cb], scalar2=cwT.ap[:, 144 + cb:145 + cb],
                                                                  op0=ALU.mult, op1=ALU.add), [zb, cwT], [zc])
                            S.op('dve', lambda e: e.scalar_tensor_tensor(out=zc.ap[:, t0:t0 + n], in0=zb.ap[:, zoff:zoff + n],
                                                                         scalar=cwT.ap[:, cb:cb + 1], in1=zc.ap[:, t0:t0 + n],
                                                                         op0=ALU.mult, op1=ALU.add), [zb, cwT, zc], [zc])
                            S.op('dve', lambda e: e.scalar_tensor_tensor(out=zc.ap[:, t0:t0 + n], in0=zb.ap[:, zoff + 2:zoff + 2 + n],
                                                                         scalar=cwT.ap[:, 96 + cb:97 + cb], in1=zc.ap[:, t0:t0 + n],
                                                                         op0=ALU.mult, op1=ALU.add), [zb, cwT, zc], [zc])
                    S.op('pool', lambda e: e.tensor_tensor(out=zcs[0].ap[:], in0=zcs[0].ap[:], in1=zcs[1].ap[:], op=ALU.mult), [zcs[0], zcs[1]], [zcs[0]])
                    S.op('sp', lambda e: e.dma_start(out=UTF[ct], in_=zcs[0].ap[:]), [zcs[0]], [tUTF], dma=True)
                    S.op('sp', lambda e: e.dma_start(out=X0F[ct], in_=zcs[2].ap[:]), [zcs[2]], [tX0F], dma=True)
                    S.op('act', lambda e: e.copy(out=ub.ap[:], in_=zcs[0].ap[:]), [zcs[0]], [ub])
                    for g in range(3):
                        for jj in range(6):
                            i = g * 6 + jj
                            S.op('pe', lambda e: e.transpose(out=PSB.ap[:, jj * 128:(jj + 1) * 128], in_=ub.ap[:, i * 128:(i + 1) * 128],
                                                             identity=Csb['ident_b'].ap[:]), [ub, Csb['ident_b']], [PSB])
                        us = ust.next()
                        S.op('dve', lambda e: e.tensor_copy(out=us.ap[:].rearrange("p a b -> p (a b)"), in_=PSB.ap[:, 0:768]), [PSB], [us])
                        for hh in range(2):
                            i0 = g * 6 + hh * 3
                            S.op('sp', lambda e: e.dma_start(out=UTM[i0 * 128:(i0 + 3) * 128, ct * 128:(ct + 1) * 128].rearrange("(i p) c -> p i c", p=128),
                                                             in_=us.ap[:, hh * 3:(hh + 1) * 3, :]), [us], [tUTM], dma=True)
                S.barrier()
            for (tag, L, tok0) in ((('C', TC, 0),) if with_ctx else ()) + (('L', TL, TC),):
                hy_filter(j, tag, L)
                hy_conv(j, tag, L, tok0)
            phase_proj_residual(li, OT, tOT, 16, I['hy_w_out'][j], 0, with_ctx)

        def hy_filter(j, tag, L):
            with contextlib.ExitStack() as ph0:
                rnorm = sb(ph0, nm('rnorm'), [128, D], F32)
                hy_filter_inner(j, tag, L, rnorm)

        def hy_filter_inner(j, tag, L, rnorm):
            nkt = L // 128
            cw = min(512, L)
            with contextlib.ExitStack() as ph:
                zT = sb(ph, nm('zT'), [33, L], F32)
                S.op('sp', lambda e: e.dma_start(out=zT.ap[:], in_=CI['hyZ' + tag]), [tIN], [zT], dma=True)
                w1 = sb(ph, nm('w1'), [33, 128], F32)
                w2 = sb(ph, nm('w2'), [64, 128], F32)
                w3 = sb(ph, nm('w3'), [64, 2 * D], F32)
                S.op('dve', lambda e: e.memset(w1.ap[:], 0.0), [], [w1])
                S.op('dve', lambda e: e.memset(w2.ap[:], 0.0), [], [w2])
                S.op('sp', lambda e: e.dma_start(out=w1.ap[:, 0:64], in_=I['hy_f_w1'][j]), [tIN], [w1], dma=True)
                S.op('sp', lambda e: e.dma_start(out=w2.ap[:, 0:64], in_=I['hy_f_w2'][j]), [tIN], [w2], dma=True)
                S.op('sp', lambda e: e.dma_start(out=w3.ap[:], in_=I['hy_f_w3'][j]), [tIN], [w3], dma=True)
                pv = sb(ph, nm('pv'), [64, 4], F32)
                for ci, k in enumerate(('hy_f_b1', 'hy_f_freq1', 'hy_f_b2', 'hy_f_freq2')):
                    S.op('sp', lambda e: e.dma_start(out=pv.ap[:, ci:ci + 1], in_=I[k][j].rearrange("(p o) -> p o", o=1)), [tIN], [pv], dma=True)
                S.op('dve', lambda e: e.tensor_scalar(out=pv.ap[:, 1:2], in0=pv.ap[:, 1:2], scalar1=1.0 / (2 * math.pi), scalar2=None, op0=ALU.mult), [pv], [pv])
                S.op('dve', lambda e: e.tensor_scalar(out=pv.ap[:, 3:4], in0=pv.ap[:, 3:4], scalar1=1.0 / (2 * math.pi), scalar2=None, op0=ALU.mult), [pv], [pv])
                h1T = sb(ph, nm('h1T'), [64, L], F32)
                h2T = sb(ph, nm('h2T'), [64, L], F32)
                r = sb(ph, nm('r'), [64, 512], F32)
                ri = sb(ph, nm('ri'), [64, 512], mybir.dt.int32)
                rf = sb(ph, nm('rf'), [64, 512], F32)
                msk = sb(ph, nm('msk'), [64, 512], F32)

                def sin_layer(wt, kdim, src, dst, bcol):
                    for c0 in range(0, L, cw):
                        ps = psring.next()
                        S.op('pe', lambda e: e.matmul(ps.ap[:, 0:cw], lhsT=wt.ap[0:kdim, :], rhs=src.ap[0:kdim, c0:c0 + cw], start=True, stop=True),
                             [wt, src], [ps])
                        S.op('dve', lambda e: e.tensor_scalar(out=r.ap[:, 0:cw], in0=ps.ap[0:64, 0:cw], scalar1=pv.ap[:, bcol:bcol + 1],
                                                              scalar2=pv.ap[:, bcol + 1:bcol + 2], op0=ALU.add, op1=ALU.mult), [ps, pv], [r])
                        S.op('dve', lambda e: e.tensor_copy(out=ri.ap[:, 0:cw], in_=r.ap[:, 0:cw]), [r], [ri])
                        S.op('dve', lambda e: e.tensor_copy(out=rf.ap[:, 0:cw], in_=ri.ap[:, 0:cw]), [ri], [rf])
                        S.op('dve', lambda e: e.tensor_tensor(out=r.ap[:, 0:cw], in0=r.ap[:, 0:cw], in1=rf.ap[:, 0:cw], op=ALU.subtract), [r, rf], [r])
                        S.op('dve', lambda e: e.tensor_scalar(out=msk.ap[:, 0:cw], in0=r.ap[:, 0:cw], scalar1=0.5, scalar2=None, op0=ALU.is_gt), [r], [msk])
                        S.op('dve', lambda e: e.tensor_tensor(out=r.ap[:, 0:cw], in0=r.ap[:, 0:cw], in1=msk.ap[:, 0:cw], op=ALU.subtract), [r, msk], [r])
                        S.op('dve', lambda e: e.tensor_scalar(out=msk.ap[:, 0:cw], in0=r.ap[:, 0:cw], scalar1=-0.5, scalar2=None, op0=ALU.is_lt), [r], [msk])
                        S.op('dve', lambda e: e.tensor_tensor(out=r.ap[:, 0:cw], in0=r.ap[:, 0:cw], in1=msk.ap[:, 0:cw], op=ALU.add), [r, msk], [r])
                        S.op('act', lambda e: e.activation(out=dst.ap[:, c0:c0 + cw], in_=r.ap[:, 0:cw], func=AF.Sin, scale=6.28318), [r], [dst])
                sin_layer(w1, 33, zT, h1T, 0)
                sin_layer(w2, 64, h1T, h2T, 2)
                dring = Ring([sb(ph, nm('dec'), [128, 512], F32) for _ in range(3)])
                hfr = Ring([sb(ph, nm('hf'), [128, 512], F32) for _ in range(3)])
                hbr = Ring([sb(ph, nm('hb'), [128, 512], F32) for _ in range(3)])
                abr = Ring([sb(ph, nm('ab'), [128, 512], F32) for _ in range(3)])
                aacc = [sb(ph, nm('aacc'), [128, 512], F32) for _ in range(4)]
                for c4 in range(4):
                    S.op('pool', lambda e: e.memset(aacc[c4].ap[:], 0.0), [], [aacc[c4]])
                hpr = Ring([sb(ph, nm('hp'), [128, 512], BF16) for _ in range(3)])
                NP = PS[0:4]
                pr = Ring(PS[4:7])
                dpf = Prefetch(dring, [(lambda t, kt=kt, c4=c4: S.op('sp', lambda e: e.dma_start(out=t.ap[:], in_=CI['hyDecay' + tag][kt * 128:(kt + 1) * 128, c4 * 512:(c4 + 1) * 512]),
                                                                      [tIN], [t], dma=True)) for kt in range(nkt) for c4 in range(4)], 2)
                for kt in range(nkt):
                    for c4 in range(4):
                        dec = dpf.get(kt * 4 + c4)
                        hh = []
                        for dr, rg in ((0, hfr), (1, hbr)):
                            ps = pr.next()
                            S.op('pe', lambda e: e.matmul(ps.ap[:], lhsT=h2T.ap[:, kt * 128:(kt + 1) * 128], rhs=w3.ap[:, dr * D + c4 * 512:dr * D + (c4 + 1) * 512],
                                                          start=True, stop=True), [h2T, w3], [ps])
                            ht = rg.next()
                            S.op('dve', lambda e: e.tensor_tensor(out=ht.ap[:], in0=ps.ap[:], in1=dec.ap[:], op=ALU.mult), [ps, dec], [ht])
                            if dr == 1 and kt == 0:
                                S.op('dve', lambda e: e.memset(ht.ap[0:1, :], 0.0), [], [ht])
                            ab = abr.next()
                            S.op('act', lambda e: e.activation(out=ab.ap[:], in_=ht.ap[:], func=AF.Abs), [ht], [ab])
                            S.op('dve', lambda e: e.tensor_tensor(out=aacc[c4].ap[:], in0=aacc[c4].ap[:], in1=ab.ap[:], op=ALU.add), [ab, aacc[c4]], [aacc[c4]])
                            hh.append(ht)
                        hp = hpr.next()
                        S.op('pool', lambda e: e.tensor_tensor(out=hp.ap[:], in0=hh[0].ap[:], in1=hh[1].ap[:], op=ALU.add), [hh[0], hh[1]], [hp])
                        S.op('sp', lambda e: e.dma_start(out=HPM[0, kt * 128:(kt + 1) * 128, c4 * 512:(c4 + 1) * 512], in_=hp.ap[:]), [hp], [tHPM], dma=True)
                        hm = hpr.next()
                        S.op('pool', lambda e: e.tensor_tensor(out=hm.ap[:], in0=hh[0].ap[:], in1=hh[1].ap[:], op=ALU.subtract), [hh[0], hh[1]], [hm])
                        S.op('sp', lambda e: e.dma_start(out=HPM[1, kt * 128:(kt + 1) * 128, c4 * 512:(c4 + 1) * 512], in_=hm.ap[:]), [hm], [tHPM], dma=True)
                for c4 in range(4):
                    S.op('pe', lambda e: e.matmul(NP[c4].ap[:], lhsT=Csb['ones_f'].ap[:], rhs=aacc[c4].ap[:], start=True, stop=True),
                         [aacc[c4], Csb['ones_f']], [NP[c4]])
                    S.op('dve', lambda e: e.reciprocal(out=rnorm.ap[:, c4 * 512:(c4 + 1) * 512], in_=NP[c4].ap[:]), [NP[c4]], [rnorm])
                S.barrier()
            with contextlib.ExitStack() as ph:
                hpr = Ring([sb(ph, nm('hpc'), [128, nkt, 512], BF16) for _ in range(2)])
                hmr = Ring([sb(ph, nm('hmc'), [128, nkt, 512], BF16) for _ in range(2)])
                ctr = Ring([sb(ph, nm('ct'), [128, nkt, 128], BF16) for _ in range(3)])
                strr = Ring([sb(ph, nm('st'), [128, nkt, 128], BF16) for _ in range(3)])
                hor = Ring([sb(ph, nm('ho'), [128, 512], F32) for _ in range(4)])
                cpf = Prefetch(ctr, [(lambda t, ft=ft: S.op('sp', lambda e: e.dma_start(out=t.ap[:].rearrange("p a b -> p (a b)"), in_=CI['hyCT' + tag][ft]),
                                                             [tIN], [t], dma=True)) for _c in range(4) for ft in range(nkt)], 2)
                spf = Prefetch(strr, [(lambda t, ft=ft: S.op('sp', lambda e: e.dma_start(out=t.ap[:].rearrange("p a b -> p (a b)"), in_=CI['hyST' + tag][ft]),
                                                              [tIN], [t], dma=True)) for _c in range(4) for ft in range(nkt)], 2)
                for c4 in range(4):
                    hp = hpr.next()
                    hm = hmr.next()
                    for kt in range(nkt):
                        S.op('sp', lambda e: e.dma_start(out=hp.ap[:, kt, :], in_=HPM[0, kt * 128:(kt + 1) * 128, c4 * 512:(c4 + 1) * 512]), [tHPM], [hp], dma=True)
                        S.op('sp', lambda e: e.dma_start(out=hm.ap[:, kt, :], in_=HPM[1, kt * 128:(kt + 1) * 128, c4 * 512:(c4 + 1) * 512]), [tHPM], [hm], dma=True)
                    for ft in range(nkt):
                        ctt = cpf.get(c4 * nkt + ft)
                        stt = spf.get(c4 * nkt + ft)
                        for si, (mt, hx) in enumerate(((ctt, hp), (stt, hm))):
                            ps = psring.next()
                            for kt in range(nkt):
                                S.op('pe', lambda e: e.matmul(ps.ap[:], lhsT=mt.ap[:, kt, :], rhs=hx.ap[:, kt, :], start=(kt == 0), stop=(kt == nkt - 1)),
                                     [mt, hx], [ps])
                            ho = hor.next()
                            S.op('dve', lambda e: e.tensor_tensor(out=ho.ap[:], in0=ps.ap[:], in1=rnorm.ap[:, c4 * 512:(c4 + 1) * 512], op=ALU.mult), [ps, rnorm], [ho])
                            S.op('sp', lambda e: e.dma_start(out=HSPEC[tag][si, ft * 128:(ft + 1) * 128, c4 * 512:(c4 + 1) * 512], in_=ho.ap[:]), [ho], [tHSPEC[tag]], dma=True)
                S.barrier()

        def hy_conv(j, tag, L, tok0):
            nkt = L // 128
            tcw = min(512, L)
            ntc = L // tcw
            nfft = 2 * L
            with contextlib.ExitStack() as ph:
                skT = sb(ph, nm('skT'), [128, KT], F32)
                S.op('sp', lambda e: e.dma_start(out=skT.ap[:], in_=SKIPT[:, :]), [tSKIPT], [skT], dma=True)
                ur = Ring([sb(ph, nm('u'), [128, nkt, 512], BF16) for _ in range(1)])
                Yr = Ring([sb(ph, nm('Y'), [128, 2 * nkt, 512], BF16) for _ in range(1)])
                ctr = Ring([sb(ph, nm('ct'), [128, nkt, 128], BF16) for _ in range(3)])
                strr = Ring([sb(ph, nm('st'), [128, nkt, 128], BF16) for _ in range(3)])
                hcr = Ring([sb(ph, nm('hc'), [128, 512], F32) for _ in range(3)])
                hsr = Ring([sb(ph, nm('hs'), [128, 512], F32) for _ in range(3)])
                ucr = Ring([sb(ph, nm('uc'), [128, 512], F32) for _ in range(2)])
                usr = Ring([sb(ph, nm('us'), [128, 512], F32) for _ in range(2)])
                tr_ = Ring([sb(ph, nm('tt'), [128, 512], F32) for _ in range(4)])
                cfr = Ring([sb(ph, nm('cf'), [128, nkt, tcw], BF16) for _ in range(2)])
                sfr = Ring([sb(ph, nm('sf'), [128, nkt, tcw], BF16) for _ in range(2)])
                utr = Ring([sb(ph, nm('ut'), [128, 512], F32) for _ in range(2)])
                x0r = Ring([sb(ph, nm('x0'), [128, 512], F32) for _ in range(2)])
                obr = Ring([sb(ph, nm('ob'), [128, 512], BF16) for _ in range(2)])
                cpf = Prefetch(ctr, [(lambda t, ft=ft: S.op('sp', lambda e: e.dma_start(out=t.ap[:].rearrange("p a b -> p (a b)"), in_=CI['hyCT' + tag][ft]),
                                                             [tIN], [t], dma=True)) for _c in range(4) for ft in range(nkt)], 2)
                spf = Prefetch(strr, [(lambda t, ft=ft: S.op('sp', lambda e: e.dma_start(out=t.ap[:].rearrange("p a b -> p (a b)"), in_=CI['hyST' + tag][ft]),
                                                              [tIN], [t], dma=True)) for _c in range(4) for ft in range(nkt)], 2)
                hcpf = Prefetch(hcr, [(lambda t, ft=ft, c4=c4: S.op('sp', lambda e: e.dma_start(out=t.ap[:], in_=HSPEC[tag][0, ft * 128:(ft + 1) * 128, c4 * 512:(c4 + 1) * 512]),
                                                                     [tHSPEC[tag]], [t], dma=True)) for c4 in range(4) for ft in range(nkt)], 2)
                hspf = Prefetch(hsr, [(lambda t, ft=ft, c4=c4: S.op('sp', lambda e: e.dma_start(out=t.ap[:], in_=HSPEC[tag][1, ft * 128:(ft + 1) * 128, c4 * 512:(c4 + 1) * 512]),
                                                                     [tHSPEC[tag]], [t], dma=True)) for c4 in range(4) for ft in range(nkt)], 2)
                cfpf = Prefetch(cfr, [(lambda t, tc=tc: S.op('sp', lambda e: e.dma_start(out=t.ap[:].rearrange("p a b -> p (a b)"), in_=CI['hyCF' + tag][tc]),
                                                              [tIN], [t], dma=True)) for _c in range(4) for tc in range(ntc)], 1)
                sfpf = Prefetch(sfr, [(lambda t, tc=tc: S.op('sp', lambda e: e.dma_start(out=t.ap[:].rearrange("p a b -> p (a b)"), in_=CI['hySF' + tag][tc]),
                                                              [tIN], [t], dma=True)) for _c in range(4) for tc in range(ntc)], 1)
                for c4 in range(4):
                    u = ur.next()
                    for kt in range(nkt):
                        S.op('sp', lambda e: e.dma_start(out=u.ap[:, kt, :], in_=UTM[tok0 + kt * 128:tok0 + (kt + 1) * 128, c4 * 512:(c4 + 1) * 512]), [tUTM], [u], dma=True)
                    Y = Yr.next()
                    for ft in range(nkt):
                        ctt = cpf.get(c4 * nkt + ft)
                        stt = spf.get(c4 * nkt + ft)
                        hc = hcpf.get(c4 * nkt + ft)
                        hs = hspf.get(c4 * nkt + ft)
                        pc = psring.next()
                        pss = psring.next()
                        for kt in range(nkt):
                            S.op('pe', lambda e: e.matmul(pc.ap[:], lhsT=ctt.ap[:, kt, :], rhs=u.ap[:, kt, :], start=(kt == 0), stop=(kt == nkt - 1)), [ctt, u], [pc])
                        for kt in range(nkt):
                            S.op('pe', lambda e: e.matmul(pss.ap[:], lhsT=stt.ap[:, kt, :], rhs=u.ap[:, kt, :], start=(kt == 0), stop=(kt == nkt - 1)), [stt, u], [pss])
                        uc = ucr.next()
                        us = usr.next()
                        S.op('act', lambda e: e.copy(out=uc.ap[:], in_=pc.ap[:]), [pc], [uc])
                        S.op('act', lambda e: e.copy(out=us.ap[:], in_=pss.ap[:]), [pss], [us])
                        t1 = tr_.next(); t2 = tr_.next(); t3 = tr_.next(); t4 = tr_.next()
                        S.op('dve', lambda e: e.tensor_tensor(out=t1.ap[:], in0=uc.ap[:], in1=hc.ap[:], op=ALU.mult), [uc, hc], [t1])
                        S.op('pool', lambda e: e.tensor_tensor(out=t2.ap[:], in0=us.ap[:], in1=hs.ap[:], op=ALU.mult), [us, hs], [t2])
                        S.op('dve', lambda e: e.tensor_tensor(out=Y.ap[:, ft, :], in0=t1.ap[:], in1=t2.ap[:], op=ALU.subtract), [t1, t2], [Y])
                        S.op('pool', lambda e: e.tensor_tensor(out=t3.ap[:], in0=uc.ap[:], in1=hs.ap[:], op=ALU.mult), [uc, hs], [t3])
                        S.op('dve', lambda e: e.tensor_tensor(out=t4.ap[:], in0=us.ap[:], in1=hc.ap[:], op=ALU.mult), [us, hc], [t4])
                        S.op('pool', lambda e: e.tensor_tensor(out=Y.ap[:, nkt + ft, :], in0=t3.ap[:], in1=t4.ap[:], op=ALU.add), [t3, t4], [Y])
                    for tc in range(ntc):
                        cf = cfpf.get(c4 * ntc + tc)
                        sf = sfpf.get(c4 * ntc + tc)
                        for ctl in range(4):
                            cg = c4 * 4 + ctl
                            ps = psring.next()
                            for ft in range(nkt):
                                S.op('pe', lambda e: e.matmul(ps.ap[:, 0:tcw], lhsT=Y.ap[:, ft, ctl * 128:(ctl + 1) * 128], rhs=cf.ap[:, ft, :],
                                                              start=(ft == 0), stop=False), [Y, cf], [ps])
                                S.op('pe', lambda e: e.matmul(ps.ap[:, 0:tcw], lhsT=Y.ap[:, nkt + ft, ctl * 128:(ctl + 1) * 128], rhs=sf.ap[:, ft, :],
                                                              start=False, stop=(ft == nkt - 1)), [Y, sf], [ps])
                            g0 = tok0 + tc * tcw
                            ut = utr.next()
                            x0 = x0r.next()
                            S.op('sp', lambda e: e.dma_start(out=ut.ap[:, 0:tcw], in_=UTF[cg, :, g0:g0 + tcw]), [tUTF], [ut], dma=True)
                            S.op('sp', lambda e: e.dma_start(out=x0.ap[:, 0:tcw], in_=X0F[cg, :, g0:g0 + tcw]), [tX0F], [x0], dma=True)
                            S.op('pool', lambda e: e.tensor_scalar(out=ut.ap[:, 0:tcw], in0=ut.ap[:, 0:tcw], scalar1=skT.ap[:, cg:cg + 1], scalar2=None, op0=ALU.mult),
                                 [ut, skT], [ut])
                            S.op('dve', lambda e: e.scalar_tensor_tensor(out=ut.ap[:, 0:tcw], in0=ps.ap[:, 0:tcw], scalar=2.0 / nfft, in1=ut.ap[:, 0:tcw],
                                                                         op0=ALU.mult, op1=ALU.add), [ps, ut], [ut])
                            ob = obr.next()
                            S.op('pool', lambda e: e.tensor_tensor(out=ob.ap[:, 0:tcw], in0=ut.ap[:, 0:tcw], in1=x0.ap[:, 0:tcw], op=ALU.mult), [ut, x0], [ob])
                            S.op('sp', lambda e: e.dma_start(out=OT[cg, :, g0:g0 + tcw], in_=ob.ap[:, 0:tcw]), [ob], [tOT], dma=True)
                S.barrier()

        def phase_moe(li, with_ctx):
            MOD = MODS[li % 2]
            with contextlib.ExitStack() as ph:
                if MOD_OVERLAP and (li + 1) in layers and (li + 1) not in mod_done:
                    phase_mod(li + 1, stk=ph)
                work = sb(ph, nm('work'), [NE, TL], F32)
                m8 = sb(ph, nm('m8'), [NE, 8], F32)
                ones_r = sb(ph, nm('ones_r'), [NE, TL], F32)
                maskT = sb(ph, nm('maskT'), [NE, TT], F32)
                cum = sb(ph, nm('cum'), [NE, TT], F32)
                S.op('pool', lambda e: e.memset(ones_r.ap[:], 1.0), [], [ones_r])
                segs = [(TC, TL, CAPL)] + ([(0, TC, CAPC)] if with_ctx else [])
                for (t0, n, cap) in segs:
                    S.op('dve', lambda e: e.tensor_copy(out=work.ap[:, 0:n], in_=probsT.ap[:, t0:t0 + n]), [probsT], [work])
                    for it in range(cap // 8):
                        S.op('dve', lambda e: e.max(out=m8.ap[:], in_=work.ap[:, 0:n]), [work], [m8])
                        if it < cap // 8 - 1:
                            S.op('dve', lambda e: e.match_replace(out=work.ap[:, 0:n], in_to_replace=m8.ap[:], in_values=work.ap[:, 0:n],
                                                                  imm_value=-1.0), [m8, work], [work])
                    S.op('dve', lambda e: e.tensor_scalar(out=maskT.ap[:, t0:t0 + n], in0=probsT.ap[:, t0:t0 + n], scalar1=m8.ap[:, 7:8],
                                                          scalar2=None, op0=ALU.is_ge), [probsT, m8], [maskT])
                    S.op('dve', lambda e: e.tensor_tensor_scan(out=cum.ap[:, t0:t0 + n], data0=ones_r.ap[:, 0:n], data1=maskT.ap[:, t0:t0 + n],
                                                               initial=0.0, op0=ALU.mult, op1=ALU.add), [ones_r, maskT], [cum])
                    S.op('dve', lambda e: e.tensor_tensor(out=cum.ap[:, t0:t0 + n], in0=cum.ap[:, t0:t0 + n], in1=maskT.ap[:, t0:t0 + n],
                                                          op=ALU.mult), [cum, maskT], [cum])
                    S.op('dve', lambda e: e.tensor_scalar(out=cum.ap[:, t0:t0 + n], in0=cum.ap[:, t0:t0 + n], scalar1=-1.0, scalar2=None,
                                                          op0=ALU.add), [cum], [cum])
                if not with_ctx:
                    S.op('dve', lambda e: e.memset(cum.ap[:, 0:TC], -1.0), [], [cum])
                S.op('sp', lambda e: e.dma_start(out=POS[:, :], in_=cum.ap[:]), [cum], [tPOS], dma=True)
                gt = sb(ph, nm('gt'), [128, NE], F32)
                gh = sb(ph, nm('gh'), [128, NE], F32)
                for i in range(NT):
                    ps = psring.next()
                    S.op('pe', lambda e: e.transpose(out=ps.ap[:, 0:NE], in_=cum.ap[:, i * 128:(i + 1) * 128],
                                                     identity=Csb['ident_f'].ap[0:NE, 0:NE]), [cum, Csb['ident_f']], [ps])
                    S.op('dve', lambda e: e.tensor_copy(out=posTM.ap[:, i, :], in_=ps.ap[:, 0:NE]), [ps], [posTM])
                    S.op('dve', lambda e: e.scalar_tensor_tensor(out=gt.ap[:], in0=posTM.ap[:, i, :], scalar=0.0, in1=probs_tm.ap[:, i, :],
                                                                 op0=ALU.is_ge, op1=ALU.mult), [posTM, probs_tm], [gt])
                    S.op('dve', lambda e: e.tensor_copy(out=GHL.ap[:, i, :, 0], in_=gt.ap[:]), [gt], [GHL])
                    S.op('dve', lambda e: e.tensor_copy(out=gh.ap[:], in_=GHL.ap[:, i, :, 0]), [GHL], [gh])
                    S.op('dve', lambda e: e.tensor_tensor(out=GHL.ap[:, i, :, 1], in0=gt.ap[:], in1=gh.ap[:], op=ALU.subtract), [gt, gh], [GHL])
                S.barrier()
            if stop == 'm2':
                return
            jts = [(0, 128), (128, 128)] + ([(256, 32)] if with_ctx else [])
            NJ = 288 if with_ctx else 256
            with contextlib.ExitStack() as ph:
                h2 = sb(ph, nm('h2'), [128, NT, D], BF16)
                for i in range(0 if with_ctx else 2, NT):
                    S.op('sp', lambda e: e.dma_start(out=h2.ap[:, i, :], in_=H2TM[i * 128:(i + 1) * 128, :]), [tH2], [h2], dma=True)
                wring = Ring([sb(ph, nm('we'), [128, KT, 512], BF16) for _ in range(4)])
                selr = Ring([sb(ph, nm('sel'), [128, NT, 256], BF16) for _ in range(2)])
                xgr = Ring([sb(ph, nm('xg'), [128, KT, 288], BF16) for _ in range(1)])
                actTr = Ring([sb(ph, nm('actT'), [128, 8, 288], BF16) for _ in range(2)])
                sar = Ring([sb(ph, nm('sa'), [128, 512], F32) for _ in range(2)])
                ygr = Ring([sb(ph, nm('yg'), [128, 3, D], BF16) for _ in range(1)])
                gsr = Ring([sb(ph, nm('gs'), [128, 4], F32) for _ in range(2)])
                gsfr = Ring([sb(ph, nm('gsf'), [128, 8], F32) for _ in range(2)])
                def build_sel(ex):
                    sel = selr.next()
                    for i in range(0 if with_ctx else 2, NT):
                        n = 32 if i < 2 else 256
                        S.op('dve',
                             lambda e: e.tensor_scalar(out=sel.ap[:, i, 0:n], in0=Csb['iota_row'].ap[:, 0:n], scalar1=posTM.ap[:, i, ex:ex + 1],
                                                       scalar2=None, op0=ALU.is_equal), [Csb['iota_row'], posTM], [sel])
                    return sel
                sel_next = build_sel(0)
                for ex in range(NE):
                    sel = sel_next
                    gs = gsr.next()
                    psg = psring.next()
                    for ji, (j0, jn) in enumerate(jts):
                        tl = [0, 1] if j0 == 256 else list(range(2, NT))
                        for idx, i in enumerate(tl):
                            lo = 0 if j0 == 256 else j0
                            S.op('pe', lambda e: e.matmul(psg.ap[0:jn, ji * 2:ji * 2 + 2], lhsT=sel.ap[:, i, lo:lo + jn], rhs=GHL.ap[:, i, ex, :],
                                                          start=(idx == 0), stop=(idx == len(tl) - 1)), [sel, GHL], [psg])
                    gsf = gsfr.next()
                    for ji, (j0, jn) in enumerate(jts):
                        S.op('act', lambda e: e.copy(out=gsf.ap[0:jn, ji * 2:ji * 2 + 2], in_=psg.ap[0:jn, ji * 2:ji * 2 + 2]), [psg], [gsf])
                        S.op('dve', lambda e: e.tensor_tensor(out=gs.ap[0:jn, ji:ji + 1], in0=gsf.ap[0:jn, ji * 2:ji * 2 + 1],
                                                              in1=gsf.ap[0:jn, ji * 2 + 1:ji * 2 + 2], op=ALU.add), [gsf], [gs])
                    xg = xgr.next()
                    for dtile in range(KT):
                        ps = psring.next()
                        for i in range(2, NT):
                            S.op('pe', lambda e: e.matmul(ps.ap[:, 0:256], lhsT=h2.ap[:, i, dtile * 128:(dtile + 1) * 128], rhs=sel.ap[:, i, 0:256],
                                                          start=(i == 2), stop=(i == NT - 1)), [h2, sel], [ps])
                        if with_ctx:
                            for i in range(2):
                                S.op('pe', lambda e: e.matmul(ps.ap[:, 256:288], lhsT=h2.ap[:, i, dtile * 128:(dtile + 1) * 128], rhs=sel.ap[:, i, 0:32],
                                                              start=(i == 0), stop=(i == 1)), [h2, sel], [ps])
                        S.op('dve' if dtile % 2 else 'act',
                             (lambda e: e.tensor_copy(out=xg.ap[:, dtile, 0:NJ], in_=ps.ap[:, 0:NJ])) if dtile % 2 else
                             (lambda e: e.copy(out=xg.ap[:, dtile, 0:NJ], in_=ps.ap[:, 0:NJ])), [ps], [xg])
                    if ex + 1 < NE:
                        sel_next = build_sel(ex + 1)
                    actT = actTr.next()
                    for fc in range(2):
                        wg = wring.next()
                        S.op('pool', lambda e: e.dma_start(out=wg.ap[:], in_=I['moe_w_gate'][li, ex].rearrange("(kt p) f -> p kt f", p=128)[:, :, fc * 512:(fc + 1) * 512]),
                             [tIN], [wg], dma=True)
                        wu = wring.next()
                        S.op('pool', lambda e: e.dma_start(out=wu.ap[:], in_=I['moe_w_up'][li, ex].rearrange("(kt p) f -> p kt f", p=128)[:, :, fc * 512:(fc + 1) * 512]),
                             [tIN], [wu], dma=True)
                        for fl in range(4):
                            ft = fc * 4 + fl
                            pA = psring.next()
                            pU = psring.next()
                            for kt in range(KT):
                                S.op('pe', lambda e: e.matmul(pA.ap[:, 0:NJ], lhsT=wg.ap[:, kt, fl * 128:(fl + 1) * 128], rhs=xg.ap[:, kt, 0:NJ],
                                                              start=(kt == 0), stop=(kt == KT - 1)), [xg, wg], [pA])
                            for kt in range(KT):
                                S.op('pe', lambda e: e.matmul(pU.ap[:, 0:NJ], lhsT=wu.ap[:, kt, fl * 128:(fl + 1) * 128], rhs=xg.ap[:, kt, 0:NJ],
                                                              start=(kt == 0), stop=(kt == KT - 1)), [xg, wu], [pU])
                            sa = sar.next()
                            S.op('act', lambda e: e.activation(out=sa.ap[:, 0:NJ], in_=pA.ap[:, 0:NJ], func=AF.Silu), [pA], [sa])
                            S.op('dve', lambda e: e.tensor_tensor(out=actT.ap[:, ft, 0:NJ], in0=pU.ap[:, 0:NJ], in1=sa.ap[:, 0:NJ],
                                                                  op=ALU.mult), [pU, sa], [actT])
                    yg = ygr.next()
                    for dc in range(4):
                        wd = wring.next()
                        S.op('pool', lambda e: e.dma_start(out=wd.ap[:, 0:8, :], in_=I['moe_w_down'][li, ex].rearrange("(kt p) f -> p kt f", p=128)[:, :, dc * 512:(dc + 1) * 512]),
                             [tIN], [wd], dma=True)
                        for ji, (j0, jn) in enumerate(jts):
                            pY = psring.next()
                            for ft in range(8):
                                S.op('pe', lambda e: e.matmul(pY.ap[0:jn, :], lhsT=actT.ap[:, ft, j0:j0 + jn], rhs=wd.ap[:, ft, :],
                                                              start=(ft == 0), stop=(ft == 7)), [actT, wd], [pY])
                            if (dc + ji) % 2:
                                S.op('act', lambda e: e.mul(out=yg.ap[0:jn, ji, dc * 512:(dc + 1) * 512], in_=pY.ap[0:jn, :],
                                                            mul=gs.ap[0:jn, ji:ji + 1]), [pY, gs], [yg])
                            else:
                                S.op('dve', lambda e: e.tensor_scalar(out=yg.ap[0:jn, ji, dc * 512:(dc + 1) * 512], in0=pY.ap[0:jn, :],
                                                                      scalar1=gs.ap[0:jn, ji:ji + 1], scalar2=None, op0=ALU.mult), [pY, gs], [yg])
                    for ji, (j0, jn) in enumerate(jts):
                        S.op('sp', lambda e: e.dma_start(out=YG[ex, j0:j0 + jn, :], in_=yg.ap[0:jn, ji, :]), [yg], [tYG], dma=True)
                S.barrier()
            if stop == 'm3':
                return
            with contextlib.ExitStack() as ph:
                g_bc = [bcast_load(ph, 'g2', MOD[which, 5 * D:6 * D]) for which in (0, 1)]
                posr = Ring([sb(ph, nm('posb'), [128, NE, 512], F32) for _ in range(1)])
                selTs = [sb(ph, nm('selT'), [128, 2, 512], BF16) for _ in range(NE)]
                ygr = Ring([sb(ph, nm('ygc'), [128, NE, 2, 512], BF16) for _ in range(2)])
                xring = Ring([sb(ph, nm('xo'), [128, 512], F32) for _ in range(4)])
                tring = Ring([sb(ph, nm('to'), [128, 512], F32) for _ in range(3)])
                tchunks = [(256 + c * 512, 512, False) for c in range(4)]
                if with_ctx:
                    tchunks = [(0, 256, True)] + tchunks
                def yg_loader(t, dc, isctx):
                    if isctx:
                        S.op('sp', lambda e: e.dma_start(out=t.ap[0:32, :, 0, :], in_=YG[:, 256:288, dc * 512:(dc + 1) * 512].rearrange("e j d -> j e d")),
                             [tYG], [t], dma=True)
                    else:
                        for jt in range(2):
                            S.op('sp', lambda e: e.dma_start(out=t.ap[:, :, jt, :],
                                                             in_=YG[:, jt * 128:(jt + 1) * 128, dc * 512:(dc + 1) * 512].rearrange("e j d -> j e d")),
                                 [tYG], [t], dma=True)
                ygpf = Prefetch(ygr, [(lambda t, dc=dc, isctx=isctx: yg_loader(t, dc, isctx)) for (_t0, _n, isctx) in tchunks for dc in range(4)], 1)
                m4units = [(t0 // 128 + ti, dc) for (t0, n, isctx) in tchunks for dc in range(4) for ti in range(n // 128)]
                x4pf = Prefetch(xring, [(lambda t, i=i, dc=dc: S.op('sp', lambda e: e.dma_start(out=t.ap[:], in_=XR[i * 128:(i + 1) * 128, dc * 512:(dc + 1) * 512]),
                                                                     [tXR[i]], [t], dma=True)) for (i, dc) in m4units], 2)
                for (t0, n, isctx) in tchunks:
                    posb = posr.next()
                    S.op('sp', lambda e: e.dma_start(out=posb.ap[:, :, 0:n], in_=POS[:, t0:t0 + n].partition_broadcast(128)), [tPOS], [posb], dma=True)
                    np_ = 32 if isctx else 128
                    for ex in range(NE):
                        for jt in range(1 if isctx else 2):
                            S.op('dve',
                                 lambda e: e.tensor_scalar(out=selTs[ex].ap[0:np_, jt, 0:n], in0=posb.ap[0:np_, ex, 0:n],
                                                           scalar1=Csb['iota_part'].ap[0:np_, jt:jt + 1], scalar2=None, op0=ALU.is_equal),
                                 [posb, Csb['iota_part']], [selTs[ex]])
                    for dc in range(4):
                        ygc = ygpf.get(tchunks.index((t0, n, isctx)) * 4 + dc)
                        for ti in range(n // 128):
                            i = t0 // 128 + ti
                            which = 1 if isctx else 0
                            ps = psring.next()
                            pairs = [(ex, jt) for ex in range(NE) for jt in range(1 if isctx else 2)]
                            for idx, (ex, jt) in enumerate(pairs):
                                S.op('pe', lambda e: e.matmul(ps.ap[:], lhsT=selTs[ex].ap[0:np_, jt, ti * 128:(ti + 1) * 128], rhs=ygc.ap[0:np_, ex, jt, :],
                                                              start=(idx == 0), stop=(idx == len(pairs) - 1)), [selTs[ex], ygc], [ps])
                            xt = x4pf.get(m4units.index((i, dc)))
                            tm = tring.next()
                            S.op('dve', lambda e: e.tensor_tensor(out=tm.ap[:], in0=ps.ap[:], in1=g_bc[which].ap[:, dc * 512:(dc + 1) * 512], op=ALU.mult),
                                 [ps, g_bc[which]], [tm])
                            S.op('pool', lambda e: e.tensor_tensor(out=tm.ap[:], in0=tm.ap[:], in1=xt.ap[:], op=ALU.add), [tm, xt], [tm])
                            S.op('sp', lambda e: e.dma_start(out=XR[i * 128:(i + 1) * 128, dc * 512:(dc + 1) * 512], in_=tm.ap[:]), [tm], [tXR[i]], dma=True)
                S.barrier()

        S.barrier()
        for li in layers:
            kind = li % 3
            j = li // 3
            with_ctx = li < DEPTH - 1
            if stop == 'setup':
                break
            if li not in mod_done:
                phase_mod(li)
            if stop == 'mod':
                break
            if debug != 'mixer_skip':
                phase_norm(li, 0)
                if stop == 'norm1':
                    break
                if kind == 0:
                    phase_attn(li, j, with_ctx)
                elif kind == 1 and 'ret' in IMPLEMENTED:
                    phase_ret(li, j, with_ctx)
                elif kind == 2 and 'hy' in IMPLEMENTED:
                    phase_hyena(li, j, with_ctx)
                else:
                    pass
            if debug != 'moe_skip':
                import os as _os
                phase_norm(li, 1, want_tm=_os.environ.get("KTM", "1") == "1", router=_os.environ.get("KRT", "1") == "1")
                if stop == 'norm2':
                    break
                phase_moe(li, with_ctx)
        phase_norm(0, 0, final=True)
        S.barrier()
    print("instructions:", S.ninst)
    return nc, consts


_W_KEYS = ['c_ctx', 'w_mod', 'b_mod', 'norm_w', 'attn_w_qkv', 'attn_q_norm', 'attn_k_norm', 'attn_w_o',
           'ret_w_in', 'ret_decay_logit', 'ret_w_o',
           'hy_w_in', 'hy_conv_w', 'hy_conv_b', 'hy_f_w1', 'hy_f_b1', 'hy_f_freq1', 'hy_f_w2', 'hy_f_b2', 'hy_f_freq2',
           'hy_f_w3', 'hy_skip', 'hy_w_out',
           'moe_router', 'moe_w_gate', 'moe_w_up', 'moe_w_down', 'final_norm_w']


def run(inputs, layers=(0, 1, 2, 3), debug=None, cores=8, stop=None, small=None):
    nc, consts = build(layers, debug, stop, small)
    in_maps = []
    shared = {k: np.ascontiguousarray(np.asarray(inputs[k], dtype=np.float32)) for k in _W_KEYS}
    if small:
        dd = small.get('depth', DEPTH)
        ne = small.get('ne', NE)
        shared['w_mod'] = np.ascontiguousarray(shared['w_mod'][:dd])
        for k in ('moe_w_gate', 'moe_w_up', 'moe_w_down'):
            shared[k] = np.ascontiguousarray(shared[k][:dd, :ne])
    for k, v in consts.items():
        shared['k_' + k] = v
    for b in range(cores):
        m = dict(shared)
        m['x'] = np.ascontiguousarray(inputs['x'][b])
        m['c'] = np.ascontiguousarray(inputs['c'][b])
        m['ctx'] = np.ascontiguousarray(inputs['ctx'][b])
        in_maps.append(m)
    res = run_bass_kernel_spmd(nc, in_maps, core_ids=list(range(cores)))
    return np.stack([r['out'] for r in res.results], axis=0)


def kernel(**inputs):
    return run(inputs).astype(np.float32)
```

```python
import contextlib
import math
import os as _os
import numpy as np
import ml_dtypes
import concourse.bass as bass
import concourse.mybir as mybir
from concourse.bass_utils import run_bass_kernel_spmd

F32 = mybir.dt.float32
BF16 = mybir.dt.bfloat16
AF = mybir.ActivationFunctionType
ALU = mybir.AluOpType
AX = mybir.AxisListType

D = 2048
TL = 2048
TC = 256
TT = TL + TC
NT = TT // 128
KT = D // 128
DEPTH = 4
EPS = 1e-6
NE = 16
CAPL = 256
CAPC = 32
FF = 1024
ENGS = ('pe', 'act', 'dve', 'pool', 'sp')
IMPLEMENTED = {'ret', 'hy'}
MOD_OVERLAP = True


class T:
    __slots__ = ('ap', 'w', 'r', 'name')

    def __init__(self, ap, name=''):
        self.ap = ap
        self.w = None
        self.r = {}
        self.name = name


class Sched:
    def __init__(self, nc, n_dma_sems=(48, 48)):
        self.nc = nc
        self.cnt = {e: 0 for e in ENGS}
        self.waited = {e: {} for e in ENGS}
        self.ndma = {'sp': n_dma_sems[0], 'pool': n_dma_sems[1]}
        self.dma_i = {'sp': 0, 'pool': 0}
        self.dma_cnt = {}
        self.sems = {}
        self.ninst = 0
        self.eng = {'pe': nc.tensor, 'act': nc.scalar, 'dve': nc.vector, 'pool': nc.gpsimd, 'sp': nc.sync}

    def alloc_sems(self, st):
        for e in ENGS:
            self.sems[e] = st.enter_context(self.nc.semaphore("s_" + e))
        for q in ('sp', 'pool'):
            for i in range(self.ndma[q]):
                self.sems[(q, i)] = st.enter_context(self.nc.semaphore(f"s_{q}{i}"))

    @staticmethod
    def _need(need, tok):
        if tok is None:
            return
        k, v = tok
        if need.get(k, 0) < v:
            need[k] = v

    def op(self, eng, fn, reads=(), writes=(), dma=False):
        need = {}
        for t in reads:
            self._need(need, t.w)
        for t in writes:
            self._need(need, t.w)
            for k, v in t.r.items():
                self._need(need, (k, v))
        if dma:
            i = self.dma_i[eng]
            self.dma_i[eng] = i + 1
            key = (eng, i % self.ndma[eng])
            prev = self.dma_cnt.get(key, 0)
            if prev:
                self._need(need, (key, prev))
            val = prev + 16
            self.dma_cnt[key] = val
            tok = (key, val)
            inc = 16
        else:
            self.cnt[eng] += 1
            key = eng
            tok = (eng, self.cnt[eng])
            inc = 1
        wd = self.waited[eng]
        eo = self.eng[eng]
        for k, v in need.items():
            if eng == 'pe' and k == 'pe':
                continue
            if wd.get(k, 0) >= v:
                continue
            wd[k] = v
            eo.wait_ge(self.sems[k], v)
        fn(eo).then_inc(self.sems[key], inc)
        self.ninst += 1
        for t in reads:
            if t.r.get(tok[0], 0) < tok[1]:
                t.r[tok[0]] = tok[1]
        for t in writes:
            t.w = tok
            t.r = {}
        return tok

    def barrier(self, engs=ENGS):
        toks = {}
        for e in ENGS:
            if e != 'sp' and self.cnt[e] > 0:
                toks[e] = self.cnt[e]
        for k, v in self.dma_cnt.items():
            toks[k] = v
        for e in engs:
            wd = self.waited[e]
            for k, v in toks.items():
                if wd.get(k, 0) >= v:
                    continue
                wd[k] = v
                self.eng[e].wait_ge(self.sems[k], v)


class Prefetch:
    def __init__(self, ring, loaders, depth):
        self.ring = ring
        self.loaders = loaders
        self.depth = depth
        self.tiles = [None] * len(loaders)
        self.nxt = 0

    def get(self, i):
        while self.nxt < len(self.loaders) and self.nxt <= i + self.depth:
            t = self.ring.next()
            self.loaders[self.nxt](t)
            self.tiles[self.nxt] = t
            self.nxt += 1
        return self.tiles[i]


class Ring:
    def __init__(self, tiles):
        self.tiles = tiles
        self.i = 0

    def next(self):
        t = self.tiles[self.i % len(self.tiles)]
        self.i += 1
        return t


def _bf(a):
    return np.ascontiguousarray(a.astype(ml_dtypes.bfloat16))


def make_consts():
    c = {}
    c['ident_f'] = np.eye(128, dtype=np.float32)
    c['ident_b'] = _bf(np.eye(128, dtype=np.float32))
    c['ones_f'] = np.ones((128, 128), np.float32)
    c['ones_b'] = _bf(np.ones((128, 128), np.float32))
    c['iota_row'] = np.tile(np.arange(256, dtype=np.float32)[None, :], (128, 1))
    ip = np.zeros((128, 4), np.float32)
    ip[:, 0] = np.arange(128)
    ip[:, 1] = np.arange(128) + 128
    c['iota_part'] = ip
    t = np.arange(TL)
    row = (t // 64).astype(np.float64)
    col = (t % 64).astype(np.float64)

    def rope_tables(hd):
        nf = hd // 4
        inv = 10000.0 ** (-np.arange(nf, dtype=np.float64) / nf)
        cosT = np.zeros((hd, TL), np.float32)
        sinT = np.zeros((hd, TL), np.float32)
        perm = np.zeros((hd, hd), np.float32)
        for d in range(hd):
            axis = d // (2 * nf)
            half = (d % (2 * nf)) // nf
            f = d % nf
            pos = row if axis == 0 else col
            ang = (pos.astype(np.float32) * np.float32(inv[f])).astype(np.float32)
            cosT[d] = np.cos(ang)
            sinT[d] = np.sin(ang) * (-1.0 if half == 0 else 1.0)
            partner = d + nf if half == 0 else d - nf
            perm[partner, d] = 1.0
        return cosT, sinT, perm
    ca, sa, pa = rope_tables(128)
    c['cosA'] = ca
    c['sinA'] = sa
    c['permA'] = pa
    cr, sr, pr = rope_tables(256)
    c['cosR0'] = np.ascontiguousarray(cr[0:128])
    c['cosR1'] = np.ascontiguousarray(cr[128:256])
    c['sinR0'] = np.ascontiguousarray(sr[0:128])
    c['sinR1'] = np.ascontiguousarray(sr[128:256])
    c['permR'] = np.ascontiguousarray(pr[0:128, 0:128])
    def hy_consts(L, tag):
        n = 2 * L
        nkt = L // 128
        tl = np.linspace(0.0, 1.0, L, dtype=np.float32)
        bands = 16
        w = (2.0 * math.pi * np.arange(L, dtype=np.float32) / L).astype(np.float32)
        f = np.linspace(1e-4, bands - 1, bands, dtype=np.float32)
        z = np.concatenate([tl[:, None], np.cos(f[None, :] * w[:, None]), -np.sin(f[None, :] * w[:, None])], axis=-1).astype(np.float32)
        c['hyZ' + tag] = np.ascontiguousarray(z.T)
        deltas = np.abs(np.linspace(math.log(1e-2) / 1.5, math.log(1e-2) / 0.3, D, dtype=np.float32))
        c['hyDecay' + tag] = np.exp(-tl[:, None] * deltas[None, :]).astype(np.float32)
        k = np.arange(L, dtype=np.int64)
        ff = np.arange(L, dtype=np.int64)
        m = ((2 * ff[None, :] + 1) * k[:, None]) % (2 * n)
        ang = m.astype(np.float64) * (math.pi / n)
        CT = np.cos(ang)
        ST = np.sin(ang)
        tcw = min(512, L)
        ntc = L // tcw

        def tile_T(M):
            return _bf(M.reshape(nkt, 128, nkt, 128).transpose(2, 1, 0, 3).reshape(nkt, 128, nkt * 128))

        def tile_F(M):
            return _bf(M.reshape(ntc, tcw, nkt, 128).transpose(0, 3, 2, 1).reshape(ntc, 128, nkt * tcw))
        c['hyCT' + tag] = tile_T(CT)
        c['hyST' + tag] = tile_T(ST)
        c['hyCF' + tag] = tile_F(CT)
        c['hySF' + tag] = tile_F(ST)
    hy_consts(TL, 'L')
    hy_consts(TC, 'C')
    p_ = np.arange(128, dtype=np.float32)[:, None]
    c['retDelta'] = np.ascontiguousarray(np.arange(3968, dtype=np.float32)[None, :] - p_ - 1920.0)
    y_ = np.arange(2176, dtype=np.float32)[None, :]
    c['retEf'] = np.ascontiguousarray(y_ - p_ + 128.0)
    c['retEb'] = np.ascontiguousarray(p_ - y_ + 2176.0)
    return c


class KB:
    pass


def build(layers=(0, 1, 2, 3), debug=None, stop=None, small=None):
    nc = bass.Bass("TRN2", target_bir_lowering=False)
    S = Sched(nc)
    consts = make_consts()

    def din(name, shape, dt=F32):
        return nc.dram_tensor(name, list(shape), dt, kind="ExternalInput").ap()

    def dscr(name, shape, dt):
        return nc.dram_tensor(name, list(shape), dt, kind="Internal").ap()

    I = {}
    I['x'] = din('x', [TL, D])
    I['c'] = din('c', [D])
    I['ctx'] = din('ctx', [TC, D])
    I['c_ctx'] = din('c_ctx', [D])
    small = small or {}
    _DD = small.get('depth', DEPTH)
    _NEW = small.get('ne', NE)
    I['w_mod'] = din('w_mod', [_DD, D, 6 * D])
    I['b_mod'] = din('b_mod', [DEPTH, 6 * D])
    I['norm_w'] = din('norm_w', [DEPTH, 2, D])
    I['attn_w_qkv'] = din('attn_w_qkv', [2, D, 3072])
    I['attn_q_norm'] = din('attn_q_norm', [2, 128])
    I['attn_k_norm'] = din('attn_k_norm', [2, 128])
    I['attn_w_o'] = din('attn_w_o', [2, D, D])
    I['ret_w_in'] = din('ret_w_in', [1, D, 12288])
    I['ret_decay_logit'] = din('ret_decay_logit', [1, 2, 8])
    I['ret_w_o'] = din('ret_w_o', [1, 4096, D])
    I['hy_w_in'] = din('hy_w_in', [1, D, 3 * D])
    I['hy_conv_w'] = din('hy_conv_w', [1, 3, 3 * D])
    I['hy_conv_b'] = din('hy_conv_b', [1, 3 * D])
    I['hy_f_w1'] = din('hy_f_w1', [1, 33, 64])
    I['hy_f_b1'] = din('hy_f_b1', [1, 64])
    I['hy_f_freq1'] = din('hy_f_freq1', [1, 64])
    I['hy_f_w2'] = din('hy_f_w2', [1, 64, 64])
    I['hy_f_b2'] = din('hy_f_b2', [1, 64])
    I['hy_f_freq2'] = din('hy_f_freq2', [1, 64])
    I['hy_f_w3'] = din('hy_f_w3', [1, 64, 2 * D])
    I['hy_skip'] = din('hy_skip', [1, D])
    I['hy_w_out'] = din('hy_w_out', [1, D, D])
    I['moe_router'] = din('moe_router', [DEPTH, D, NE])
    I['moe_w_gate'] = din('moe_w_gate', [_DD, _NEW, D, FF])
    I['moe_w_up'] = din('moe_w_up', [_DD, _NEW, D, FF])
    I['moe_w_down'] = din('moe_w_down', [_DD, _NEW, FF, D])
    I['final_norm_w'] = din('final_norm_w', [D])
    CI = {}
    for k, v in consts.items():
        CI[k] = din('k_' + k, v.shape, BF16 if v.dtype == ml_dtypes.bfloat16 else F32)
    OUT = nc.dram_tensor('out', [TL, D], F32, kind="ExternalOutput").ap()

    XR = dscr('XR', [TT, D], F32)
    HT = dscr('HT', [KT, 128, TT], BF16)
    H2TM = dscr('H2TM', [TT, D], BF16)
    MODS = [dscr('MOD0', [2, 6 * D], F32), dscr('MOD1', [2, 6 * D], F32)]
    QT = dscr('QT', [16, 128, TT], BF16)
    KTs = dscr('KTs', [4, 128, TT], BF16)
    Vs = dscr('Vs', [TT, 512], BF16)
    OT = dscr('OT', [32, 128, TT], BF16)
    RK = dscr('RK', [16, 128, TT], BF16)
    RG = dscr('RG', [32, 128, TT], BF16)
    RV = dscr('RV', [TT, 4096], BF16)
    UTM = dscr('UTM', [TT, D], BF16)
    UTF = dscr('UTF', [KT, 128, TT], F32)
    X0F = dscr('X0F', [KT, 128, TT], F32)
    HPM = dscr('HPM', [2, TL, D], BF16)
    HSPEC = {'L': dscr('HSPECL', [2, TL, D], F32), 'C': dscr('HSPECC', [2, TC, D], F32)}
    SKIPT = dscr('SKIPT', [128, KT], F32)
    POS = dscr('POS', [NE, TT], F32)
    YG = dscr('YG', [NE, 288, D], BF16)

    dT = {}

    def dt_(name, ap):
        dT[name] = T(ap, name)
        return dT[name]
    tXR = [dt_(f'XR{i}', XR) for i in range(NT)]
    tHT = dt_('HT', HT)
    tH2 = dt_('H2TM', H2TM)
    tMODS = [dt_('MOD0', MODS[0]), dt_('MOD1', MODS[1])]
    tQT = dt_('QT', QT)
    tKT = dt_('KTs', KTs)
    tV = dt_('Vs', Vs)
    tOT = dt_('OT', OT)
    tRK = dt_('RK', RK)
    tRG = dt_('RG', RG)
    tRV = dt_('RV', RV)
    tUTM = dt_('UTM', UTM)
    tUTF = dt_('UTF', UTF)
    tX0F = dt_('X0F', X0F)
    tHPM = dt_('HPM', HPM)
    tHSPEC = {'L': dt_('HSPECL', HSPEC['L']), 'C': dt_('HSPECC', HSPEC['C'])}
    tSKIPT = dt_('SKIPT', SKIPT)
    tPOS = dt_('POS', POS)
    tYG = dt_('YG', YG)
    tIN = T(None, 'inputs')
    tOUT = dt_('OUT', OUT)

    with contextlib.ExitStack() as top:
        S.alloc_sems(top)

        def sb(stk, name, shape, dt):
            return T(top_or(stk).enter_context(nc.sbuf_tensor(name, list(shape), dt)), name)

        def top_or(stk):
            return stk if stk is not None else top

        uid = [0]

        def nm(p):
            uid[0] += 1
            return f"{p}_{uid[0]}"

        PS = [T(top.enter_context(nc.psum_tensor(f"ps{i}", [128, 512], F32)), f"ps{i}") for i in range(7)]
        PSB = T(top.enter_context(nc.psum_tensor("psb", [128, 1024], BF16)), "psb")
        psring = Ring(PS)

        Csb = {}
        for k in ('ident_f', 'ident_b', 'ones_f', 'ones_b', 'iota_row', 'iota_part', 'permA', 'permR'):
            v = consts[k]
            Csb[k] = sb(None, 'c_' + k, v.shape, BF16 if v.dtype == ml_dtypes.bfloat16 else F32)
            S.op('sp', lambda e, k=k: e.dma_start(out=Csb[k].ap[:], in_=CI[k]), [tIN], [Csb[k]], dma=True)
        ones_col = sb(None, 'ones_col', [128, 1], F32)
        S.op('dve', lambda e: e.memset(ones_col.ap[:], 1.0), [], [ones_col])
        craw = sb(None, 'craw', [128, KT, 2], F32)
        sT = sb(None, 'sT', [128, KT, 2], BF16)
        S.op('sp', lambda e: e.dma_start(out=craw.ap[:, :, 0], in_=I['c'].rearrange("(kt p) -> p kt", p=128),
                                         allow_slow_non_contiguous=True), [tIN], [craw], dma=True)
        S.op('sp', lambda e: e.dma_start(out=craw.ap[:, :, 1], in_=I['c_ctx'].rearrange("(kt p) -> p kt", p=128),
                                         allow_slow_non_contiguous=True), [tIN], [craw], dma=True)
        S.op('act', lambda e: e.activation(out=sT.ap[:], in_=craw.ap[:], func=AF.Silu), [craw], [sT])
        probs_tm = sb(None, 'probs_tm', [128, NT, NE], F32)
        probsT = sb(None, 'probsT', [NE, TT], F32)
        posTM = sb(None, 'posTM', [128, NT, NE], F32)
        GHL = sb(None, 'GHL', [128, NT, NE, 2], BF16)

        for i in range(NT):
            src = I['ctx'][i * 128:(i + 1) * 128, :] if i < 2 else I['x'][(i - 2) * 128:(i - 1) * 128, :]
            S.op('sp', lambda e: e.dma_start(out=XR[i * 128:(i + 1) * 128, :], in_=src), [tIN], [tXR[i]], dma=True)

        def bcast_load(stk, name, vec_ap):
            n = vec_ap.shape[-1]
            t = sb(stk, nm(name), [128, n], F32)
            S.op('sp', lambda e: e.dma_start(out=t.ap[:], in_=vec_ap.partition_broadcast(128)), [tMODS[0], tMODS[1], tIN], [t], dma=True)
            return t

        mod_done = set()

        def phase_mod(li, stk=None):
            mod_done.add(li)
            MOD = MODS[li % 2]
            tM = tMODS[li % 2]
            own = stk is None
            ph = contextlib.ExitStack() if own else stk
            wring = Ring([sb(ph, nm('wm'), [128, KT, 512], BF16) for _ in range(3)])
            bring = Ring([sb(ph, nm('bm'), [1, 512], F32) for _ in range(3)])
            oring = Ring([sb(ph, nm('om'), [2, 512], F32) for _ in range(3)])
            wsrc = I['w_mod'][li].rearrange("(kt p) n -> p kt n", p=128)
            wpf = Prefetch(wring, [(lambda t, cch=cch: S.op('pool', lambda e: e.dma_start(out=t.ap[:], in_=wsrc[:, :, cch * 512:(cch + 1) * 512]),
                                                             [tIN], [t], dma=True)) for cch in range(24)], 2)
            for cch in range(24):
                w = wpf.get(cch)
                br = bring.next()
                S.op('sp', lambda e: e.dma_start(out=br.ap[:], in_=I['b_mod'][li, cch * 512:(cch + 1) * 512].rearrange("(o n) -> o n", o=1)),
                     [tIN], [br], dma=True)
                ps = psring.next()
                S.op('pe', lambda e: e.matmul(ps.ap[:, :], lhsT=Csb['ones_f'].ap[0:1, :], rhs=br.ap[0:1, :], start=True, stop=False),
                     [Csb['ones_f'], br], [ps])
                for kt in range(KT):
                    S.op('pe', lambda e: e.matmul(ps.ap[0:2, :], lhsT=sT.ap[:, kt, :], rhs=w.ap[:, kt, :],
                                                  start=False, stop=(kt == KT - 1)), [sT, w], [ps])
                om = oring.next()
                S.op('act', lambda e: e.copy(out=om.ap[:], in_=ps.ap[0:2, :]), [ps], [om])
                S.op('sp', lambda e: e.dma_start(out=MOD[:, cch * 512:(cch + 1) * 512], in_=om.ap[:]), [om], [tM], dma=True)
            if own:
                S.barrier()
                ph.close()

        def phase_norm(li, k, want_tm=False, router=False, final=False):
            MOD = MODS[li % 2]
            with contextlib.ExitStack() as ph:
                if final:
                    nw = bcast_load(ph, 'nw', I['final_norm_w'])
                    A_bc = [nw, nw]
                    B_bc = [None, None]
                else:
                    nw = bcast_load(ph, 'nw', I['norm_w'][li, k])
                    A_bc, B_bc = [], []
                    for which in (0, 1):
                        sc = bcast_load(ph, 'sc', MOD[which, (3 * k + 1) * D:(3 * k + 2) * D])
                        S.op('dve', lambda e: e.scalar_tensor_tensor(out=sc.ap[:], in0=sc.ap[:], scalar=1.0, in1=nw.ap[:],
                                                                     op0=ALU.add, op1=ALU.mult), [sc, nw], [sc])
                        A_bc.append(sc)
                        B_bc.append(bcast_load(ph, 'sh', MOD[which, (3 * k) * D:(3 * k + 1) * D]))
                if router:
                    wr = sb(ph, nm('wr'), [128, KT, NE], F32)
                    for kt in range(KT):
                        S.op('sp', lambda e: e.dma_start(out=wr.ap[:, kt, :], in_=I['moe_router'][li, kt * 128:(kt + 1) * 128, :]),
                             [tIN], [wr], dma=True)
                ptile = sb(ph, nm('ptile'), [128, 128], F32)
                S.op('dve', lambda e: e.memset(ptile.ap[:], 0.0), [], [ptile])
                xring = Ring([sb(ph, nm('xt'), [128, D], F32) for _ in range(2)])
                hring = Ring([sb(ph, nm('h'), [128, D], F32) for _ in range(2)])
                hbring = Ring([sb(ph, nm('hb'), [128, D], BF16) for _ in range(2)])
                junk = sb(ph, nm('junk'), [128, D], BF16)
                htring = Ring([sb(ph, nm('htb'), [128, 4, 128], BF16) for _ in range(4)])
                hfring = Ring([sb(ph, nm('htf'), [128, KT, 128], F32) for _ in range(2)])
                stat = Ring([sb(ph, nm('st'), [128, 4], F32) for _ in range(3)])
                tiles = range(2, NT) if final else range(NT)
                for i in tiles:
                    which = 1 if i < 2 else 0
                    xt = xring.next()
                    S.op('sp', lambda e: e.dma_start(out=xt.ap[:], in_=XR[i * 128:(i + 1) * 128, :]), [tXR[i]], [xt], dma=True)
                    s4 = stat.next()
                    S.op('act', lambda e: e.activation(out=junk.ap[:], in_=xt.ap[:], func=AF.Square, accum_out=s4.ap[:, 0:1]),
                         [xt], [junk, s4])
                    S.op('dve', lambda e: e.tensor_scalar(out=s4.ap[:, 1:2], in0=s4.ap[:, 0:1], scalar1=1.0 / D, scalar2=EPS,
                                                          op0=ALU.mult, op1=ALU.add), [s4], [s4])
                    S.op('act', lambda e: e.activation(out=s4.ap[:, 2:3], in_=s4.ap[:, 1:2], func=AF.Sqrt), [s4], [s4])
                    S.op('dve', lambda e: e.reciprocal(out=s4.ap[:, 3:4], in_=s4.ap[:, 2:3]), [s4], [s4])
                    h = hring.next()
                    S.op('dve', lambda e: e.scalar_tensor_tensor(out=h.ap[:], in0=xt.ap[:], scalar=s4.ap[:, 3:4], in1=A_bc[which].ap[:],
                                                                 op0=ALU.mult, op1=ALU.mult), [xt, s4, A_bc[which]], [h])
                    if final:
                        S.op('sp', lambda e: e.dma_start(out=OUT[(i - 2) * 128:(i - 1) * 128, :], in_=h.ap[:]), [h], [tOUT], dma=True)
                        continue
                    S.op('pool', lambda e: e.tensor_tensor(out=h.ap[:], in0=h.ap[:], in1=B_bc[which].ap[:], op=ALU.add),
                         [h, B_bc[which]], [h])
                    if want_tm:
                        hb = hbring.next()
                        S.op('act', lambda e: e.copy(out=hb.ap[:], in_=h.ap[:]), [h], [hb])
                        S.op('sp', lambda e: e.dma_start(out=H2TM[i * 128:(i + 1) * 128, :], in_=hb.ap[:]), [hb], [tH2], dma=True)
                    hf = hfring.next() if router else None
                    for g in range(4):
                        ps = psring.next()
                        for j in range(4):
                            kt = g * 4 + j
                            S.op('pe', lambda e: e.transpose(out=ps.ap[:, j * 128:(j + 1) * 128], in_=h.ap[:, kt * 128:(kt + 1) * 128],
                                                             identity=Csb['ident_f'].ap[:]), [h, Csb['ident_f']], [ps])
                        hb4 = htring.next()
                        if router:
                            S.op('act', lambda e: e.copy(out=hf.ap[:, g * 4:(g + 1) * 4, :], in_=ps.ap[:].rearrange("p (a b) -> p a b", b=128)),
                                 [ps], [hf])
                            S.op('dve', lambda e: e.tensor_copy(out=hb4.ap[:], in_=hf.ap[:, g * 4:(g + 1) * 4, :]), [hf], [hb4])
                        else:
                            S.op('dve', lambda e: e.tensor_copy(out=hb4.ap[:].rearrange("p a b -> p (a b)"), in_=ps.ap[:]), [ps], [hb4])
                        S.op('sp', lambda e: e.dma_start(out=HT[g * 4:(g + 1) * 4, :, i * 128:(i + 1) * 128].rearrange("k p t -> p k t"),
                                                         in_=hb4.ap[:]), [hb4], [tHT], dma=True)
                    if router and _os.environ.get("KRT3", "0") != "1":
                        ps = psring.next()
                        for kt in range(KT):
                            S.op('pe', lambda e: e.matmul(ps.ap[:, 0:NE], lhsT=hf.ap[:, kt, :], rhs=wr.ap[:, kt, :],
                                                          start=(kt == 0), stop=(kt == KT - 1)), [hf, wr], [ps])
                        s5 = stat.next()
                        S.op('dve', lambda e: e.reduce_max(out=s5.ap[:, 0:1], in_=ps.ap[:, 0:NE], axis=AX.X), [ps], [s5])
                        S.op('dve', lambda e: e.tensor_scalar(out=s5.ap[:, 1:2], in0=s5.ap[:, 0:1], scalar1=-1.0, scalar2=None,
                                                              op0=ALU.mult), [s5], [s5])
                        S.op('act', lambda e: e.activation(out=probs_tm.ap[:, i, :], in_=ps.ap[:, 0:NE], func=AF.Exp,
                                                           bias=s5.ap[:, 1:2], scale=1.0, accum_out=s5.ap[:, 2:3]),
                             [ps, s5], [probs_tm, s5])
                        S.op('dve', lambda e: e.reciprocal(out=s5.ap[:, 3:4], in_=s5.ap[:, 2:3]), [s5], [s5])
                        S.op('dve', lambda e: e.tensor_scalar(out=probs_tm.ap[:, i, :], in0=probs_tm.ap[:, i, :], scalar1=s5.ap[:, 3:4],
                                                              scalar2=None, op0=ALU.mult), [probs_tm, s5], [probs_tm])
                        ps2 = psring.next()
                        if _os.environ.get("KRT2", "0") == "1":
                            continue
                        S.op('dve', lambda e: e.tensor_copy(out=ptile.ap[:, 0:NE], in_=probs_tm.ap[:, i, :]), [probs_tm], [ptile])
                        S.op('pe', lambda e: e.transpose(out=ps2.ap[:, 0:128], in_=ptile.ap[:], identity=Csb['ident_f'].ap[:]),
                             [ptile, Csb['ident_f']], [ps2])
                        S.op('act', lambda e: e.copy(out=probsT.ap[:, i * 128:(i + 1) * 128], in_=ps2.ap[0:NE, 0:128]), [ps2], [probsT])
                S.barrier()

        def load_fm(stk, name, src, tsrc, nkt, t0, n):
            t = sb(stk, nm(name), [128, nkt, n], BF16)
            for kt in range(nkt):
                S.op('sp', lambda e: e.dma_start(out=t.ap[:, kt, :], in_=src[kt, :, t0:t0 + n]), [tsrc], [t], dma=True)
            return t

        def phase_proj_residual(li, src, tsrc, nkt, W, gate_k, with_ctx):
            MOD = MODS[li % 2]
            wsrc = W.rearrange("(kt p) n -> p kt n", p=128)
            ngrp = 1 if nkt <= 16 else 2
            tile_lo = 0 if with_ctx else 2
            per = (NT - tile_lo + ngrp - 1) // ngrp
            for gi in range(ngrp):
                tl = list(range(tile_lo + gi * per, min(NT, tile_lo + (gi + 1) * per)))
                with contextlib.ExitStack() as ph:
                    g_bc = [bcast_load(ph, 'g', MOD[which, (3 * gate_k + 2) * D:(3 * gate_k + 3) * D]) for which in (0, 1)]
                    t0 = tl[0] * 128
                    a = load_fm(ph, 'a', src, tsrc, nkt, t0, len(tl) * 128)
                    nwb = 3 if nkt <= 16 else 2
                    wring = Ring([sb(ph, nm('wo'), [128, nkt, 512], BF16) for _ in range(nwb)])
                    xring = Ring([sb(ph, nm('xo'), [128, 512], F32) for _ in range(4)])
                    tring = Ring([sb(ph, nm('to'), [128, 512], F32) for _ in range(3)])
                    wpf = Prefetch(wring, [(lambda t, cch=cch: S.op('pool', lambda e: e.dma_start(out=t.ap[:], in_=wsrc[:, :, cch * 512:(cch + 1) * 512]),
                                                                     [tIN], [t], dma=True)) for cch in range(4)], nwb - 1)
                    units = [(cch, i) for cch in range(4) for i in tl]
                    xpf = Prefetch(xring, [(lambda t, cch=cch, i=i: S.op('sp', lambda e: e.dma_start(out=t.ap[:], in_=XR[i * 128:(i + 1) * 128, cch * 512:(cch + 1) * 512]),
                                                                         [tXR[i]], [t], dma=True)) for (cch, i) in units], 2)
                    for cch in range(4):
                        w = wpf.get(cch)
                        for i in tl:
                            which = 1 if i < 2 else 0
                            ps = psring.next()
                            c0 = i * 128 - t0
                            for kt in range(nkt):
                                S.op('pe', lambda e: e.matmul(ps.ap[:], lhsT=a.ap[:, kt, c0:c0 + 128], rhs=w.ap[:, kt, :],
                                                              start=(kt == 0), stop=(kt == nkt - 1)), [a, w], [ps])
                            xt = xpf.get(units.index((cch, i)))
                            tm = tring.next()
                            S.op('dve', lambda e: e.tensor_tensor(out=tm.ap[:], in0=ps.ap[:], in1=g_bc[which].ap[:, cch * 512:(cch + 1) * 512],
                                                                  op=ALU.mult), [ps, g_bc[which]], [tm])
                            S.op('pool', lambda e: e.tensor_tensor(out=tm.ap[:], in0=tm.ap[:], in1=xt.ap[:], op=ALU.add), [tm, xt], [tm])
                            S.op('sp', lambda e: e.dma_start(out=XR[i * 128:(i + 1) * 128, cch * 512:(cch + 1) * 512], in_=tm.ap[:]),
                                 [tm], [tXR[i]], dma=True)
                    S.barrier()

        def phase_attn(li, j, with_ctx):
            Wqkv = I['attn_w_qkv'][j]
            wsrc = Wqkv.rearrange("(kt p) n -> p kt n", p=128)
            with contextlib.ExitStack() as ph:
                hT = load_fm(ph, 'hT', HT, tHT, KT, 0, TT)
                cosA = sb(ph, nm('cosA'), [128, TL], F32)
                sinA = sb(ph, nm('sinA'), [128, TL], F32)
                S.op('sp', lambda e: e.dma_start(out=cosA.ap[:], in_=CI['cosA']), [tIN], [cosA], dma=True)
                S.op('sp', lambda e: e.dma_start(out=sinA.ap[:], in_=CI['sinA']), [tIN], [sinA], dma=True)
                nq = sb(ph, nm('nq'), [128, 2], F32)
                S.op('sp', lambda e: e.dma_start(out=nq.ap[:, 0:1], in_=I['attn_q_norm'][j].rearrange("(p o) -> p o", o=1)), [tIN], [nq], dma=True)
                S.op('sp', lambda e: e.dma_start(out=nq.ap[:, 1:2], in_=I['attn_k_norm'][j].rearrange("(p o) -> p o", o=1)), [tIN], [nq], dma=True)
                wring = Ring([sb(ph, nm('wq'), [128, KT, 128], BF16) for _ in range(3)])
                sqr = Ring([sb(ph, nm('sq'), [128, 512], F32) for _ in range(3)])
                rsr = Ring([sb(ph, nm('rs'), [128, 512], F32) for _ in range(3)])
                qnr = Ring([sb(ph, nm('qn'), [128, 512], F32) for _ in range(4)])
                epsc = sb(ph, nm('epsc'), [128, 1], F32)
                S.op('dve', lambda e: e.memset(epsc.ap[:], EPS), [], [epsc])
                t1r = Ring([sb(ph, nm('t1'), [128, 512], F32) for _ in range(2)])
                t2r = Ring([sb(ph, nm('t2'), [128, 512], F32) for _ in range(2)])
                obr = Ring([sb(ph, nm('ob'), [128, 512], BF16) for _ in range(3)])
                chunks = [(0, 256, False)] + [(256 + c * 512, 512, True) for c in range(4)]
                q1, q2 = [], []

                def a2_tail1(psA, sq, n, col, lat, t0, cb):
                    psB = psring.next()
                    S.op('pe', lambda e: e.matmul(psB.ap[:, 0:n], lhsT=Csb['ones_f'].ap[:], rhs=sq.ap[:, 0:n], start=True, stop=True),
                         [sq, Csb['ones_f']], [psB])
                    rs = rsr.next()
                    S.op('act', lambda e: e.activation(out=rs.ap[:, 0:n], in_=psB.ap[:, 0:n], func=AF.Sqrt, bias=epsc.ap[:, 0:1], scale=1.0 / 128),
                         [psB, epsc], [rs])
                    S.op('dve', lambda e: e.reciprocal(out=rs.ap[:, 0:n], in_=rs.ap[:, 0:n]), [rs], [rs])
                    qn = qnr.next()
                    S.op('dve', lambda e: e.scalar_tensor_tensor(out=qn.ap[:, 0:n], in0=psA.ap[:, 0:n], scalar=nq.ap[:, col:col + 1],
                                                                 in1=rs.ap[:, 0:n], op0=ALU.mult, op1=ALU.mult), [psA, nq, rs], [qn])
                    return qn

                def a2_tail2(qn, n, lat, t0, cb):
                    ob = obr.next()
                    if lat:
                        psC = psring.next()
                        S.op('pe', lambda e: e.matmul(psC.ap[:, 0:n], lhsT=Csb['permA'].ap[:], rhs=qn.ap[:, 0:n], start=True, stop=True),
                             [qn, Csb['permA']], [psC])
                        l0 = t0 - TC
                        t1 = t1r.next()
                        t2 = t2r.next()
                        S.op('pool', lambda e: e.tensor_tensor(out=t1.ap[:, 0:n], in0=qn.ap[:, 0:n], in1=cosA.ap[:, l0:l0 + n], op=ALU.mult),
                             [qn, cosA], [t1])
                        S.op('dve', lambda e: e.tensor_tensor(out=t2.ap[:, 0:n], in0=psC.ap[:, 0:n], in1=sinA.ap[:, l0:l0 + n], op=ALU.mult),
                             [psC, sinA], [t2])
                        S.op('pool', lambda e: e.tensor_tensor(out=ob.ap[:, 0:n], in0=t1.ap[:, 0:n], in1=t2.ap[:, 0:n], op=ALU.add),
                             [t1, t2], [ob])
                    else:
                        S.op('act', lambda e: e.copy(out=ob.ap[:, 0:n], in_=qn.ap[:, 0:n]), [qn], [ob])
                    if cb < 16:
                        S.op('sp', lambda e: e.dma_start(out=QT[cb, :, t0:t0 + n], in_=ob.ap[:, 0:n]), [ob], [tQT], dma=True)
                    else:
                        S.op('sp', lambda e: e.dma_start(out=KTs[cb - 16, :, t0:t0 + n], in_=ob.ap[:, 0:n]), [ob], [tKT], dma=True)

                def a2_advance(flush=False):
                    while len(q1) > (0 if flush else 1):
                        (psA, sq, n, col, lat, t0, cb) = q1.pop(0)
                        qn = a2_tail1(psA, sq, n, col, lat, t0, cb)
                        q2.append((qn, n, lat, t0, cb))
                    while len(q2) > (0 if flush else 1):
                        a2_tail2(*q2.pop(0))

                wpf = Prefetch(wring, [(lambda t, cb=cb: S.op('pool', lambda e: e.dma_start(out=t.ap[:], in_=wsrc[:, :, cb * 128:(cb + 1) * 128]),
                                                                 [tIN], [t], dma=True)) for cb in range(20)], 2)
                for cb in range(20):
                    is_q = cb < 16
                    w = wpf.get(cb)
                    for (t0, n, lat) in chunks:
                        if is_q and (not lat) and (not with_ctx):
                            continue
                        psA = psring.next()
                        for kt in range(KT):
                            S.op('pe', lambda e: e.matmul(psA.ap[:, 0:n], lhsT=w.ap[:, kt, :], rhs=hT.ap[:, kt, t0:t0 + n],
                                                          start=(kt == 0), stop=(kt == KT - 1)), [w, hT], [psA])
                        sq = sqr.next()
                        S.op('act', lambda e: e.activation(out=sq.ap[:, 0:n], in_=psA.ap[:, 0:n], func=AF.Square), [psA], [sq])
                        q1.append((psA, sq, n, 0 if is_q else 1, lat, t0, cb))
                        a2_advance()
                a2_advance(flush=True)
                wv = sb(ph, nm('wv'), [128, KT, 512], BF16)
                S.op('pool', lambda e: e.dma_start(out=wv.ap[:], in_=wsrc[:, :, 2560:3072]), [tIN], [wv], dma=True)
                for i in range(NT):
                    ps = psring.next()
                    for kt in range(KT):
                        S.op('pe', lambda e: e.matmul(ps.ap[:], lhsT=hT.ap[:, kt, i * 128:(i + 1) * 128], rhs=wv.ap[:, kt, :],
                                                      start=(kt == 0), stop=(kt == KT - 1)), [hT, wv], [ps])
                    ob = obr.next()
                    S.op('act', lambda e: e.copy(out=ob.ap[:], in_=ps.ap[:]), [ps], [ob])
                    S.op('sp', lambda e: e.dma_start(out=Vs[i * 128:(i + 1) * 128, :], in_=ob.ap[:]), [ob], [tV], dma=True)
                S.barrier()
            with contextlib.ExitStack() as ph:
                scale = 128 ** -0.5
                ktr = Ring([sb(ph, nm('kt'), [128, TT], BF16) for _ in range(2)])
                vr = Ring([sb(ph, nm('v'), [128, NT, 128], BF16) for _ in range(2)])
                qr = Ring([sb(ph, nm('q'), [128, TT], BF16) for _ in range(2)])
                er = Ring([sb(ph, nm('e'), [128, 512], BF16) for _ in range(5)])
                rdr = Ring([sb(ph, nm('rd'), [128, 512], F32) for _ in range(2)])
                oor = Ring([sb(ph, nm('oo'), [128, 512], BF16) for _ in range(2)])
                psS = Ring(PS[0:3])
                psO = Ring(PS[3:5])
                psD = Ring(PS[5:7])
                LOOK = 2
                pend = []

                def emit_pv(ee, n, jt, pO, pD, idx, nk, v, t0, h_):
                    S.op('pe', lambda e: e.matmul(pO.ap[:, 0:n], lhsT=v.ap[:, jt, :], rhs=ee.ap[:, 0:n],
                                                  start=(idx == 0), stop=(idx == nk - 1)), [v, ee], [pO])
                    S.op('pe', lambda e: e.matmul(pD.ap[:, 0:n], lhsT=Csb['ones_b'].ap[:], rhs=ee.ap[:, 0:n],
                                                  start=(idx == 0), stop=(idx == nk - 1)), [Csb['ones_b'], ee], [pD])
                    if idx == nk - 1:
                        rd = rdr.next()
                        S.op('dve', lambda e: e.reciprocal(out=rd.ap[:, 0:n], in_=pD.ap[:, 0:n]), [pD], [rd])
                        oo = oor.next()
                        S.op('dve', lambda e: e.tensor_tensor(out=oo.ap[:, 0:n], in0=pO.ap[:, 0:n], in1=rd.ap[:, 0:n], op=ALU.mult),
                             [pO, rd], [oo])
                        S.op('sp', lambda e: e.dma_start(out=OT[h_, :, t0:t0 + n], in_=oo.ap[:, 0:n]), [oo], [tOT], dma=True)

                for kv in range(4):
                    kt_ = ktr.next()
                    S.op('sp', lambda e: e.dma_start(out=kt_.ap[:], in_=KTs[kv]), [tKT], [kt_], dma=True)
                    v = vr.next()
                    for i in range(NT):
                        S.op('sp', lambda e: e.dma_start(out=v.ap[:, i, :], in_=Vs[i * 128:(i + 1) * 128, kv * 128:(kv + 1) * 128]),
                             [tV], [v], dma=True)
                    for hh in range(4):
                        h_ = kv * 4 + hh
                        q = qr.next()
                        S.op('sp', lambda e: e.dma_start(out=q.ap[:], in_=QT[h_]), [tQT], [q], dma=True)
                        qchunks = [(256 + c * 512, 512, list(range(NT))) for c in range(4)]
                        if with_ctx:
                            qchunks = [(0, 256, [0, 1])] + qchunks
                        for (t0, n, ktiles) in qchunks:
                            pO = psO.next()
                            pD = psD.next()
                            for idx, jt in enumerate(ktiles):
                                pS = psS.next()
                                S.op('pe', lambda e: e.matmul(pS.ap[:, 0:n], lhsT=kt_.ap[:, jt * 128:(jt + 1) * 128], rhs=q.ap[:, t0:t0 + n],
                                                              start=True, stop=True), [kt_, q], [pS])
                                ee = er.next()
                                S.op('act', lambda e: e.activation(out=ee.ap[:, 0:n], in_=pS.ap[:, 0:n], func=AF.Exp, scale=scale), [pS], [ee])
                                pend.append((ee, n, jt, pO, pD, idx, len(ktiles), v, t0, h_))
                                if len(pend) > LOOK:
                                    emit_pv(*pend.pop(0))
                while pend:
                    emit_pv(*pend.pop(0))
                S.barrier()
            phase_proj_residual(li, OT, tOT, 16, I['attn_w_o'][j], 0, with_ctx)


        def phase_ret(li, j, with_ctx):
            Win = I['ret_w_in'][j]
            wsrc = Win.rearrange("(kt p) n -> p kt n", p=128)
            chunks = [(0, 256, False)] + [(256 + c * 512, 512, True) for c in range(4)]
            with contextlib.ExitStack() as ph:
                hT = load_fm(ph, 'hT', HT, tHT, KT, 0, TT)
                cs = {}
                for k in ('cosR0', 'sinR0', 'cosR1', 'sinR1'):
                    cs[k] = sb(ph, nm(k), [128, TL], F32)
                    S.op('sp', lambda e: e.dma_start(out=cs[k].ap[:], in_=CI[k]), [tIN], [cs[k]], dma=True)
                wring = Ring([sb(ph, nm('wq'), [128, KT, 128], BF16) for _ in range(3)])
                qnr = Ring([sb(ph, nm('qn'), [128, 512], F32) for _ in range(2)])
                t1r = Ring([sb(ph, nm('t1'), [128, 512], F32) for _ in range(2)])
                t2r = Ring([sb(ph, nm('t2'), [128, 512], F32) for _ in range(2)])
                obr = Ring([sb(ph, nm('ob'), [128, 512], BF16) for _ in range(3)])
                rq = []

                def r2_store(ob, n, t0, cb):
                    if cb < 16:
                        S.op('sp', lambda e: e.dma_start(out=QT[cb, :, t0:t0 + n], in_=ob.ap[:, 0:n]), [ob], [tQT], dma=True)
                    else:
                        S.op('sp', lambda e: e.dma_start(out=RK[cb - 16, :, t0:t0 + n], in_=ob.ap[:, 0:n]), [ob], [tRK], dma=True)

                def r2_tail(qn, n, t0, a, cb):
                    ob = obr.next()
                    psC = psring.next()
                    S.op('pe', lambda e: e.matmul(psC.ap[:, 0:n], lhsT=Csb['permR'].ap[:], rhs=qn.ap[:, 0:n], start=True, stop=True),
                         [qn, Csb['permR']], [psC])
                    l0 = t0 - TC
                    t1 = t1r.next()
                    t2 = t2r.next()
                    ck = cs['cosR%d' % a]
                    sk = cs['sinR%d' % a]
                    S.op('pool', lambda e: e.tensor_tensor(out=t1.ap[:, 0:n], in0=qn.ap[:, 0:n], in1=ck.ap[:, l0:l0 + n], op=ALU.mult),
                         [qn, ck], [t1])
                    S.op('dve', lambda e: e.tensor_tensor(out=t2.ap[:, 0:n], in0=psC.ap[:, 0:n], in1=sk.ap[:, l0:l0 + n], op=ALU.mult),
                         [psC, sk], [t2])
                    S.op('pool', lambda e: e.tensor_tensor(out=ob.ap[:, 0:n], in0=t1.ap[:, 0:n], in1=t2.ap[:, 0:n], op=ALU.add),
                         [t1, t2], [ob])
                    r2_store(ob, n, t0, cb)

                rcols = [cb * 128 for cb in range(32)] + [8192 + gb * 128 for gb in range(32)]
                wpf = Prefetch(wring, [(lambda t, c0=c0: S.op('pool', lambda e: e.dma_start(out=t.ap[:], in_=wsrc[:, :, c0:c0 + 128]),
                                                                 [tIN], [t], dma=True)) for c0 in rcols], 2)
                for cb in range(32):
                    is_q = cb < 16
                    a = cb % 2
                    col0 = cb * 128 if is_q else 2048 + (cb - 16) * 128
                    scl = 1.0 if is_q else 1.0 / 16.0
                    w = wpf.get(cb)
                    for (t0, n, lat) in chunks:
                        psA = psring.next()
                        for kt in range(KT):
                            S.op('pe', lambda e: e.matmul(psA.ap[:, 0:n], lhsT=w.ap[:, kt, :], rhs=hT.ap[:, kt, t0:t0 + n],
                                                          start=(kt == 0), stop=(kt == KT - 1)), [w, hT], [psA])
                        if lat:
                            qn = qnr.next()
                            S.op('act', lambda e: e.mul(out=qn.ap[:, 0:n], in_=psA.ap[:, 0:n], mul=scl), [psA], [qn])
                            rq.append((qn, n, t0, a, cb))
                            if len(rq) > 1:
                                r2_tail(*rq.pop(0))
                        else:
                            ob = obr.next()
                            S.op('act', lambda e: e.mul(out=ob.ap[:, 0:n], in_=psA.ap[:, 0:n], mul=scl), [psA], [ob])
                            r2_store(ob, n, t0, cb)
                while rq:
                    r2_tail(*rq.pop(0))
                for gb in range(32):
                    col0 = 8192 + gb * 128
                    w = wpf.get(32 + gb)
                    for (t0, n, lat) in chunks:
                        psA = psring.next()
                        for kt in range(KT):
                            S.op('pe', lambda e: e.matmul(psA.ap[:, 0:n], lhsT=w.ap[:, kt, :], rhs=hT.ap[:, kt, t0:t0 + n],
                                                          start=(kt == 0), stop=(kt == KT - 1)), [w, hT], [psA])
                        ob = obr.next()
                        S.op('act', lambda e: e.activation(out=ob.ap[:, 0:n], in_=psA.ap[:, 0:n], func=AF.Silu), [psA], [ob])
                        S.op('sp', lambda e: e.dma_start(out=RG[gb, :, t0:t0 + n], in_=ob.ap[:, 0:n]), [ob], [tRG], dma=True)
                wvr = Ring([sb(ph, nm('wv'), [128, KT, 512], BF16) for _ in range(2)])
                for vc in range(8):
                    wv = wvr.next()
                    S.op('pool', lambda e: e.dma_start(out=wv.ap[:], in_=wsrc[:, :, 4096 + vc * 512:4096 + (vc + 1) * 512]), [tIN], [wv], dma=True)
                    for i in range(NT):
                        ps = psring.next()
                        for kt in range(KT):
                            S.op('pe', lambda e: e.matmul(ps.ap[:], lhsT=hT.ap[:, kt, i * 128:(i + 1) * 128], rhs=wv.ap[:, kt, :],
                                                          start=(kt == 0), stop=(kt == KT - 1)), [hT, wv], [ps])
                        ob = obr.next()
                        if i % 2:
                            S.op('act', lambda e: e.copy(out=ob.ap[:], in_=ps.ap[:]), [ps], [ob])
                        else:
                            S.op('dve', lambda e: e.tensor_copy(out=ob.ap[:], in_=ps.ap[:]), [ps], [ob])
                        S.op('sp', lambda e: e.dma_start(out=RV[i * 128:(i + 1) * 128, vc * 512:(vc + 1) * 512], in_=ob.ap[:]), [ob], [tRV], dma=True)
                S.barrier()
            with contextlib.ExitStack() as ph:
                OFF = 1920
                SW = 3968
                CW = 2176
                ip = sb(ph, nm('ip'), [128, SW], F32)
                rp = sb(ph, nm('rp'), [128, SW], F32)
                rn = sb(ph, nm('rn'), [128, SW], F32)
                strip = sb(ph, nm('strip'), [128, SW], F32)
                tmpB = sb(ph, nm('tmpB'), [128, SW], F32)
                Ef = sb(ph, nm('Ef'), [128, CW], F32)
                Eb = sb(ph, nm('Eb'), [128, CW], F32)
                Cs = sb(ph, nm('Cs'), [128, CW], F32)
                tmpC = sb(ph, nm('tmpC'), [128, CW], F32)
                S.op('sp', lambda e: e.dma_start(out=ip.ap[:], in_=CI['retDelta']), [tIN], [ip], dma=True)
                S.op('sp', lambda e: e.dma_start(out=Ef.ap[:], in_=CI['retEf']), [tIN], [Ef], dma=True)
                S.op('sp', lambda e: e.dma_start(out=Eb.ap[:], in_=CI['retEb']), [tIN], [Eb], dma=True)
                S.op('dve', lambda e: e.tensor_scalar(out=rp.ap[:], in0=ip.ap[:], scalar1=0.0, scalar2=None, op0=ALU.max), [ip], [rp])
                S.op('dve', lambda e: e.tensor_tensor(out=rn.ap[:], in0=rp.ap[:], in1=ip.ap[:], op=ALU.subtract), [rp, ip], [rn])
                S.op('dve', lambda e: e.tensor_scalar(out=ip.ap[:], in0=ip.ap[:], scalar1=0.0, scalar2=None, op0=ALU.is_ge), [ip], [ip])
                lg = sb(ph, nm('lg'), [128, 16], F32)
                S.op('sp', lambda e: e.dma_start(out=lg.ap[:], in_=I['ret_decay_logit'][j].rearrange("a h -> (a h)").partition_broadcast(128)),
                     [tIN], [lg], dma=True)
                S.op('act', lambda e: e.activation(out=lg.ap[:], in_=lg.ap[:], func=AF.Exp, scale=-1.0), [lg], [lg])
                S.op('dve', lambda e: e.tensor_scalar(out=lg.ap[:], in0=lg.ap[:], scalar1=1.0, scalar2=None, op0=ALU.add), [lg], [lg])
                S.op('act', lambda e: e.activation(out=lg.ap[:], in_=lg.ap[:], func=AF.Ln), [lg], [lg])
                S.op('dve', lambda e: e.tensor_scalar(out=lg.ap[:], in0=lg.ap[:], scalar1=-1.0, scalar2=None, op0=ALU.mult), [lg], [lg])
                q2r = Ring([sb(ph, nm('q2'), [128, 2, TT], BF16) for _ in range(1)])
                k2r = Ring([sb(ph, nm('k2'), [128, 2, TT], BF16) for _ in range(1)])
                vhr = Ring([sb(ph, nm('vh'), [128, NT, 512], BF16) for _ in range(1)])
                smr = Ring([sb(ph, nm('sm'), [128, 512], BF16) for _ in range(3)])
                sqr = Ring([sb(ph, nm('sq'), [128, 512], F32) for _ in range(2)])
                rsr = Ring([sb(ph, nm('rs'), [128, 512], F32) for _ in range(1)])
                gr = Ring([sb(ph, nm('g'), [128, 512], BF16) for _ in range(2)])
                tmr = Ring([sb(ph, nm('tm'), [128, 512], F32) for _ in range(2)])
                obr = Ring([sb(ph, nm('ob'), [128, 512], BF16) for _ in range(2)])
                O = PS[0:4]
                pSr = Ring(PS[4:6])
                pN = PS[6]
                rpend = []

                def emit_rpv(sm, n, jt, idx, nsrc, vh):
                    for vt in range(4):
                        S.op('pe', lambda e: e.matmul(O[vt].ap[:, 0:n], lhsT=vh.ap[:, jt, vt * 128:(vt + 1) * 128], rhs=sm.ap[:, 0:n],
                                                      start=(idx == 0), stop=(idx == nsrc - 1)), [vh, sm], [O[vt]])

                for h_ in range(8):
                    S.op('act', lambda e: e.activation(out=strip.ap[:], in_=rp.ap[:], func=AF.Exp, scale=lg.ap[:, h_:h_ + 1]), [rp, lg], [strip])
                    S.op('act', lambda e: e.activation(out=tmpB.ap[:], in_=rn.ap[:], func=AF.Exp, scale=lg.ap[:, 8 + h_:9 + h_]), [rn, lg], [tmpB])
                    S.op('pool', lambda e: e.tensor_tensor(out=strip.ap[:], in0=strip.ap[:], in1=tmpB.ap[:], op=ALU.subtract), [strip, tmpB], [strip])
                    S.op('dve', lambda e: e.tensor_tensor(out=strip.ap[:], in0=strip.ap[:], in1=ip.ap[:], op=ALU.mult), [strip, ip], [strip])
                    S.op('pool', lambda e: e.tensor_tensor(out=strip.ap[:], in0=strip.ap[:], in1=tmpB.ap[:], op=ALU.add), [strip, tmpB], [strip])
                    S.op('act', lambda e: e.activation(out=Cs.ap[:], in_=Ef.ap[:], func=AF.Exp, scale=lg.ap[:, h_:h_ + 1]), [Ef, lg], [Cs])
                    S.op('act', lambda e: e.activation(out=tmpC.ap[:], in_=Eb.ap[:], func=AF.Exp, scale=lg.ap[:, 8 + h_:9 + h_]), [Eb, lg], [tmpC])
                    S.op('pool', lambda e: e.tensor_tensor(out=Cs.ap[:], in0=Cs.ap[:], in1=tmpC.ap[:], op=ALU.add), [Cs, tmpC], [Cs])
                    q2 = q2r.next()
                    k2 = k2r.next()
                    vh = vhr.next()
                    for a in range(2):
                        S.op('sp', lambda e: e.dma_start(out=q2.ap[:, a, :], in_=QT[2 * h_ + a]), [tQT], [q2], dma=True)
                        S.op('sp', lambda e: e.dma_start(out=k2.ap[:, a, :], in_=RK[2 * h_ + a]), [tRK], [k2], dma=True)
                    for i in range(NT):
                        S.op('sp', lambda e: e.dma_start(out=vh.ap[:, i, :], in_=RV[i * 128:(i + 1) * 128, h_ * 512:(h_ + 1) * 512]), [tRV], [vh], dma=True)
                    for (t0, n, lat) in chunks:
                        srcs = list(range(NT)) if lat else [0, 1]
                        for idx, jt in enumerate(srcs):
                            pS = pSr.next()
                            for a in range(2):
                                S.op('pe', lambda e: e.matmul(pS.ap[:, 0:n], lhsT=k2.ap[:, a, jt * 128:(jt + 1) * 128], rhs=q2.ap[:, a, t0:t0 + n],
                                                              start=(a == 0), stop=(a == 1)), [k2, q2], [pS])
                            if not lat:
                                x0 = 0 - 128 * jt + OFF
                                mk, mt = strip.ap[:, x0:x0 + n], strip
                            elif jt < 2:
                                y0 = (t0 - TC) + (128 if jt == 0 else 0)
                                mk, mt = Cs.ap[:, y0:y0 + n], Cs
                            else:
                                x0 = (t0 - TC) - 128 * (jt - 2) + OFF
                                mk, mt = strip.ap[:, x0:x0 + n], strip
                            sm = smr.next()
                            S.op('dve', lambda e: e.tensor_tensor(out=sm.ap[:, 0:n], in0=pS.ap[:, 0:n], in1=mk, op=ALU.mult), [pS, mt], [sm])
                            rpend.append((sm, n, jt, idx, len(srcs), vh))
                            if len(rpend) > 1:
                                emit_rpv(*rpend.pop(0))
                        while rpend:
                            emit_rpv(*rpend.pop(0))
                        for vt in range(4):
                            sq = sqr.next()
                            S.op('act', lambda e: e.activation(out=sq.ap[:, 0:n], in_=O[vt].ap[:, 0:n], func=AF.Square), [O[vt]], [sq])
                            S.op('pe', lambda e: e.matmul(pN.ap[:, 0:n], lhsT=Csb['ones_f'].ap[:], rhs=sq.ap[:, 0:n], start=(vt == 0), stop=(vt == 3)),
                                 [sq, Csb['ones_f']], [pN])
                        rs = rsr.next()
                        S.op('dve', lambda e: e.tensor_scalar(out=rs.ap[:, 0:n], in0=pN.ap[:, 0:n], scalar1=1.0 / 512, scalar2=EPS,
                                                              op0=ALU.mult, op1=ALU.add), [pN], [rs])
                        S.op('act', lambda e: e.activation(out=rs.ap[:, 0:n], in_=rs.ap[:, 0:n], func=AF.Sqrt), [rs], [rs])
                        S.op('dve', lambda e: e.reciprocal(out=rs.ap[:, 0:n], in_=rs.ap[:, 0:n]), [rs], [rs])
                        for vt in range(4):
                            g = gr.next()
                            S.op('sp', lambda e: e.dma_start(out=g.ap[:, 0:n], in_=RG[h_ * 4 + vt, :, t0:t0 + n]), [tRG], [g], dma=True)
                            tm = tmr.next()
                            S.op('dve', lambda e: e.tensor_tensor(out=tm.ap[:, 0:n], in0=O[vt].ap[:, 0:n], in1=rs.ap[:, 0:n], op=ALU.mult), [O[vt], rs], [tm])
                            ob = obr.next()
                            S.op('pool', lambda e: e.tensor_tensor(out=ob.ap[:, 0:n], in0=tm.ap[:, 0:n], in1=g.ap[:, 0:n], op=ALU.mult), [tm, g], [ob])
                            S.op('sp', lambda e: e.dma_start(out=OT[h_ * 4 + vt, :, t0:t0 + n], in_=ob.ap[:, 0:n]), [ob], [tOT], dma=True)
                S.barrier()
            phase_proj_residual(li, OT, tOT, 32, I['ret_w_o'][j], 0, with_ctx)


        def phase_hyena(li, j, with_ctx):
            wsrc = I['hy_w_in'][j].rearrange("(kt p) n -> p kt n", p=128)
            chunks = [(0, 256, False)] + [(256 + c * 512, 512, True) for c in range(4)]
            ZW = 2308
            with contextlib.ExitStack() as ph:
                hT = load_fm(ph, 'hT', HT, tHT, KT, 0, TT)
                A1 = sb(ph, nm('A1'), [128, 128], F32)
                A2 = sb(ph, nm('A2'), [80, 128], F32)
                cwT = sb(ph, nm('cwT'), [128, 208], F32)
                cwv = I['hy_conv_w'][j].rearrange("k (cb p) -> (k cb) p", p=128)
                S.op('sp', lambda e: e.dma_start(out=A1.ap[:], in_=cwv[0:128, :]), [tIN], [A1], dma=True)
                S.op('sp', lambda e: e.dma_start(out=A2.ap[0:16, :], in_=cwv[128:144, :]), [tIN], [A2], dma=True)
                S.op('sp', lambda e: e.dma_start(out=A2.ap[16:64, :], in_=I['hy_conv_b'][j].rearrange("(cb p) -> cb p", p=128)), [tIN], [A2], dma=True)
                S.op('sp', lambda e: e.dma_start(out=A2.ap[64:80, :], in_=I['hy_skip'][j].rearrange("(cb p) -> cb p", p=128)), [tIN], [A2], dma=True)
                ps = psring.next()
                S.op('pe', lambda e: e.transpose(out=ps.ap[:, 0:128], in_=A1.ap[:], identity=Csb['ident_f'].ap[:]), [A1, Csb['ident_f']], [ps])
                S.op('pe', lambda e: e.transpose(out=ps.ap[:, 128:208], in_=A2.ap[:], identity=Csb['ident_f'].ap[0:80, 0:80]), [A2, Csb['ident_f']], [ps])
                S.op('dve', lambda e: e.tensor_copy(out=cwT.ap[:], in_=ps.ap[:, 0:208]), [ps], [cwT])
                S.op('sp', lambda e: e.dma_start(out=SKIPT[:, :], in_=cwT.ap[:, 192:208]), [cwT], [tSKIPT], dma=True)
                wring = Ring([sb(ph, nm('wq'), [128, KT, 128], BF16) for _ in range(3)])
                zbs = [sb(ph, nm('zb'), [128, ZW], F32) for _ in range(3)]
                for zb in zbs:
                    S.op('pool', lambda e: e.memset(zb.ap[:], 0.0), [], [zb])
                zcs = [sb(ph, nm('zc'), [128, TT], F32) for _ in range(3)]
                ub = sb(ph, nm('ub'), [128, TT], BF16)
                ust = Ring([sb(ph, nm('ust'), [128, 6, 128], BF16) for _ in range(2)])
                hblocks = [cb for ct in range(KT) for cb in (16 + ct, 32 + ct, ct)]
                wpf = Prefetch(wring, [(lambda t, cb=cb: S.op('pool', lambda e: e.dma_start(out=t.ap[:], in_=wsrc[:, :, cb * 128:(cb + 1) * 128]),
                                                                 [tIN], [t], dma=True)) for cb in hblocks], 2)
                for ct in range(KT):
                    for bi, cb in enumerate((16 + ct, 32 + ct, ct)):
                        w = wpf.get(ct * 3 + bi)
                        zb = zbs[bi]
                        for (t0, n, lat) in chunks:
                            psA = psring.next()
                            for kt in range(KT):
                                S.op('pe', lambda e: e.matmul(psA.ap[:, 0:n], lhsT=w.ap[:, kt, :], rhs=hT.ap[:, kt, t0:t0 + n],
                                                              start=(kt == 0), stop=(kt == KT - 1)), [w, hT], [psA])
                            z0 = (1 + t0) if not lat else (259 + t0 - TC)
                            S.op('act', lambda e: e.copy(out=zb.ap[:, z0:z0 + n], in_=psA.ap[:, 0:n]), [psA], [zb])
                        zc = zcs[bi]
                        for (zoff, t0, n) in ((0, 0, TC), (258, TC, TL)):
                            S.op('dve', lambda e: e.tensor_scalar(out=zc.ap[:, t0:t0 + n], in0=zb.ap[:, zoff + 1:zoff + 1 + n],
                                                                  scalar1=cwT.ap[:, 48 + cb:49 + cb], scalar2=cwT.ap[:, 144 + cb:145 + cb],
                                                                  op0=ALU.mult, op1=ALU.add), [zb, cwT], [zc])
                            S.op('dve', lambda e: e.scalar_tensor_tensor(out=zc.ap[:, t0:t0 + n], in0=zb.ap[:, zoff:zoff + n],
                                                                         scalar=cwT.ap[:, cb:cb + 1], in1=zc.ap[:, t0:t0 + n],
                                                                         op0=ALU.mult, op1=ALU.add), [zb, cwT, zc], [zc])
                            S.op('dve', lambda e: e.scalar_tensor_tensor(out=zc.ap[:, t0:t0 + n], in0=zb.ap[:, zoff + 2:zoff + 2 + n],
                                                                         scalar=cwT.ap[:, 96 + cb:97 + cb], in1=zc.ap[:, t0:t0 + n],
                                                                         op0=ALU.mult, op1=ALU.add), [zb, cwT, zc], [zc])
                    S.op('pool', lambda e: e.tensor_tensor(out=zcs[0].ap[:], in0=zcs[0].ap[:], in1=zcs[1].ap[:], op=ALU.mult), [zcs[0], zcs[1]], [zcs[0]])
                    S.op('sp', lambda e: e.dma_start(out=UTF[ct], in_=zcs[0].ap[:]), [zcs[0]], [tUTF], dma=True)
                    S.op('sp', lambda e: e.dma_start(out=X0F[ct], in_=zcs[2].ap[:]), [zcs[2]], [tX0F], dma=True)
                    S.op('act', lambda e: e.copy(out=ub.ap[:], in_=zcs[0].ap[:]), [zcs[0]], [ub])
                    for g in range(3):
                        for jj in range(6):
                            i = g * 6 + jj
                            S.op('pe', lambda e: e.transpose(out=PSB.ap[:, jj * 128:(jj + 1) * 128], in_=ub.ap[:, i * 128:(i + 1) * 128],
                                                             identity=Csb['ident_b'].ap[:]), [ub, Csb['ident_b']], [PSB])
                        us = ust.next()
                        S.op('dve', lambda e: e.tensor_copy(out=us.ap[:].rearrange("p a b -> p (a b)"), in_=PSB.ap[:, 0:768]), [PSB], [us])
                        for hh in range(2):
                            i0 = g * 6 + hh * 3
                            S.op('sp', lambda e: e.dma_start(out=UTM[i0 * 128:(i0 + 3) * 128, ct * 128:(ct + 1) * 128].rearrange("(i p) c -> p i c", p=128),
                                                             in_=us.ap[:, hh * 3:(hh + 1) * 3, :]), [us], [tUTM], dma=True)
                S.barrier()
            for (tag, L, tok0) in ((('C', TC, 0),) if with_ctx else ()) + (('L', TL, TC),):
                hy_filter(j, tag, L)
                hy_conv(j, tag, L, tok0)
            phase_proj_residual(li, OT, tOT, 16, I['hy_w_out'][j], 0, with_ctx)

        def hy_filter(j, tag, L):
            with contextlib.ExitStack() as ph0:
                rnorm = sb(ph0, nm('rnorm'), [128, D], F32)
                hy_filter_inner(j, tag, L, rnorm)

        def hy_filter_inner(j, tag, L, rnorm):
            nkt = L // 128
            cw = min(512, L)
            with contextlib.ExitStack() as ph:
                zT = sb(ph, nm('zT'), [33, L], F32)
                S.op('sp', lambda e: e.dma_start(out=zT.ap[:], in_=CI['hyZ' + tag]), [tIN], [zT], dma=True)
                w1 = sb(ph, nm('w1'), [33, 128], F32)
                w2 = sb(ph, nm('w2'), [64, 128], F32)
                w3 = sb(ph, nm('w3'), [64, 2 * D], F32)
                S.op('dve', lambda e: e.memset(w1.ap[:], 0.0), [], [w1])
                S.op('dve', lambda e: e.memset(w2.ap[:], 0.0), [], [w2])
                S.op('sp', lambda e: e.dma_start(out=w1.ap[:, 0:64], in_=I['hy_f_w1'][j]), [tIN], [w1], dma=True)
                S.op('sp', lambda e: e.dma_start(out=w2.ap[:, 0:64], in_=I['hy_f_w2'][j]), [tIN], [w2], dma=True)
                S.op('sp', lambda e: e.dma_start(out=w3.ap[:], in_=I['hy_f_w3'][j]), [tIN], [w3], dma=True)
                pv = sb(ph, nm('pv'), [64, 4], F32)
                for ci, k in enumerate(('hy_f_b1', 'hy_f_freq1', 'hy_f_b2', 'hy_f_freq2')):
                    S.op('sp', lambda e: e.dma_start(out=pv.ap[:, ci:ci + 1], in_=I[k][j].rearrange("(p o) -> p o", o=1)), [tIN], [pv], dma=True)
                S.op('dve', lambda e: e.tensor_scalar(out=pv.ap[:, 1:2], in0=pv.ap[:, 1:2], scalar1=1.0 / (2 * math.pi), scalar2=None, op0=ALU.mult), [pv], [pv])
                S.op('dve', lambda e: e.tensor_scalar(out=pv.ap[:, 3:4], in0=pv.ap[:, 3:4], scalar1=1.0 / (2 * math.pi), scalar2=None, op0=ALU.mult), [pv], [pv])
                h1T = sb(ph, nm('h1T'), [64, L], F32)
                h2T = sb(ph, nm('h2T'), [64, L], F32)
                r = sb(ph, nm('r'), [64, 512], F32)
                ri = sb(ph, nm('ri'), [64, 512], mybir.dt.int32)
                rf = sb(ph, nm('rf'), [64, 512], F32)
                msk = sb(ph, nm('msk'), [64, 512], F32)

                def sin_layer(wt, kdim, src, dst, bcol):
                    for c0 in range(0, L, cw):
                        ps = psring.next()
                        S.op('pe', lambda e: e.matmul(ps.ap[:, 0:cw], lhsT=wt.ap[0:kdim, :], rhs=src.ap[0:kdim, c0:c0 + cw], start=True, stop=True),
                             [wt, src], [ps])
                        S.op('dve', lambda e: e.tensor_scalar(out=r.ap[:, 0:cw], in0=ps.ap[0:64, 0:cw], scalar1=pv.ap[:, bcol:bcol + 1],
                                                              scalar2=pv.ap[:, bcol + 1:bcol + 2], op0=ALU.add, op1=ALU.mult), [ps, pv], [r])
                        S.op('dve', lambda e: e.tensor_copy(out=ri.ap[:, 0:cw], in_=r.ap[:, 0:cw]), [r], [ri])
                        S.op('dve', lambda e: e.tensor_copy(out=rf.ap[:, 0:cw], in_=ri.ap[:, 0:cw]), [ri], [rf])
                        S.op('dve', lambda e: e.tensor_tensor(out=r.ap[:, 0:cw], in0=r.ap[:, 0:cw], in1=rf.ap[:, 0:cw], op=ALU.subtract), [r, rf], [r])
                        S.op('dve', lambda e: e.tensor_scalar(out=msk.ap[:, 0:cw], in0=r.ap[:, 0:cw], scalar1=0.5, scalar2=None, op0=ALU.is_gt), [r], [msk])
                        S.op('dve', lambda e: e.tensor_tensor(out=r.ap[:, 0:cw], in0=r.ap[:, 0:cw], in1=msk.ap[:, 0:cw], op=ALU.subtract), [r, msk], [r])
                        S.op('dve', lambda e: e.tensor_scalar(out=msk.ap[:, 0:cw], in0=r.ap[:, 0:cw], scalar1=-0.5, scalar2=None, op0=ALU.is_lt), [r], [msk])
                        S.op('dve', lambda e: e.tensor_tensor(out=r.ap[:, 0:cw], in0=r.ap[:, 0:cw], in1=msk.ap[:, 0:cw], op=ALU.add), [r, msk], [r])
                        S.op('act', lambda e: e.activation(out=dst.ap[:, c0:c0 + cw], in_=r.ap[:, 0:cw], func=AF.Sin, scale=6.28318), [r], [dst])
                sin_layer(w1, 33, zT, h1T, 0)
                sin_layer(w2, 64, h1T, h2T, 2)
                dring = Ring([sb(ph, nm('dec'), [128, 512], F32) for _ in range(3)])
                hfr = Ring([sb(ph, nm('hf'), [128, 512], F32) for _ in range(3)])
                hbr = Ring([sb(ph, nm('hb'), [128, 512], F32) for _ in range(3)])
                abr = Ring([sb(ph, nm('ab'), [128, 512], F32) for _ in range(3)])
                aacc = [sb(ph, nm('aacc'), [128, 512], F32) for _ in range(4)]
                for c4 in range(4):
                    S.op('pool', lambda e: e.memset(aacc[c4].ap[:], 0.0), [], [aacc[c4]])
                hpr = Ring([sb(ph, nm('hp'), [128, 512], BF16) for _ in range(3)])
                NP = PS[0:4]
                pr = Ring(PS[4:7])
                dpf = Prefetch(dring, [(lambda t, kt=kt, c4=c4: S.op('sp', lambda e: e.dma_start(out=t.ap[:], in_=CI['hyDecay' + tag][kt * 128:(kt + 1) * 128, c4 * 512:(c4 + 1) * 512]),
                                                                      [tIN], [t], dma=True)) for kt in range(nkt) for c4 in range(4)], 2)
                for kt in range(nkt):
                    for c4 in range(4):
                        dec = dpf.get(kt * 4 + c4)
                        hh = []
                        for dr, rg in ((0, hfr), (1, hbr)):
                            ps = pr.next()
                            S.op('pe', lambda e: e.matmul(ps.ap[:], lhsT=h2T.ap[:, kt * 128:(kt + 1) * 128], rhs=w3.ap[:, dr * D + c4 * 512:dr * D + (c4 + 1) * 512],
                                                          start=True, stop=True), [h2T, w3], [ps])
                            ht = rg.next()
                            S.op('dve', lambda e: e.tensor_tensor(out=ht.ap[:], in0=ps.ap[:], in1=dec.ap[:], op=ALU.mult), [ps, dec], [ht])
                            if dr == 1 and kt == 0:
                                S.op('dve', lambda e: e.memset(ht.ap[0:1, :], 0.0), [], [ht])
                            ab = abr.next()
                            S.op('act', lambda e: e.activation(out=ab.ap[:], in_=ht.ap[:], func=AF.Abs), [ht], [ab])
                            S.op('dve', lambda e: e.tensor_tensor(out=aacc[c4].ap[:], in0=aacc[c4].ap[:], in1=ab.ap[:], op=ALU.add), [ab, aacc[c4]], [aacc[c4]])
                            hh.append(ht)
                        hp = hpr.next()
                        S.op('pool', lambda e: e.tensor_tensor(out=hp.ap[:], in0=hh[0].ap[:], in1=hh[1].ap[:], op=ALU.add), [hh[0], hh[1]], [hp])
                        S.op('sp', lambda e: e.dma_start(out=HPM[0, kt * 128:(kt + 1) * 128, c4 * 512:(c4 + 1) * 512], in_=hp.ap[:]), [hp], [tHPM], dma=True)
                        hm = hpr.next()
                        S.op('pool', lambda e: e.tensor_tensor(out=hm.ap[:], in0=hh[0].ap[:], in1=hh[1].ap[:], op=ALU.subtract), [hh[0], hh[1]], [hm])
                        S.op('sp', lambda e: e.dma_start(out=HPM[1, kt * 128:(kt + 1) * 128, c4 * 512:(c4 + 1) * 512], in_=hm.ap[:]), [hm], [tHPM], dma=True)
                for c4 in range(4):
                    S.op('pe', lambda e: e.matmul(NP[c4].ap[:], lhsT=Csb['ones_f'].ap[:], rhs=aacc[c4].ap[:], start=True, stop=True),
                         [aacc[c4], Csb['ones_f']], [NP[c4]])
                    S.op('dve', lambda e: e.reciprocal(out=rnorm.ap[:, c4 * 512:(c4 + 1) * 512], in_=NP[c4].ap[:]), [NP[c4]], [rnorm])
                S.barrier()
            with contextlib.ExitStack() as ph:
                hpr = Ring([sb(ph, nm('hpc'), [128, nkt, 512], BF16) for _ in range(2)])
                hmr = Ring([sb(ph, nm('hmc'), [128, nkt, 512], BF16) for _ in range(2)])
                ctr = Ring([sb(ph, nm('ct'), [128, nkt, 128], BF16) for _ in range(3)])
                strr = Ring([sb(ph, nm('st'), [128, nkt, 128], BF16) for _ in range(3)])
                hor = Ring([sb(ph, nm('ho'), [128, 512], F32) for _ in range(4)])
                cpf = Prefetch(ctr, [(lambda t, ft=ft: S.op('sp', lambda e: e.dma_start(out=t.ap[:].rearrange("p a b -> p (a b)"), in_=CI['hyCT' + tag][ft]),
                                                             [tIN], [t], dma=True)) for _c in range(4) for ft in range(nkt)], 2)
                spf = Prefetch(strr, [(lambda t, ft=ft: S.op('sp', lambda e: e.dma_start(out=t.ap[:].rearrange("p a b -> p (a b)"), in_=CI['hyST' + tag][ft]),
                                                              [tIN], [t], dma=True)) for _c in range(4) for ft in range(nkt)], 2)
                for c4 in range(4):
                    hp = hpr.next()
                    hm = hmr.next()
                    for kt in range(nkt):
                        S.op('sp', lambda e: e.dma_start(out=hp.ap[:, kt, :], in_=HPM[0, kt * 128:(kt + 1) * 128, c4 * 512:(c4 + 1) * 512]), [tHPM], [hp], dma=True)
                        S.op('sp', lambda e: e.dma_start(out=hm.ap[:, kt, :], in_=HPM[1, kt * 128:(kt + 1) * 128, c4 * 512:(c4 + 1) * 512]), [tHPM], [hm], dma=True)
                    for ft in range(nkt):
                        ctt = cpf.get(c4 * nkt + ft)
                        stt = spf.get(c4 * nkt + ft)
                        for si, (mt, hx) in enumerate(((ctt, hp), (stt, hm))):
                            ps = psring.next()
                            for kt in range(nkt):
                                S.op('pe', lambda e: e.matmul(ps.ap[:], lhsT=mt.ap[:, kt, :], rhs=hx.ap[:, kt, :], start=(kt == 0), stop=(kt == nkt - 1)),
                                     [mt, hx], [ps])
                            ho = hor.next()
                            S.op('dve', lambda e: e.tensor_tensor(out=ho.ap[:], in0=ps.ap[:], in1=rnorm.ap[:, c4 * 512:(c4 + 1) * 512], op=ALU.mult), [ps, rnorm], [ho])
                            S.op('sp', lambda e: e.dma_start(out=HSPEC[tag][si, ft * 128:(ft + 1) * 128, c4 * 512:(c4 + 1) * 512], in_=ho.ap[:]), [ho], [tHSPEC[tag]], dma=True)
                S.barrier()

        def hy_conv(j, tag, L, tok0):
            nkt = L // 128
            tcw = min(512, L)
            ntc = L // tcw
            nfft = 2 * L
            with contextlib.ExitStack() as ph:
                skT = sb(ph, nm('skT'), [128, KT], F32)
                S.op('sp', lambda e: e.dma_start(out=skT.ap[:], in_=SKIPT[:, :]), [tSKIPT], [skT], dma=True)
                ur = Ring([sb(ph, nm('u'), [128, nkt, 512], BF16) for _ in range(1)])
                Yr = Ring([sb(ph, nm('Y'), [128, 2 * nkt, 512], BF16) for _ in range(1)])
                ctr = Ring([sb(ph, nm('ct'), [128, nkt, 128], BF16) for _ in range(3)])
                strr = Ring([sb(ph, nm('st'), [128, nkt, 128], BF16) for _ in range(3)])
                hcr = Ring([sb(ph, nm('hc'), [128, 512], F32) for _ in range(3)])
                hsr = Ring([sb(ph, nm('hs'), [128, 512], F32) for _ in range(3)])
                ucr = Ring([sb(ph, nm('uc'), [128, 512], F32) for _ in range(2)])
                usr = Ring([sb(ph, nm('us'), [128, 512], F32) for _ in range(2)])
                tr_ = Ring([sb(ph, nm('tt'), [128, 512], F32) for _ in range(4)])
                cfr = Ring([sb(ph, nm('cf'), [128, nkt, tcw], BF16) for _ in range(2)])
                sfr = Ring([sb(ph, nm('sf'), [128, nkt, tcw], BF16) for _ in range(2)])
                utr = Ring([sb(ph, nm('ut'), [128, 512], F32) for _ in range(2)])
                x0r = Ring([sb(ph, nm('x0'), [128, 512], F32) for _ in range(2)])
                obr = Ring([sb(ph, nm('ob'), [128, 512], BF16) for _ in range(2)])
                cpf = Prefetch(ctr, [(lambda t, ft=ft: S.op('sp', lambda e: e.dma_start(out=t.ap[:].rearrange("p a b -> p (a b)"), in_=CI['hyCT' + tag][ft]),
                                                             [tIN], [t], dma=True)) for _c in range(4) for ft in range(nkt)], 2)
                spf = Prefetch(strr, [(lambda t, ft=ft: S.op('sp', lambda e: e.dma_start(out=t.ap[:].rearrange("p a b -> p (a b)"), in_=CI['hyST' + tag][ft]),
                                                              [tIN], [t], dma=True)) for _c in range(4) for ft in range(nkt)], 2)
                hcpf = Prefetch(hcr, [(lambda t, ft=ft, c4=c4: S.op('sp', lambda e: e.dma_start(out=t.ap[:], in_=HSPEC[tag][0, ft * 128:(ft + 1) * 128, c4 * 512:(c4 + 1) * 512]),
                                                                     [tHSPEC[tag]], [t], dma=True)) for c4 in range(4) for ft in range(nkt)], 2)
                hspf = Prefetch(hsr, [(lambda t, ft=ft, c4=c4: S.op('sp', lambda e: e.dma_start(out=t.ap[:], in_=HSPEC[tag][1, ft * 128:(ft + 1) * 128, c4 * 512:(c4 + 1) * 512]),
                                                                     [tHSPEC[tag]], [t], dma=True)) for c4 in range(4) for ft in range(nkt)], 2)
                cfpf = Prefetch(cfr, [(lambda t, tc=tc: S.op('sp', lambda e: e.dma_start(out=t.ap[:].rearrange("p a b -> p (a b)"), in_=CI['hyCF' + tag][tc]),
                                                              [tIN], [t], dma=True)) for _c in range(4) for tc in range(ntc)], 1)
                sfpf = Prefetch(sfr, [(lambda t, tc=tc: S.op('sp', lambda e: e.dma_start(out=t.ap[:].rearrange("p a b -> p (a b)"), in_=CI['hySF' + tag][tc]),
                                                              [tIN], [t], dma=True)) for _c in range(4) for tc in range(ntc)], 1)
                for c4 in range(4):
                    u = ur.next()
                    for kt in range(nkt):
                        S.op('sp', lambda e: e.dma_start(out=u.ap[:, kt, :], in_=UTM[tok0 + kt * 128:tok0 + (kt + 1) * 128, c4 * 512:(c4 + 1) * 512]), [tUTM], [u], dma=True)
                    Y = Yr.next()
                    for ft in range(nkt):
                        ctt = cpf.get(c4 * nkt + ft)
                        stt = spf.get(c4 * nkt + ft)
                        hc = hcpf.get(c4 * nkt + ft)
                        hs = hspf.get(c4 * nkt + ft)
                        pc = psring.next()
                        pss = psring.next()
                        for kt in range(nkt):
                            S.op('pe', lambda e: e.matmul(pc.ap[:], lhsT=ctt.ap[:, kt, :], rhs=u.ap[:, kt, :], start=(kt == 0), stop=(kt == nkt - 1)), [ctt, u], [pc])
                        for kt in range(nkt):
                            S.op('pe', lambda e: e.matmul(pss.ap[:], lhsT=stt.ap[:, kt, :], rhs=u.ap[:, kt, :], start=(kt == 0), stop=(kt == nkt - 1)), [stt, u], [pss])
                        uc = ucr.next()
                        us = usr.next()
                        S.op('act', lambda e: e.copy(out=uc.ap[:], in_=pc.ap[:]), [pc], [uc])
                        S.op('act', lambda e: e.copy(out=us.ap[:], in_=pss.ap[:]), [pss], [us])
                        t1 = tr_.next(); t2 = tr_.next(); t3 = tr_.next(); t4 = tr_.next()
                        S.op('dve', lambda e: e.tensor_tensor(out=t1.ap[:], in0=uc.ap[:], in1=hc.ap[:], op=ALU.mult), [uc, hc], [t1])
                        S.op('pool', lambda e: e.tensor_tensor(out=t2.ap[:], in0=us.ap[:], in1=hs.ap[:], op=ALU.mult), [us, hs], [t2])
                        S.op('dve', lambda e: e.tensor_tensor(out=Y.ap[:, ft, :], in0=t1.ap[:], in1=t2.ap[:], op=ALU.subtract), [t1, t2], [Y])
                        S.op('pool', lambda e: e.tensor_tensor(out=t3.ap[:], in0=uc.ap[:], in1=hs.ap[:], op=ALU.mult), [uc, hs], [t3])
                        S.op('dve', lambda e: e.tensor_tensor(out=t4.ap[:], in0=us.ap[:], in1=hc.ap[:], op=ALU.mult), [us, hc], [t4])
                        S.op('pool', lambda e: e.tensor_tensor(out=Y.ap[:, nkt + ft, :], in0=t3.ap[:], in1=t4.ap[:], op=ALU.add), [t3, t4], [Y])
                    for tc in range(ntc):
                        cf = cfpf.get(c4 * ntc + tc)
                        sf = sfpf.get(c4 * ntc + tc)
                        for ctl in range(4):
                            cg = c4 * 4 + ctl
                            ps = psring.next()
                            for ft in range(nkt):
                                S.op('pe', lambda e: e.matmul(ps.ap[:, 0:tcw], lhsT=Y.ap[:, ft, ctl * 128:(ctl + 1) * 128], rhs=cf.ap[:, ft, :],
                                                              start=(ft == 0), stop=False), [Y, cf], [ps])
                                S.op('pe', lambda e: e.matmul(ps.ap[:, 0:tcw], lhsT=Y.ap[:, nkt + ft, ctl * 128:(ctl + 1) * 128], rhs=sf.ap[:, ft, :],
                                                              start=False, stop=(ft == nkt - 1)), [Y, sf], [ps])
                            g0 = tok0 + tc * tcw
                            ut = utr.next()
                            x0 = x0r.next()
                            S.op('sp', lambda e: e.dma_start(out=ut.ap[:, 0:tcw], in_=UTF[cg, :, g0:g0 + tcw]), [tUTF], [ut], dma=True)
                            S.op('sp', lambda e: e.dma_start(out=x0.ap[:, 0:tcw], in_=X0F[cg, :, g0:g0 + tcw]), [tX0F], [x0], dma=True)
                            S.op('pool', lambda e: e.tensor_scalar(out=ut.ap[:, 0:tcw], in0=ut.ap[:, 0:tcw], scalar1=skT.ap[:, cg:cg + 1], scalar2=None, op0=ALU.mult),
                                 [ut, skT], [ut])
                            S.op('dve', lambda e: e.scalar_tensor_tensor(out=ut.ap[:, 0:tcw], in0=ps.ap[:, 0:tcw], scalar=2.0 / nfft, in1=ut.ap[:, 0:tcw],
                                                                         op0=ALU.mult, op1=ALU.add), [ps, ut], [ut])
                            ob = obr.next()
                            S.op('pool', lambda e: e.tensor_tensor(out=ob.ap[:, 0:tcw], in0=ut.ap[:, 0:tcw], in1=x0.ap[:, 0:tcw], op=ALU.mult), [ut, x0], [ob])
                            S.op('sp', lambda e: e.dma_start(out=OT[cg, :, g0:g0 + tcw], in_=ob.ap[:, 0:tcw]), [ob], [tOT], dma=True)
                S.barrier()

        def phase_moe(li, with_ctx):
            MOD = MODS[li % 2]
            with contextlib.ExitStack() as ph:
                if MOD_OVERLAP and (li + 1) in layers and (li + 1) not in mod_done:
                    phase_mod(li + 1, stk=ph)
                work = sb(ph, nm('work'), [NE, TL], F32)
                m8 = sb(ph, nm('m8'), [NE, 8], F32)
                ones_r = sb(ph, nm('ones_r'), [NE, TL], F32)
                maskT = sb(ph, nm('maskT'), [NE, TT], F32)
                cum = sb(ph, nm('cum'), [NE, TT], F32)
                S.op('pool', lambda e: e.memset(ones_r.ap[:], 1.0), [], [ones_r])
                segs = [(TC, TL, CAPL)] + ([(0, TC, CAPC)] if with_ctx else [])
                for (t0, n, cap) in segs:
                    S.op('dve', lambda e: e.tensor_copy(out=work.ap[:, 0:n], in_=probsT.ap[:, t0:t0 + n]), [probsT], [work])
                    for it in range(cap // 8):
                        S.op('dve', lambda e: e.max(out=m8.ap[:], in_=work.ap[:, 0:n]), [work], [m8])
                        if it < cap // 8 - 1:
                            S.op('dve', lambda e: e.match_replace(out=work.ap[:, 0:n], in_to_replace=m8.ap[:], in_values=work.ap[:, 0:n],
                                                                  imm_value=-1.0), [m8, work], [work])
                    S.op('dve', lambda e: e.tensor_scalar(out=maskT.ap[:, t0:t0 + n], in0=probsT.ap[:, t0:t0 + n], scalar1=m8.ap[:, 7:8],
                                                          scalar2=None, op0=ALU.is_ge), [probsT, m8], [maskT])
                    S.op('dve', lambda e: e.tensor_tensor_scan(out=cum.ap[:, t0:t0 + n], data0=ones_r.ap[:, 0:n], data1=maskT.ap[:, t0:t0 + n],
                                                               initial=0.0, op0=ALU.mult, op1=ALU.add), [ones_r, maskT], [cum])
                    S.op('dve', lambda e: e.tensor_tensor(out=cum.ap[:, t0:t0 + n], in0=cum.ap[:, t0:t0 + n], in1=maskT.ap[:, t0:t0 + n],
                                                          op=ALU.mult), [cum, maskT], [cum])
                    S.op('dve', lambda e: e.tensor_scalar(out=cum.ap[:, t0:t0 + n], in0=cum.ap[:, t0:t0 + n], scalar1=-1.0, scalar2=None,
                                                          op0=ALU.add), [cum], [cum])
                if not with_ctx:
                    S.op('dve', lambda e: e.memset(cum.ap[:, 0:TC], -1.0), [], [cum])
                S.op('sp', lambda e: e.dma_start(out=POS[:, :], in_=cum.ap[:]), [cum], [tPOS], dma=True)
                gt = sb(ph, nm('gt'), [128, NE], F32)
                gh = sb(ph, nm('gh'), [128, NE], F32)
                for i in range(NT):
                    ps = psring.next()
                    S.op('pe', lambda e: e.transpose(out=ps.ap[:, 0:NE], in_=cum.ap[:, i * 128:(i + 1) * 128],
                                                     identity=Csb['ident_f'].ap[0:NE, 0:NE]), [cum, Csb['ident_f']], [ps])
                    S.op('dve', lambda e: e.tensor_copy(out=posTM.ap[:, i, :], in_=ps.ap[:, 0:NE]), [ps], [posTM])
                    S.op('dve', lambda e: e.scalar_tensor_tensor(out=gt.ap[:], in0=posTM.ap[:, i, :], scalar=0.0, in1=probs_tm.ap[:, i, :],
                                                                 op0=ALU.is_ge, op1=ALU.mult), [posTM, probs_tm], [gt])
                    S.op('dve', lambda e: e.tensor_copy(out=GHL.ap[:, i, :, 0], in_=gt.ap[:]), [gt], [GHL])
                    S.op('dve', lambda e: e.tensor_copy(out=gh.ap[:], in_=GHL.ap[:, i, :, 0]), [GHL], [gh])
                    S.op('dve', lambda e: e.tensor_tensor(out=GHL.ap[:, i, :, 1], in0=gt.ap[:], in1=gh.ap[:], op=ALU.subtract), [gt, gh], [GHL])
                S.barrier()
            if stop == 'm2':
                return
            jts = [(0, 128), (128, 128)] + ([(256, 32)] if with_ctx else [])
            NJ = 288 if with_ctx else 256
            with contextlib.ExitStack() as ph:
                h2 = sb(ph, nm('h2'), [128, NT, D], BF16)
                for i in range(0 if with_ctx else 2, NT):
                    S.op('sp', lambda e: e.dma_start(out=h2.ap[:, i, :], in_=H2TM[i * 128:(i + 1) * 128, :]), [tH2], [h2], dma=True)
                wring = Ring([sb(ph, nm('we'), [128, KT, 512], BF16) for _ in range(4)])
                selr = Ring([sb(ph, nm('sel'), [128, NT, 256], BF16) for _ in range(2)])
                xgr = Ring([sb(ph, nm('xg'), [128, KT, 288], BF16) for _ in range(1)])
                actTr = Ring([sb(ph, nm('actT'), [128, 8, 288], BF16) for _ in range(2)])
                sar = Ring([sb(ph, nm('sa'), [128, 512], F32) for _ in range(2)])
                ygr = Ring([sb(ph, nm('yg'), [128, 3, D], BF16) for _ in range(1)])
                gsr = Ring([sb(ph, nm('gs'), [128, 4], F32) for _ in range(2)])
                gsfr = Ring([sb(ph, nm('gsf'), [128, 8], F32) for _ in range(2)])
                def build_sel(ex):
                    sel = selr.next()
                    for i in range(0 if with_ctx else 2, NT):
                        n = 32 if i < 2 else 256
                        S.op('dve',
                             lambda e: e.tensor_scalar(out=sel.ap[:, i, 0:n], in0=Csb['iota_row'].ap[:, 0:n], scalar1=posTM.ap[:, i, ex:ex + 1],
                                                       scalar2=None, op0=ALU.is_equal), [Csb['iota_row'], posTM], [sel])
                    return sel
                sel_next = build_sel(0)
                for ex in range(NE):
                    sel = sel_next
                    gs = gsr.next()
                    psg = psring.next()
                    for ji, (j0, jn) in enumerate(jts):
                        tl = [0, 1] if j0 == 256 else list(range(2, NT))
                        for idx, i in enumerate(tl):
                            lo = 0 if j0 == 256 else j0
                            S.op('pe', lambda e: e.matmul(psg.ap[0:jn, ji * 2:ji * 2 + 2], lhsT=sel.ap[:, i, lo:lo + jn], rhs=GHL.ap[:, i, ex, :],
                                                          start=(idx == 0), stop=(idx == len(tl) - 1)), [sel, GHL], [psg])
                    gsf = gsfr.next()
                    for ji, (j0, jn) in enumerate(jts):
                        S.op('act', lambda e: e.copy(out=gsf.ap[0:jn, ji * 2:ji * 2 + 2], in_=psg.ap[0:jn, ji * 2:ji * 2 + 2]), [psg], [gsf])
                        S.op('dve', lambda e: e.tensor_tensor(out=gs.ap[0:jn, ji:ji + 1], in0=gsf.ap[0:jn, ji * 2:ji * 2 + 1],
                                                              in1=gsf.ap[0:jn, ji * 2 + 1:ji * 2 + 2], op=ALU.add), [gsf], [gs])
                    xg = xgr.next()
                    for dtile in range(KT):
                        ps = psring.next()
                        for i in range(2, NT):
                            S.op('pe', lambda e: e.matmul(ps.ap[:, 0:256], lhsT=h2.ap[:, i, dtile * 128:(dtile + 1) * 128], rhs=sel.ap[:, i, 0:256],
                                                          start=(i == 2), stop=(i == NT - 1)), [h2, sel], [ps])
                        if with_ctx:
                            for i in range(2):
                                S.op('pe', lambda e: e.matmul(ps.ap[:, 256:288], lhsT=h2.ap[:, i, dtile * 128:(dtile + 1) * 128], rhs=sel.ap[:, i, 0:32],
                                                              start=(i == 0), stop=(i == 1)), [h2, sel], [ps])
                        S.op('dve' if dtile % 2 else 'act',
                             (lambda e: e.tensor_copy(out=xg.ap[:, dtile, 0:NJ], in_=ps.ap[:, 0:NJ])) if dtile % 2 else
                             (lambda e: e.copy(out=xg.ap[:, dtile, 0:NJ], in_=ps.ap[:, 0:NJ])), [ps], [xg])
                    if ex + 1 < NE:
                        sel_next = build_sel(ex + 1)
                    actT = actTr.next()
                    for fc in range(2):
                        wg = wring.next()
                        S.op('pool', lambda e: e.dma_start(out=wg.ap[:], in_=I['moe_w_gate'][li, ex].rearrange("(kt p) f -> p kt f", p=128)[:, :, fc * 512:(fc + 1) * 512]),
                             [tIN], [wg], dma=True)
                        wu = wring.next()
                        S.op('pool', lambda e: e.dma_start(out=wu.ap[:], in_=I['moe_w_up'][li, ex].rearrange("(kt p) f -> p kt f", p=128)[:, :, fc * 512:(fc + 1) * 512]),
                             [tIN], [wu], dma=True)
                        for fl in range(4):
                            ft = fc * 4 + fl
                            pA = psring.next()
                            pU = psring.next()
                            for kt in range(KT):
                                S.op('pe', lambda e: e.matmul(pA.ap[:, 0:NJ], lhsT=wg.ap[:, kt, fl * 128:(fl + 1) * 128], rhs=xg.ap[:, kt, 0:NJ],
                                                              start=(kt == 0), stop=(kt == KT - 1)), [xg, wg], [pA])
                            for kt in range(KT):
                                S.op('pe', lambda e: e.matmul(pU.ap[:, 0:NJ], lhsT=wu.ap[:, kt, fl * 128:(fl + 1) * 128], rhs=xg.ap[:, kt, 0:NJ],
                                                              start=(kt == 0), stop=(kt == KT - 1)), [xg, wu], [pU])
                            sa = sar.next()
                            S.op('act', lambda e: e.activation(out=sa.ap[:, 0:NJ], in_=pA.ap[:, 0:NJ], func=AF.Silu), [pA], [sa])
                            S.op('dve', lambda e: e.tensor_tensor(out=actT.ap[:, ft, 0:NJ], in0=pU.ap[:, 0:NJ], in1=sa.ap[:, 0:NJ],
                                                                  op=ALU.mult), [pU, sa], [actT])
                    yg = ygr.next()
                    for dc in range(4):
                        wd = wring.next()
                        S.op('pool', lambda e: e.dma_start(out=wd.ap[:, 0:8, :], in_=I['moe_w_down'][li, ex].rearrange("(kt p) f -> p kt f", p=128)[:, :, dc * 512:(dc + 1) * 512]),
                             [tIN], [wd], dma=True)
                        for ji, (j0, jn) in enumerate(jts):
                            pY = psring.next()
                            for ft in range(8):
                                S.op('pe', lambda e: e.matmul(pY.ap[0:jn, :], lhsT=actT.ap[:, ft, j0:j0 + jn], rhs=wd.ap[:, ft, :],
                                                              start=(ft == 0), stop=(ft == 7)), [actT, wd], [pY])
                            if (dc + ji) % 2:
                                S.op('act', lambda e: e.mul(out=yg.ap[0:jn, ji, dc * 512:(dc + 1) * 512], in_=pY.ap[0:jn, :],
                                                            mul=gs.ap[0:jn, ji:ji + 1]), [pY, gs], [yg])
                            else:
                                S.op('dve', lambda e: e.tensor_scalar(out=yg.ap[0:jn, ji, dc * 512:(dc + 1) * 512], in0=pY.ap[0:jn, :],
                                                                      scalar1=gs.ap[0:jn, ji:ji + 1], scalar2=None, op0=ALU.mult), [pY, gs], [yg])
                    for ji, (j0, jn) in enumerate(jts):
                        S.op('sp', lambda e: e.dma_start(out=YG[ex, j0:j0 + jn, :], in_=yg.ap[0:jn, ji, :]), [yg], [tYG], dma=True)
                S.barrier()
            if stop == 'm3':
                return
            with contextlib.ExitStack() as ph:
                g_bc = [bcast_load(ph, 'g2', MOD[which, 5 * D:6 * D]) for which in (0, 1)]
                posr = Ring([sb(ph, nm('posb'), [128, NE, 512], F32) for _ in range(1)])
                selTs = [sb(ph, nm('selT'), [128, 2, 512], BF16) for _ in range(NE)]
                ygr = Ring([sb(ph, nm('ygc'), [128, NE, 2, 512], BF16) for _ in range(2)])
                xring = Ring([sb(ph, nm('xo'), [128, 512], F32) for _ in range(4)])
                tring = Ring([sb(ph, nm('to'), [128, 512], F32) for _ in range(3)])
                tchunks = [(256 + c * 512, 512, False) for c in range(4)]
                if with_ctx:
                    tchunks = [(0, 256, True)] + tchunks
                def yg_loader(t, dc, isctx):
                    if isctx:
                        S.op('sp', lambda e: e.dma_start(out=t.ap[0:32, :, 0, :], in_=YG[:, 256:288, dc * 512:(dc + 1) * 512].rearrange("e j d -> j e d")),
                             [tYG], [t], dma=True)
                    else:
                        for jt in range(2):
                            S.op('sp', lambda e: e.dma_start(out=t.ap[:, :, jt, :],
                                                             in_=YG[:, jt * 128:(jt + 1) * 128, dc * 512:(dc + 1) * 512].rearrange("e j d -> j e d")),
                                 [tYG], [t], dma=True)
                ygpf = Prefetch(ygr, [(lambda t, dc=dc, isctx=isctx: yg_loader(t, dc, isctx)) for (_t0, _n, isctx) in tchunks for dc in range(4)], 1)
                m4units = [(t0 // 128 + ti, dc) for (t0, n, isctx) in tchunks for dc in range(4) for ti in range(n // 128)]
                x4pf = Prefetch(xring, [(lambda t, i=i, dc=dc: S.op('sp', lambda e: e.dma_start(out=t.ap[:], in_=XR[i * 128:(i + 1) * 128, dc * 512:(dc + 1) * 512]),
                                                                     [tXR[i]], [t], dma=True)) for (i, dc) in m4units], 2)
                for (t0, n, isctx) in tchunks:
                    posb = posr.next()
                    S.op('sp', lambda e: e.dma_start(out=posb.ap[:, :, 0:n], in_=POS[:, t0:t0 + n].partition_broadcast(128)), [tPOS], [posb], dma=True)
                    np_ = 32 if isctx else 128
                    for ex in range(NE):
                        for jt in range(1 if isctx else 2):
                            S.op('dve',
                                 lambda e: e.tensor_scalar(out=selTs[ex].ap[0:np_, jt, 0:n], in0=posb.ap[0:np_, ex, 0:n],
                                                           scalar1=Csb['iota_part'].ap[0:np_, jt:jt + 1], scalar2=None, op0=ALU.is_equal),
                                 [posb, Csb['iota_part']], [selTs[ex]])
                    for dc in range(4):
                        ygc = ygpf.get(tchunks.index((t0, n, isctx)) * 4 + dc)
                        for ti in range(n // 128):
                            i = t0 // 128 + ti
                            which = 1 if isctx else 0
                            ps = psring.next()
                            pairs = [(ex, jt) for ex in range(NE) for jt in range(1 if isctx else 2)]
                            for idx, (ex, jt) in enumerate(pairs):
                                S.op('pe', lambda e: e.matmul(ps.ap[:], lhsT=selTs[ex].ap[0:np_, jt, ti * 128:(ti + 1) * 128], rhs=ygc.ap[0:np_, ex, jt, :],
                                                              start=(idx == 0), stop=(idx == len(pairs) - 1)), [selTs[ex], ygc], [ps])
                            xt = x4pf.get(m4units.index((i, dc)))
                            tm = tring.next()
                            S.op('dve', lambda e: e.tensor_tensor(out=tm.ap[:], in0=ps.ap[:], in1=g_bc[which].ap[:, dc * 512:(dc + 1) * 512], op=ALU.mult),
                                 [ps, g_bc[which]], [tm])
                            S.op('pool', lambda e: e.tensor_tensor(out=tm.ap[:], in0=tm.ap[:], in1=xt.ap[:], op=ALU.add), [tm, xt], [tm])
                            S.op('sp', lambda e: e.dma_start(out=XR[i * 128:(i + 1) * 128, dc * 512:(dc + 1) * 512], in_=tm.ap[:]), [tm], [tXR[i]], dma=True)
                S.barrier()

        S.barrier()
        for li in layers:
            kind = li % 3
            j = li // 3
            with_ctx = li < DEPTH - 1
            if stop == 'setup':
                break
            if li not in mod_done:
                phase_mod(li)
            if stop == 'mod':
                break
            if debug != 'mixer_skip':
                phase_norm(li, 0)
                if stop == 'norm1':
                    break
                if kind == 0:
                    phase_attn(li, j, with_ctx)
                elif kind == 1 and 'ret' in IMPLEMENTED:
                    phase_ret(li, j, with_ctx)
                elif kind == 2 and 'hy' in IMPLEMENTED:
                    phase_hyena(li, j, with_ctx)
                else:
                    pass
            if debug != 'moe_skip':
                import os as _os
                phase_norm(li, 1, want_tm=_os.environ.get("KTM", "1") == "1", router=_os.environ.get("KRT", "1") == "1")
                if stop == 'norm2':
                    break
                phase_moe(li, with_ctx)
        phase_norm(0, 0, final=True)
        S.barrier()
    print("instructions:", S.ninst)
    return nc, consts


_W_KEYS = ['c_ctx', 'w_mod', 'b_mod', 'norm_w', 'attn_w_qkv', 'attn_q_norm', 'attn_k_norm', 'attn_w_o',
           'ret_w_in', 'ret_decay_logit', 'ret_w_o',
           'hy_w_in', 'hy_conv_w', 'hy_conv_b', 'hy_f_w1', 'hy_f_b1', 'hy_f_freq1', 'hy_f_w2', 'hy_f_b2', 'hy_f_freq2',
           'hy_f_w3', 'hy_skip', 'hy_w_out',
           'moe_router', 'moe_w_gate', 'moe_w_up', 'moe_w_down', 'final_norm_w']


def run(inputs, layers=(0, 1, 2, 3), debug=None, cores=8, stop=None, small=None):
    nc, consts = build(layers, debug, stop, small)
    in_maps = []
    shared = {k: np.ascontiguousarray(np.asarray(inputs[k], dtype=np.float32)) for k in _W_KEYS}
    if small:
        dd = small.get('depth', DEPTH)
        ne = small.get('ne', NE)
        shared['w_mod'] = np.ascontiguousarray(shared['w_mod'][:dd])
        for k in ('moe_w_gate', 'moe_w_up', 'moe_w_down'):
            shared[k] = np.ascontiguousarray(shared[k][:dd, :ne])
    for k, v in consts.items():
        shared['k_' + k] = v
    for b in range(cores):
        m = dict(shared)
        m['x'] = np.ascontiguousarray(inputs['x'][b])
        m['c'] = np.ascontiguousarray(inputs['c'][b])
        m['ctx'] = np.ascontiguousarray(inputs['ctx'][b])
        in_maps.append(m)
    res = run_bass_kernel_spmd(nc, in_maps, core_ids=list(range(cores)))
    return np.stack([r['out'] for r in res.results], axis=0)


def kernel(**inputs):
    return run(inputs).astype(np.float32)
```
